# Optimizing a Trainium2 kernel written in Bass

```python
import math
import jax, jax.numpy as jnp
from jax import lax
import numpy as np

D_MODEL = 4096
BATCH = 1
SEQ = 8192
DEPTH = 4

N_MIXERS = 2
HEAD_DIM = 128
GQA_HEADS = D_MODEL // HEAD_DIM
GQA_KV_HEADS = GQA_HEADS // 4
DIFF_HEAD_DIM = 128
DIFF_HEADS = D_MODEL // (2 * DIFF_HEAD_DIM)
DIFF_V_DIM = 2 * DIFF_HEAD_DIM
N_EXPERTS = 16
EXPERT_FF = 896
EC_CAPACITY_FACTOR = 2
NUM_BUCKETS = 32
MAX_DISTANCE = 128
GRID_W = 64
BLOCK_Q = 128
ROPE_THETA = 10000.0
NORM_EPS = 1e-6

kernel_name = 'hybrid_axialgqa_diffattn_ecmoe_encoder'


def rms_norm(x, g):
    xf = x.astype(jnp.float32)
    y = xf * lax.rsqrt(jnp.mean(xf * xf, axis=-1, keepdims=True) + NORM_EPS)
    return (y * g.astype(jnp.float32)).astype(x.dtype)


def axial_rope(t, row, col):
    half = t.shape[-1] // 2
    freq = ROPE_THETA ** (-jnp.arange(0, half, 2, dtype=jnp.float32) / half)

    def rot(u, pos):
        ang = pos.astype(jnp.float32)[:, None] * freq[None, :]
        c = jnp.cos(ang)[None, :, None, :].astype(u.dtype)
        s = jnp.sin(ang)[None, :, None, :].astype(u.dtype)
        u1, u2 = jnp.split(u, 2, axis=-1)
        return jnp.concatenate([u1 * c - u2 * s, u2 * c + u1 * s], axis=-1)

    return jnp.concatenate([rot(t[..., :half], row), rot(t[..., half:], col)], axis=-1)


def t5_bucket(rel):
    nb = NUM_BUCKETS // 2
    max_exact = nb // 2
    n = jnp.abs(rel)
    large = max_exact + (jnp.log(jnp.maximum(n, 1).astype(jnp.float32) / max_exact)
                         / math.log(MAX_DISTANCE / max_exact) * (nb - max_exact)).astype(jnp.int32)
    large = jnp.minimum(large, nb - 1)
    return jnp.where(rel > 0, nb, 0) + jnp.where(n < max_exact, n, large)


def gqa_axial_mixer(h, w_in, q_gain, k_gain, w_out, row, col):
    B, S, _ = h.shape
    qd = GQA_HEADS * HEAD_DIM
    kd = GQA_KV_HEADS * HEAD_DIM
    proj = h @ w_in
    q = proj[..., :qd].reshape(B, S, GQA_HEADS, HEAD_DIM)
    k = proj[..., qd:qd + kd].reshape(B, S, GQA_KV_HEADS, HEAD_DIM)
    v = proj[..., qd + kd:].reshape(B, S, GQA_KV_HEADS, HEAD_DIM)
    q = axial_rope(rms_norm(q, q_gain), row, col)
    k = axial_rope(rms_norm(k, k_gain), row, col)
    G = GQA_HEADS // GQA_KV_HEADS
    nblk = S // BLOCK_Q
    qb = q.reshape(B, nblk, BLOCK_Q, GQA_KV_HEADS, G, HEAD_DIM).transpose(1, 0, 2, 3, 4, 5)
    scale = HEAD_DIM ** -0.5

    def block(qblk):
        s = jnp.einsum('bqkgd,bskd->bkgqs', qblk, k).astype(jnp.float32) * scale
        p = jax.nn.softmax(s, axis=-1).astype(v.dtype)
        return jnp.einsum('bkgqs,bskd->bqkgd', p, v)

    o = lax.map(block, qb)
    o = o.transpose(1, 0, 2, 3, 4, 5).reshape(B, S, qd)
    return o @ w_out


def diff_mixer(h, w_in, lq1, lk1, lq2, lk2, sub_gain, w_out, rel_bias, lambda_init):
    B, S, _ = h.shape
    qk = DIFF_HEADS * 2 * DIFF_HEAD_DIM
    proj = h @ w_in
    q = proj[..., :qk].reshape(B, S, DIFF_HEADS, 2, DIFF_HEAD_DIM)
    k = proj[..., qk:2 * qk].reshape(B, S, DIFF_HEADS, 2, DIFF_HEAD_DIM)
    v = proj[..., 2 * qk:].reshape(B, S, DIFF_HEADS, DIFF_V_DIM)
    f32 = jnp.float32
    lam = (jnp.exp(jnp.sum(lq1.astype(f32) * lk1.astype(f32)))
           - jnp.exp(jnp.sum(lq2.astype(f32) * lk2.astype(f32))) + lambda_init)
    nblk = S // BLOCK_Q
    qb = q.reshape(B, nblk, BLOCK_Q, DIFF_HEADS, 2, DIFF_HEAD_DIM).transpose(1, 0, 2, 3, 4, 5)
    starts = jnp.arange(nblk, dtype=jnp.int32) * BLOCK_Q
    kpos = jnp.arange(S, dtype=jnp.int32)
    scale = DIFF_HEAD_DIM ** -0.5

    def block(args):
        qblk, q0 = args
        qpos = q0 + jnp.arange(BLOCK_Q, dtype=jnp.int32)
        bucket = t5_bucket(kpos[None, :] - qpos[:, None])
        bias = jnp.transpose(rel_bias[bucket].astype(f32), (2, 0, 1))
        s = jnp.einsum('bqhmd,bshmd->bhmqs', qblk, k).astype(f32) * scale + bias[None, :, None]
        p = jax.nn.softmax(s, axis=-1)
        a = (p[:, :, 0] - lam * p[:, :, 1]).astype(v.dtype)
        return jnp.einsum('bhqs,bshe->bqhe', a, v)

    o = lax.map(block, (qb, starts))
    o = o.transpose(1, 0, 2, 3, 4).reshape(B, S, DIFF_HEADS, DIFF_V_DIM)
    o = rms_norm(o, sub_gain) * (1.0 - lambda_init)
    return o.reshape(B, S, DIFF_HEADS * DIFF_V_DIM) @ w_out


def expert_choice_ffn(h, router_w, w_gate, w_up, w_down):
    B, S, D = h.shape
    E = w_gate.shape[0]
    cap = EC_CAPACITY_FACTOR * S // E
    logits = jnp.einsum('bsd,de->bse', h, router_w).astype(jnp.float32)
    aff = jax.nn.softmax(logits, axis=-1)
    gate, idx = lax.top_k(jnp.transpose(aff, (0, 2, 1)), cap)
    flat_idx = idx + (jnp.arange(B, dtype=jnp.int32) * S)[:, None, None]
    h_flat = h.reshape(B * S, D)
    xin = h_flat[flat_idx]
    g = jnp.einsum('becd,edf->becf', xin, w_gate)
    u = jnp.einsum('becd,edf->becf', xin, w_up)
    y = jnp.einsum('becf,efd->becd', jax.nn.silu(g) * u, w_down)
    y = y * gate[..., None].astype(y.dtype)
    out = jnp.zeros_like(h_flat).at[flat_idx.reshape(-1)].add(y.reshape(-1, D))
    return out.reshape(B, S, D)


def setup_inputs(seed: int = 0) -> dict:
    key = jax.random.key(seed)
    ks = jax.random.split(key, 24)
    f32 = jnp.float32
    n_a = len(range(0, DEPTH, N_MIXERS))
    n_b = DEPTH - n_a
    qd = GQA_HEADS * HEAD_DIM
    kd = GQA_KV_HEADS * HEAD_DIM
    dqk = DIFF_HEADS * 2 * DIFF_HEAD_DIM
    dv = DIFF_HEADS * DIFF_V_DIM

    def nrm(k, shape, scale):
        return jax.random.normal(k, shape, f32) * scale

    def gain(k, shape):
        return 1.0 + 0.02 * jax.random.normal(k, shape, f32)

    return {
        'x': nrm(ks[0], (BATCH, SEQ, D_MODEL), 1.0),
        'attn_norm': gain(ks[1], (DEPTH, D_MODEL)),
        'ffn_norm': gain(ks[2], (DEPTH, D_MODEL)),
        'final_norm': gain(ks[3], (D_MODEL,)),
        'gqa_w_in': nrm(ks[4], (n_a, D_MODEL, qd + 2 * kd), D_MODEL ** -0.5),
        'gqa_q_norm': gain(ks[5], (n_a, HEAD_DIM)),
        'gqa_k_norm': gain(ks[6], (n_a, HEAD_DIM)),
        'gqa_w_out': nrm(ks[7], (n_a, qd, D_MODEL), qd ** -0.5),
        'diff_w_in': nrm(ks[8], (n_b, D_MODEL, 2 * dqk + dv), D_MODEL ** -0.5),
        'diff_lambda_q1': nrm(ks[9], (n_b, DIFF_HEAD_DIM), 0.1),
        'diff_lambda_k1': nrm(ks[10], (n_b, DIFF_HEAD_DIM), 0.1),
        'diff_lambda_q2': nrm(ks[11], (n_b, DIFF_HEAD_DIM), 0.1),
        'diff_lambda_k2': nrm(ks[12], (n_b, DIFF_HEAD_DIM), 0.1),
        'diff_sub_norm': gain(ks[13], (n_b, DIFF_V_DIM)),
        'diff_w_out': nrm(ks[14], (n_b, dv, D_MODEL), dv ** -0.5),
        'rel_bias': nrm(ks[15], (NUM_BUCKETS, DIFF_HEADS), 0.2),
        'router_w': nrm(ks[16], (DEPTH, D_MODEL, N_EXPERTS), D_MODEL ** -0.5),
        'expert_w_gate': nrm(ks[17], (DEPTH, N_EXPERTS, D_MODEL, EXPERT_FF), D_MODEL ** -0.5),
        'expert_w_up': nrm(ks[18], (DEPTH, N_EXPERTS, D_MODEL, EXPERT_FF), D_MODEL ** -0.5),
        'expert_w_down': nrm(ks[19], (DEPTH, N_EXPERTS, EXPERT_FF, D_MODEL), EXPERT_FF ** -0.5),
    }


def reference(x, attn_norm, ffn_norm, final_norm, gqa_w_in, gqa_q_norm, gqa_k_norm, gqa_w_out,
              diff_w_in, diff_lambda_q1, diff_lambda_k1, diff_lambda_q2, diff_lambda_k2,
              diff_sub_norm, diff_w_out, rel_bias, router_w, expert_w_gate, expert_w_up,
              expert_w_down):
    S = x.shape[1]
    rows = S // GRID_W
    row = jnp.repeat(jnp.arange(rows, dtype=jnp.int32), GRID_W)
    col = jnp.tile(jnp.arange(GRID_W, dtype=jnp.int32), rows)
    for i in range(DEPTH):
        h = rms_norm(x, attn_norm[i])
        j = i // N_MIXERS
        if i % N_MIXERS == 0:
            x = x + gqa_axial_mixer(h, gqa_w_in[j], gqa_q_norm[j], gqa_k_norm[j], gqa_w_out[j], row, col)
        else:
            lambda_init = 0.8 - 0.6 * math.exp(-0.3 * i)
            x = x + diff_mixer(h, diff_w_in[j], diff_lambda_q1[j], diff_lambda_k1[j],
                               diff_lambda_q2[j], diff_lambda_k2[j], diff_sub_norm[j],
                               diff_w_out[j], rel_bias, lambda_init)
        h = rms_norm(x, ffn_norm[i])
        x = x + expert_choice_ffn(h, router_w[i], expert_w_gate[i], expert_w_up[i], expert_w_down[i])
    return rms_norm(x, final_norm)
```

```python
import math
from contextlib import ExitStack
import numpy as np
import concourse.bass as bass
import concourse.mybir as mybir
from concourse.bass_utils import run_bass_kernel_spmd

F32 = mybir.dt.float32
BF16 = mybir.dt.bfloat16
I32 = mybir.dt.int32
U32 = mybir.dt.uint32
AF = mybir.ActivationFunctionType
ALU = mybir.AluOpType
AX = mybir.AxisListType

EPS = 1e-6


class Cfg:
    def __init__(self, NC=8, S=8192, D=4096, E=16, FF=896, GRID_W=64, DEPTH=4):
        self.NC, self.S, self.D, self.E, self.FF, self.GRID_W, self.DEPTH = NC, S, D, E, FF, GRID_W, DEPTH
        self.KC = D // 128
        self.NT = S // 128
        self.CAP = 2 * S // E
        self.CT = self.CAP // 128
        self.FC = FF // 128
        self.QH = 4 * NC
        self.KVH = NC
        self.DH = 2 * NC
        self.CW = D // NC
        assert self.CW == 512 and E == 2 * NC


class Em:
    def __init__(self, nc, es):
        self.nc, self.es = nc, es
        self.E = {'pe': nc.tensor, 'act': nc.scalar, 'dve': nc.vector, 'pool': nc.gpsimd, 'sp': nc.sync}
        self.esem = {e: es.enter_context(nc.semaphore('s_' + e)) for e in ['pe', 'act', 'dve', 'pool']}
        self.ecnt = {e: 0 for e in self.esem}
        self.waited = {e: {} for e in self.E}
        self.dsem = {}
        self.res = {}
        self.limit = None
        self.ncalls = 0

    def _wait(self, eng, evs):
        need = {}
        for (name, sem, val, src) in evs:
            if src == 'pe' and eng == 'pe':
                continue
            if val <= 0:
                continue
            if need.get(name, (None, 0))[1] < val:
                need[name] = (sem, val)
        for name, (sem, val) in need.items():
            if self.waited[eng].get(name, 0) >= val:
                continue
            self.E[eng].wait_ge(sem, val)
            self.waited[eng][name] = val

    def _deps(self, eng, reads, writes):
        evs = []
        for r in reads:
            st = self.res.get(r)
            if st and st['w']:
                evs.append(st['w'])
        for w in writes:
            st = self.res.get(w)
            if st:
                if st['w']:
                    evs.append(st['w'])
                evs.extend(st['r'].values())
        self._wait(eng, evs)

    def _record(self, ev, reads, writes):
        for r in reads:
            st = self.res.setdefault(r, {'w': None, 'r': {}})
            old = st['r'].get(ev[0])
            if old is None or old[2] < ev[2]:
                st['r'][ev[0]] = ev
        for w in writes:
            self.res[w] = {'w': ev, 'r': {}}

    def _lim(self):
        self.ncalls += 1
        return self.limit is not None and self.ncalls > self.limit

    def op(self, eng, fn, reads, writes, *args, **kw):
        if self._lim():
            return None
        self._deps(eng, reads, writes)
        ins = getattr(self.E[eng], fn)(*args, **kw)
        self.ecnt[eng] += 1
        ins.then_inc(self.esem[eng], 1)
        ev = ('s_' + eng, self.esem[eng], self.ecnt[eng], eng)
        self._record(ev, reads, writes)
        return ins

    def mm(self, out, pairs, reads, writes, transpose=False):
        if self._lim():
            return
        self._deps('pe', reads, writes)
        n = len(pairs)
        ins = None
        for i, (a, b) in enumerate(pairs):
            if transpose:
                ins = self.nc.tensor.transpose(out=out, in_=a, identity=b)
            else:
                ins = self.nc.tensor.matmul(out, a, b, start=(i == 0), stop=(i == n - 1))
        self.ecnt['pe'] += 1
        ins.then_inc(self.esem['pe'], 1)
        ev = ('s_pe', self.esem['pe'], self.ecnt['pe'], 'pe')
        self._record(ev, reads, writes)

    def mm_multi(self, items, reads, writes):
        if self._lim():
            return
        self._deps('pe', reads, writes)
        ins = None
        for (o, a, b, tr) in items:
            if tr:
                ins = self.nc.tensor.transpose(out=o, in_=a, identity=b)
            else:
                ins = self.nc.tensor.matmul(o, a, b, start=True, stop=True)
        self.ecnt['pe'] += 1
        ins.then_inc(self.esem['pe'], 1)
        ev = ('s_pe', self.esem['pe'], self.ecnt['pe'], 'pe')
        self._record(ev, reads, writes)

    def _dkey(self, key):
        if key not in self.dsem:
            self.dsem[key] = [self.es.enter_context(self.nc.semaphore('d_' + key)), 0]
        return self.dsem[key]

    def dma(self, eng, key, out, in_, reads, writes, new_group=True, indirect=None, **kw):
        if self._lim():
            return
        ent = self._dkey(key)
        if new_group:
            self._wait(eng, [('d_' + key, ent[0], ent[1], 'dma')])
        self._deps(eng, reads, writes)
        if indirect is None:
            ins = self.E[eng].dma_start(out=out, in_=in_, **kw)
        else:
            ins = self.nc.gpsimd.indirect_dma_start(out=out, in_=in_, **indirect, **kw)
        ent[1] += 16
        ins.then_inc(ent[0], 16)
        ev = ('d_' + key, ent[0], ent[1], 'dma')
        self._record(ev, reads, writes)

    def barrier(self):
        evs = [('s_' + e, self.esem[e], self.ecnt[e], 'x') for e in self.esem]
        evs += [('d_' + k, v[0], v[1], 'dma') for k, v in self.dsem.items()]
        for eng in self.E:
            self._wait(eng, evs)
        self.res = {}

    def finish(self):
        evs = [('s_' + e, self.esem[e], self.ecnt[e], 'x') for e in self.esem]
        evs += [('d_' + k, v[0], v[1], 'dma') for k, v in self.dsem.items()]
        self._wait('sp', evs)


_UNIQ = [0]


def _sb(es, nc, name, shape, dt):
    _UNIQ[0] += 1
    return es.enter_context(nc.sbuf_tensor('%s_%d' % (name, _UNIQ[0]), list(shape), dt))


def _ps(es, nc, name, shape, dt):
    _UNIQ[0] += 1
    return es.enter_context(nc.psum_tensor('%s_%d' % (name, _UNIQ[0]), list(shape), dt))


def _consts(em, es, nc):
    idf = _sb(es, nc, 'idf', [128, 128], F32)
    idb = _sb(es, nc, 'idb', [128, 128], BF16)
    onesb = _sb(es, nc, 'onesb', [128, 128], BF16)
    onesf = _sb(es, nc, 'onesf', [128, 128], F32)
    em.op('pool', 'memset', [], ['idf'], idf[:], 0.0)
    em.op('pool', 'affine_select', ['idf'], ['idf'], out=idf[:], in_=idf[:], pattern=[[-1, 128]],
          compare_op=ALU.not_equal, fill=1.0, base=0, channel_multiplier=1)
    em.op('dve', 'tensor_copy', ['idf'], ['idb'], out=idb[:], in_=idf[:])
    em.op('pool', 'memset', [], ['onesb'], onesb[:], 1.0)
    em.op('pool', 'memset', [], ['onesf'], onesf[:], 1.0)
    return idf, idb, onesb, onesf


def _rstd(em, nc, ss, rs, tmp, n, key_ss, key_rs, key_tmp, epsb):
    em.op('act', 'activation', [key_ss, 'epsb'], [key_tmp], out=tmp, in_=ss, func=AF.Sqrt, bias=epsb, scale=1.0 / n)
    em.op('dve', 'reciprocal', [key_tmp], [key_rs], out=rs, in_=tmp)


def _norm_transpose_tile(em, nc, cfg, t, x_ap, bufs):
    KC, D, NT = cfg.KC, cfg.D, cfg.NT
    xts, hbf, hTs, gcol, ss, rs, tmp, epsb, idb, pst = bufs
    nx = len(xts)
    if t == 0:
        em.dma('sp', 'xin0', xts[0][:], x_ap[0:128, :], [], ['xt0'])
    if nx > 1 and t + 1 < NT:
        b = (t + 1) % nx
        em.dma('sp', 'xin%d' % b, xts[b][:], x_ap[(t + 1) * 128:(t + 2) * 128, :], [], ['xt%d' % b])
    elif nx == 1 and t > 0:
        em.dma('sp', 'xin0', xts[0][:], x_ap[t * 128:(t + 1) * 128, :], [], ['xt0'])
    xk = 'xt%d' % (t % nx)
    xt = xts[t % nx]
    par = t % len(hTs)
    hT, hk = hTs[par], 'hT%d' % par
    c = par
    sk, rk, tk = 'ss%d' % c, 'rs%d' % c, 'tmp%d' % c
    em.op('act', 'activation', [xk], ['hbf', sk], out=hbf[:], in_=xt[:], func=AF.Square, accum_out=ss[:, c:c + 1])
    _rstd(em, nc, ss[:, c:c + 1], rs[:, c:c + 1], tmp[:, c:c + 1], D, sk, rk, tk, epsb[:, 0:1])
    em.op('dve', 'tensor_scalar', [xk, rk], ['hbf'], out=hbf[:], in0=xt[:], scalar1=rs[:, c:c + 1], scalar2=None, op0=ALU.mult)
    nb = KC // 8 if KC >= 8 else 1
    per = min(8, KC)
    for b in range(nb):
        pk = 'pst%d' % (b % 2)
        p = pst[b % 2]
        items = [(p[:, j, :], hbf[:, (b * per + j) * 128:(b * per + j + 1) * 128], idb[:], True) for j in range(per)]
        em.mm_multi(items, ['hbf', 'idb'], [pk])
        g_b = gcol[:, b * per:(b + 1) * per]
        em.op('dve', 'tensor_tensor', [pk, 'gcol'], [hk], out=hT[:, b * per:(b + 1) * per, :], in0=p[:, 0:per, :],
              in1=_bc_last(g_b, 128), op=ALU.mult)
    return hT, hk


def _bc_last(ap2d, n):
    t = ap2d
    return bass.AP(tensor=t.tensor, offset=t.offset, ap=list(t.ap) + [[0, n]])


def _bc_mid(ap2d, m):
    t = ap2d
    a = list(t.ap)
    return bass.AP(tensor=t.tensor, offset=t.offset, ap=[a[0], [0, m]] + a[1:])


def build_att(cfg, kind, lambda_init=0.0, stop=None):
    S, D, KC, NT = cfg.S, cfg.D, cfg.KC, cfg.NT
    gqa = kind == 'gqa'
    NP = 1 if gqa else 2
    nc = bass.Bass("TRN2", target_bir_lowering=False)
    x = nc.dram_tensor("x", [S, D], F32, kind="ExternalInput").ap()
    gn = nc.dram_tensor("gn", [D], F32, kind="ExternalInput").ap()
    w = nc.dram_tensor("w", [NP, D, 768], F32, kind="ExternalInput").ap()
    if gqa:
        qkg = nc.dram_tensor("qkg", [5 * 128], F32, kind="ExternalInput").ap()
        rope = nc.dram_tensor("rope", [S, 128], F32, kind="ExternalInput").ap()
    else:
        lamv = nc.dram_tensor("lam", [4 * 128], F32, kind="ExternalInput").ap()
        subg = nc.dram_tensor("subg", [256], F32, kind="ExternalInput").ap()
        rb = nc.dram_tensor("rb", [64], F32, kind="ExternalInput").ap()
        bmap = nc.dram_tensor("bmap", [128, 384], F32, kind="ExternalInput").ap()
    oT = nc.dram_tensor("oT", [512, S], BF16, kind="ExternalOutput").ap()
    scale = 128 ** -0.5

    with ExitStack() as es:
        em = Em(nc, es)
        idf, idb, onesb, onesf = _consts(em, es, nc)
        epsb = _sb(es, nc, 'epsb', [128, 1], F32)
        em.op('pool', 'memset', [], ['epsb'], epsb[:], EPS)
        gcol = _sb(es, nc, 'gcol', [128, KC], F32)
        em.dma('sp', 'c0', gcol[:], gn.rearrange("(kc p) -> p kc", p=128), [], ['gcol'], allow_slow_non_contiguous=True)
        QT = _sb(es, nc, 'QT', [128, 4, S], BF16)
        if gqa:
            KT = _sb(es, nc, 'KT', [128, S], BF16)
            V = _sb(es, nc, 'V', [128, NT, 128], BF16)
        else:
            V = _sb(es, nc, 'V', [128, NT, 256], BF16)
        if gqa:
            gq = _sb(es, nc, 'gq', [128, 5, 128], F32)
            em.dma('sp', 'c1', gq[:], qkg.rearrange("(h d) -> h d", d=128).partition_broadcast(128), [], ['gq'])
        else:
            lam4 = _sb(es, nc, 'lam4', [128, 4, 128], F32)
            em.dma('sp', 'c1', lam4[:], lamv.rearrange("(h d) -> h d", d=128).partition_broadcast(128), [], ['lam4'])
            sgc = _sb(es, nc, 'sgc', [128, 2], F32)
            em.dma('sp', 'c2', sgc[:], subg.rearrange("(h p) -> p h", p=128), [], ['sgc'], allow_slow_non_contiguous=True)
            rbb = _sb(es, nc, 'rbb', [128, 64], F32)
            em.dma('sp', 'c3', rbb[:], rb.rearrange("(o n) -> o n", o=1).partition_broadcast(128), [], ['rbb'])
            bm = _sb(es, nc, 'bm', [128, 384], F32)
            em.dma('sp', 'c4', bm[:], bmap[:, :], [], ['bm'])
            lp = _sb(es, nc, 'lp', [128, 2, 128], F32)
            ls = _sb(es, nc, 'ls', [128, 4], F32)
            em.op('dve', 'tensor_tensor', ['lam4'], ['lp'], out=lp[:], in0=lam4[:, 0:4:2, :], in1=lam4[:, 1:4:2, :], op=ALU.mult)
            em.op('dve', 'tensor_reduce', ['lp'], ['ls'], out=ls[:, 0:2], in_=lp[:], axis=AX.X, op=ALU.add)
            em.op('act', 'activation', ['ls'], ['ls'], out=ls[:, 0:2], in_=ls[:, 0:2], func=AF.Exp)
            em.op('dve', 'tensor_tensor', ['ls'], ['ls'], out=ls[:, 2:3], in0=ls[:, 1:2], in1=ls[:, 0:1], op=ALU.subtract)
            em.op('dve', 'tensor_scalar', ['ls'], ['ls'], out=ls[:, 3:4], in0=ls[:, 2:3], scalar1=-float(lambda_init), scalar2=None, op0=ALU.add)
            neglam = ls[:, 3:4]
            BT = _sb(es, nc, 'BT', [128, 2, 384], F32)
            btmp = _sb(es, nc, 'btmp', [128, 384], F32)
            em.op('pool', 'memset', [], ['BT'], BT[:], 0.0)
            for h in range(2):
                for b in range(32):
                    em.op('dve', 'tensor_scalar', ['bm', 'rbb'], ['btmp'], out=btmp[:], in0=bm[:], scalar1=float(b),
                          scalar2=rbb[:, b * 2 + h:b * 2 + h + 1], op0=ALU.is_equal, op1=ALU.mult)
                    em.op('dve', 'tensor_tensor', ['btmp', 'BT'], ['BT'], out=BT[:, h, :], in0=BT[:, h, :], in1=btmp[:], op=ALU.add)

        for ps_i in range(NP):
            if stop == 'consts':
                break
            em.barrier()
            with ExitStack() as pes:
                W = _sb(pes, nc, 'W', [128, KC, 768], BF16)
                for kc in range(KC):
                    em.dma('pool', 'wl', W[:, kc, :], w[ps_i, kc * 128:(kc + 1) * 128, :], [], ['W'], new_group=(kc == 0))
                xt = [_sb(pes, nc, 'xt%d' % q_, [128, D], F32) for q_ in range(1)]
                hbf = _sb(pes, nc, 'hbf', [128, D], BF16)
                hTs = [_sb(pes, nc, 'hT%d' % q_, [128, KC, 128], BF16) for q_ in range(2)]
                ss = _sb(pes, nc, 'ss', [128, 8], F32)
                rs = _sb(pes, nc, 'rs', [128, 8], F32)
                tmp = _sb(pes, nc, 'tmp', [128, 8], F32)
                pst = [_ps(pes, nc, 'pst%d' % i, [128, 8, 128], BF16) for i in range(2)]
                ppA = [_ps(pes, nc, 'ppA%d' % i, [128, 512], F32) for i in range(2)]
                ppB = [_ps(pes, nc, 'ppB%d' % i, [128, 512], F32) for i in range(2)]
                ptr = _ps(pes, nc, 'ptr', [128, 8, 128], BF16)
                if gqa:
                    sq = [_sb(pes, nc, 'sq%d' % q_, [128, 5, 128], F32) for q_ in range(2)]
                    qn = [_sb(pes, nc, 'qn%d' % q_, [128, 5, 128], F32) for q_ in range(2)]
                    qr = [_sb(pes, nc, 'qr%d' % q_, [128, 5, 128], BF16) for q_ in range(2)]
                    rt = [_sb(pes, nc, 'rt%d' % q_, [128, 128], F32) for q_ in range(2)]
                    t1 = [_sb(pes, nc, 't1%d' % q_, [128, 5, 2, 32], F32) for q_ in range(2)]
                    t2 = [_sb(pes, nc, 't2%d' % q_, [128, 5, 2, 32], F32) for q_ in range(2)]
                    ssh = _sb(pes, nc, 'ssh', [128, 16], F32)
                    rsh = _sb(pes, nc, 'rsh', [128, 16], F32)
                    tmh = _sb(pes, nc, 'tmh', [128, 16], F32)
                else:
                    qb16 = [_sb(pes, nc, 'qb16%d' % q_, [128, 4, 128], BF16) for q_ in range(2)]
                bufs = (xt, hbf, hTs, gcol, ss, rs, tmp, epsb, idb, pst)
                for t in range(NT):
                    c = t % 2
                    hT, hk = _norm_transpose_tile(em, nc, cfg, t, x, bufs)
                    pA, pB, kA, kB = ppA[c], ppB[c], 'ppA%d' % c, 'ppB%d' % c
                    em.mm(pA[:, :], [(hT[:, kc, :], W[:, kc, 0:512]) for kc in range(KC)], [hk, 'W'], [kA])
                    em.mm(pB[:, 0:256], [(hT[:, kc, :], W[:, kc, 512:768]) for kc in range(KC)], [hk, 'W'], [kB])
                    if gqa:
                        sqk, qnk, qrk, rtk, t1k, t2k = 'sq%d' % c, 'qn%d' % c, 'qr%d' % c, 'rt%d' % c, 't1%d' % c, 't2%d' % c
                        sshk, rshk, tmhk = 'ssh%d' % c, 'rsh%d' % c, 'tmh%d' % c
                        o8 = c * 8
                        em.dma('sp', 'rt%d' % c, rt[c][:], rope[t * 128:(t + 1) * 128, :], [], [rtk])
                        em.op('act', 'activation', [kA], [sqk], out=sq[c][:, 0:4, :], in_=pA[:, :].rearrange("p (h d) -> p h d", d=128), func=AF.Square)
                        em.op('act', 'activation', [kB], [sqk], out=sq[c][:, 4, :], in_=pB[:, 0:128], func=AF.Square)
                        em.op('act', 'copy', [kB], [('V', t)], out=V[:, t, :], in_=pB[:, 128:256])
                        em.op('dve', 'tensor_reduce', [sqk], [sshk], out=ssh[:, o8:o8 + 5], in_=sq[c][:], axis=AX.X, op=ALU.add)
                        _rstd(em, nc, ssh[:, o8:o8 + 5], rsh[:, o8:o8 + 5], tmh[:, o8:o8 + 5], 128, sshk, rshk, tmhk, epsb[:, 0:1])
                        em.op('dve', 'tensor_tensor', [kA, rshk], [qnk], out=qn[c][:, 0:4, :], in0=pA[:, :].rearrange("p (h d) -> p h d", d=128),
                              in1=_bc_last(rsh[:, o8:o8 + 4], 128), op=ALU.mult)
                        em.op('dve', 'tensor_tensor', [kB, rshk], [qnk], out=qn[c][:, 4, :], in0=pB[:, 0:128],
                              in1=rsh[:, o8 + 4:o8 + 5].to_broadcast([128, 128]), op=ALU.mult)
                        em.op('dve', 'tensor_tensor', [qnk, 'gq'], [qnk], out=qn[c][:], in0=qn[c][:], in1=gq[:], op=ALU.mult)
                        qv = qn[c][:].rearrange("p h (hh ab i) -> p h hh ab i", hh=2, ab=2)
                        qo = qr[c][:].rearrange("p h (hh ab i) -> p h hh ab i", hh=2, ab=2)
                        A_, B_ = qv[:, :, :, 0, :], qv[:, :, :, 1, :]
                        C_ = _bc_mid(rt[c][:, 0:64], 5).rearrange("p h (hh i) -> p h hh i", hh=2)
                        S_ = _bc_mid(rt[c][:, 64:128], 5).rearrange("p h (hh i) -> p h hh i", hh=2)
                        em.op('dve', 'tensor_tensor', [qnk, rtk], [t1k], out=t1[c][:], in0=A_, in1=C_, op=ALU.mult)
                        em.op('pool', 'tensor_tensor', [qnk, rtk], [t2k], out=t2[c][:], in0=B_, in1=S_, op=ALU.mult)
                        em.op('dve', 'tensor_tensor', [t1k, t2k], [qrk], out=qo[:, :, :, 0, :], in0=t1[c][:], in1=t2[c][:], op=ALU.subtract)
                        em.op('dve', 'tensor_tensor', [qnk, rtk], [t1k], out=t1[c][:], in0=B_, in1=C_, op=ALU.mult)
                        em.op('pool', 'tensor_tensor', [qnk, rtk], [t2k], out=t2[c][:], in0=A_, in1=S_, op=ALU.mult)
                        em.op('dve', 'tensor_tensor', [t1k, t2k], [qrk], out=qo[:, :, :, 1, :], in0=t1[c][:], in1=t2[c][:], op=ALU.add)
                        em.mm_multi([(ptr[:, j, :], qr[c][:, j, :], idb[:], True) for j in range(5)], [qrk, 'idb'], ['ptr'])
                        em.op('act', 'copy', ['ptr'], [('QT', t)], out=QT[:, :, t * 128:(t + 1) * 128], in_=ptr[:, 0:4, :])
                        em.op('act', 'copy', ['ptr'], [('KT', t)], out=KT[:, t * 128:(t + 1) * 128], in_=ptr[:, 4, :])
                    else:
                        qk_ = 'qb16%d' % c
                        em.op('act', 'copy', [kA], [qk_], out=qb16[c][:], in_=pA[:, :].rearrange("p (h d) -> p h d", d=128))
                        em.op('act', 'copy', [kB], [('V', t)], out=V[:, t, :], in_=pB[:, 0:256])
                        em.mm_multi([(ptr[:, j, :], qb16[c][:, j, :], idb[:], True) for j in range(4)], [qk_, 'idb'], ['ptr'])
                        em.op('act', 'copy', ['ptr'], [('QT', t)], out=QT[:, :, t * 128:(t + 1) * 128], in_=ptr[:, 0:4, :])

            if stop in ('tile0', 'proj', 'nt') or isinstance(stop, int):
                break
            em.barrier()
            with ExitStack() as pes:
                NB = 3 if gqa else 2
                pS = [_ps(pes, nc, 'pS%d' % i, [128, 512], F32) for i in range(NB)]
                pT = [_sb(pes, nc, 'pT%d' % i, [128, 512], BF16) for i in range(NB)]
                if gqa:
                    pO = [[_ps(pes, nc, 'pO%d' % a, [128, 512], F32)] for a in range(2)]
                else:
                    pO = [[_ps(pes, nc, 'pO%d_%d' % (a, b), [128, 512], F32) for b in range(2)] for a in range(2)]
                pL = [_ps(pes, nc, 'pL%d' % a, [128, 512], F32) for a in range(2)]
                rl = _sb(pes, nc, 'rl', [128, 512], F32)
                accs = {e_: [_sb(pes, nc, 'acc%s%d' % (e_, a_), [128, 512], F32) for a_ in range(2)] for e_ in ('dve', 'pool')}
                if gqa:
                    steps = [(qb, 0, kt) for qb in range(NT) for kt in range(NT)]
                else:
                    h = ps_i
                    o1 = _sb(pes, nc, 'o1', [128, 2, 512], F32)
                    o2 = _sb(pes, nc, 'o2', [128, 2, 512], F32)
                    sq2 = _sb(pes, nc, 'sq2', [128, 2, 512], BF16)
                    sbs = [_sb(pes, nc, 'sbias%d' % q_, [128, 128], F32) for q_ in range(4)]
                    steps = [(qb, m, kt) for qb in range(S // 512) for m in range(2) for kt in range(NT)]

                def qkeys(qb):
                    return [('QT', qb)] if gqa else [('QT', j) for j in range(qb * 4, qb * 4 + 4)]

                def emit_S(s):
                    qb, m, kt = steps[s]
                    i = s % NB
                    if gqa:
                        em.mm(pS[i][:, :].rearrange("p (h q) -> p h q", h=4), [(KT[:, kt * 128:(kt + 1) * 128], QT[:, :, qb * 128:(qb + 1) * 128])],
                              [('QT', qb), ('KT', kt)], ['pS%d' % i])
                    else:
                        em.mm(pS[i][:, :], [(QT[:, 2 + m, kt * 128:(kt + 1) * 128], QT[:, m, qb * 512:(qb + 1) * 512])],
                              qkeys(qb) + [('QT', kt)], ['pS%d' % i, 'pSd%d' % i])

                def emit_exp(s):
                    qb, m, kt = steps[s]
                    i = s % NB
                    if gqa:
                        em.op('act', 'activation', ['pS%d' % i], ['pT%d' % i], out=pT[i][:], in_=pS[i][:, :], func=AF.Exp, scale=scale)
                        return
                    cls = []
                    for j in range(4):
                        dlt = kt - (qb * 4 + j)
                        cls.append('lo' if dlt < -1 else ('hi' if dlt > 1 else dlt))
                    near = [j for j in range(4) if cls[j] not in ('lo', 'hi')]
                    for ni, j in enumerate(near):
                        c = cls[j]
                        em.op('dve', 'scalar_tensor_tensor', ['pS%d' % i, 'BT'], ['sbias%d' % ni, 'pSd%d' % i], out=sbs[ni][:], in0=pS[i][:, j * 128:(j + 1) * 128],
                              scalar=scale, in1=BT[:, h, (c + 1) * 128:(c + 2) * 128], op0=ALU.mult, op1=ALU.add)
                    for ni, j in enumerate(near):
                        em.op('act', 'activation', ['sbias%d' % ni], ['pT%d' % i], out=pT[i][:, j * 128:(j + 1) * 128], in_=sbs[ni][:], func=AF.Exp)
                    j = 0
                    while j < 4:
                        c = cls[j]
                        if c in ('lo', 'hi'):
                            j2 = j
                            while j2 < 4 and cls[j2] == c:
                                j2 += 1
                            bcol = (15 if c == 'lo' else 31) * 2 + h
                            em.op('act', 'activation', ['pS%d' % i, 'pSd%d' % i, 'rbb'], ['pT%d' % i], out=pT[i][:, j * 128:j2 * 128], in_=pS[i][:, j * 128:j2 * 128],
                                  func=AF.Exp, scale=scale, bias=rbb[:, bcol:bcol + 1])
                            j = j2
                        else:
                            j += 1

                def emit_PV(s):
                    qb, m, kt = steps[s]
                    i = s % NB
                    a = (qb * (1 if gqa else 2) + m) % 2
                    first, last = kt == 0, kt == NT - 1
                    okeys = ['pO%d_%d' % (a, b) for b in range(len(pO[a]))]
                    rd = ['pT%d' % i, ('V', kt)]
                    em._deps('pe', rd, okeys if first else [])
                    if gqa:
                        ins = nc.tensor.matmul(pO[a][0][:, :], V[:, kt, :], pT[i][:], start=first, stop=last)
                    else:
                        nc.tensor.matmul(pO[a][0][:, :], V[:, kt, 0:128], pT[i][:], start=first, stop=last)
                        ins = nc.tensor.matmul(pO[a][1][:, :], V[:, kt, 128:256], pT[i][:], start=first, stop=last)
                    em.ecnt['pe'] += 1
                    ins.then_inc(em.esem['pe'], 1)
                    ev = ('s_pe', em.esem['pe'], em.ecnt['pe'], 'pe')
                    em._record(ev, rd, okeys if (first or last) else [])
                    eng = 'dve' if kt % 2 == 0 else 'pool'
                    ak = 'acc%s%d' % (eng, a)
                    ac = accs[eng][a]
                    if kt < 2:
                        em.op(eng, 'tensor_copy', ['pT%d' % i], [ak], out=ac[:], in_=pT[i][:])
                    else:
                        em.op(eng, 'tensor_tensor', ['pT%d' % i, ak], [ak], out=ac[:], in0=ac[:], in1=pT[i][:], op=ALU.add)
                    if not last:
                        return
                    em.mm(pL[a][:, :], [(onesf[:], accs['dve'][a][:]), (onesf[:], accs['pool'][a][:])], ['accdve%d' % a, 'accpool%d' % a, 'onesf'], ['pL%d' % a])
                    okeys = okeys + ['pL%d' % a]
                    em.op('dve', 'reciprocal', ['pL%d' % a], ['rl'], out=rl[:], in_=pL[a][:, :])
                    if gqa:
                        em.op('dve', 'tensor_tensor', [okeys[0], 'rl'], [('QT', qb)], out=QT[:, :, qb * 128:(qb + 1) * 128],
                              in0=pO[a][0][:, :].rearrange("p (h q) -> p h q", h=4), in1=rl[:].rearrange("p (h q) -> p h q", h=4), op=ALU.mult)
                        return
                    om, ok = (o1, 'o1') if m == 0 else (o2, 'o2')
                    for b in range(2):
                        em.op('dve', 'tensor_tensor', [okeys[b], 'rl'], [ok], out=om[:, b, :], in0=pO[a][b][:, :], in1=rl[:], op=ALU.mult)
                    if m == 0:
                        return
                    em.op('dve', 'scalar_tensor_tensor', ['o1', 'o2', 'ls'], ['o1'], out=o1[:], in0=o2[:], scalar=neglam, in1=o1[:], op0=ALU.mult, op1=ALU.add)
                    em.op('act', 'activation', ['o1'], ['sq2'], out=sq2[:], in_=o1[:], func=AF.Square)
                    em.mm(pL[a][:, :], [(onesb[:], sq2[:, 0, :]), (onesb[:], sq2[:, 1, :])], ['sq2', 'onesb'], ['pL%d' % a])
                    em.op('act', 'activation', ['pL%d' % a, 'epsb'], ['rl'], out=rl[:], in_=pL[a][:, :], func=AF.Sqrt, bias=epsb[:, 0:1], scale=1.0 / 256)
                    em.op('dve', 'reciprocal', ['rl'], ['rl'], out=rl[:], in_=rl[:])
                    for hf in range(2):
                        em.op('dve', 'scalar_tensor_tensor', ['o1', 'rl', 'sgc'], ['o2'], out=o2[:, hf, :], in0=o1[:, hf, :], scalar=sgc[:, hf:hf + 1], in1=rl[:],
                              op0=ALU.mult, op1=ALU.mult)
                    em.op('dve', 'tensor_scalar', ['o2'], qkeys(qb), out=QT[:, 0:2, qb * 512:(qb + 1) * 512], in0=o2[:],
                          scalar1=float(1.0 - lambda_init), scalar2=None, op0=ALU.mult)

                emit_S(0)
                for s in range(len(steps)):
                    if s + 1 < len(steps):
                        emit_S(s + 1)
                    emit_exp(s)
                    emit_PV(s)
                if gqa:
                    em.dma('sp', 'oout', oT.rearrange("(h d) s -> d h s", d=128), QT[:], [('QT', qb) for qb in range(NT)], [])
                else:
                    em.dma('sp', 'oout', oT[h * 256:(h + 1) * 256, :].rearrange("(c d) s -> d c s", d=128), QT[:, 0:2, :], [('QT', j) for j in range(NT)], [])
        em.finish()
    return nc


def rope_table(cfg):
    S = cfg.S
    pos = np.arange(S)
    row = (pos // cfg.GRID_W).astype(np.float32)
    col = (pos % cfg.GRID_W).astype(np.float32)
    half = 64
    freq = (10000.0 ** (-np.arange(0, half, 2, dtype=np.float32) / half)).astype(np.float32)
    ar = row[:, None] * freq[None, :]
    ac = col[:, None] * freq[None, :]
    return np.ascontiguousarray(np.concatenate([np.cos(ar), np.cos(ac), np.sin(ar), np.sin(ac)], axis=1).astype(np.float32))


def bucket_map():
    nb, max_exact, max_distance = 16, 8, 128
    k = np.arange(128)[:, None]
    q = np.arange(128)[None, :]
    out = np.zeros((128, 384), np.float32)
    for d in (-1, 0, 1):
        rel = k - q + 128 * d
        n = np.abs(rel)
        large = max_exact + (np.log(np.maximum(n, 1).astype(np.float32) / max_exact) / math.log(max_distance / max_exact) * (nb - max_exact)).astype(np.int32)
        large = np.minimum(large, nb - 1)
        b = np.where(rel > 0, nb, 0) + np.where(n < max_exact, n, large)
        out[:, (d + 1) * 128:(d + 2) * 128] = b
    return out


def att_inputs(cfg, kind, x, gn, w_in, extra):
    NC = cfg.NC
    maps = []
    x = np.ascontiguousarray(x, dtype=np.float32)
    for c in range(NC):
        m = {"x": x, "gn": np.ascontiguousarray(gn, dtype=np.float32)}
        if kind == 'gqa':
            qd = cfg.QH * 128
            kd = cfg.KVH * 128
            cols = np.concatenate([np.arange(512 * c, 512 * (c + 1)), qd + np.arange(128 * c, 128 * (c + 1)),
                                   qd + kd + np.arange(128 * c, 128 * (c + 1))])
            m["w"] = np.ascontiguousarray(w_in[:, cols][None])
            m["qkg"] = np.ascontiguousarray(np.concatenate([np.tile(extra['qg'], 4), extra['kg']]).astype(np.float32))
            m["rope"] = rope_table(cfg)
        else:
            qk = cfg.DH * 256
            ws = []
            for hh in (2 * c, 2 * c + 1):
                cols = np.concatenate([hh * 256 + np.arange(256), qk + hh * 256 + np.arange(256), 2 * qk + hh * 256 + np.arange(256)])
                ws.append(w_in[:, cols])
            m["w"] = np.ascontiguousarray(np.stack(ws))
            m["lam"] = np.ascontiguousarray(np.concatenate([extra['lq1'], extra['lk1'], extra['lq2'], extra['lk2']]).astype(np.float32))
            m["subg"] = np.ascontiguousarray(extra['subg'], dtype=np.float32)
            m["rb"] = np.ascontiguousarray(extra['rel_bias'][:, 2 * c:2 * c + 2].reshape(-1), dtype=np.float32)
            m["bmap"] = bucket_map()
        maps.append(m)
    return maps


def build_pout(cfg):
    S, D, KC, NT = cfg.S, cfg.D, cfg.KC, cfg.NT
    nc = bass.Bass("TRN2", target_bir_lowering=False)
    oTf = nc.dram_tensor("oTf", [D, S], BF16, kind="ExternalInput").ap()
    wo = nc.dram_tensor("wo", [D, 512], F32, kind="ExternalInput").ap()
    xc = nc.dram_tensor("xc", [S, 512], F32, kind="ExternalInput").ap()
    xo = nc.dram_tensor("xo", [S, 512], F32, kind="ExternalOutput").ap()
    with ExitStack() as es:
        em = Em(nc, es)
        W = _sb(es, nc, 'W', [128, KC, 512], BF16)
        for kc in range(KC):
            em.dma('pool', 'wl', W[:, kc, :], wo[kc * 128:(kc + 1) * 128, :], [], ['W'], new_group=(kc == 0))
        ob = [_sb(es, nc, 'ob%d' % i, [128, KC, 512], BF16) for i in range(2)]
        xb = [_sb(es, nc, 'xb%d' % i, [128, 512], F32) for i in range(2)]
        yb = [_sb(es, nc, 'yb%d' % i, [128, 512], F32) for i in range(2)]
        pp = [_ps(es, nc, 'pp%d' % i, [128, 512], F32) for i in range(2)]
        oview = oTf.rearrange("(kc p) s -> p kc s", p=128)
        n = 0
        for tb in range(S // 512):
            bi = tb % 2
            em.dma('sp', 'ob%d' % bi, ob[bi][:], oview[:, :, tb * 512:(tb + 1) * 512], [], ['ob%d' % bi])
            for j in range(4):
                t = tb * 4 + j
                i = n % 2
                n += 1
                em.dma('sp', 'xb%d' % i, xb[i][:], xc[t * 128:(t + 1) * 128, :], [], ['xb%d' % i])
                em.mm(pp[i][:, :], [(ob[bi][:, kc, j * 128:(j + 1) * 128], W[:, kc, :]) for kc in range(KC)], ['ob%d' % bi, 'W'], ['pp%d' % i])
                em.op('dve', 'tensor_tensor', ['pp%d' % i, 'xb%d' % i], ['yb%d' % i], out=yb[i][:], in0=pp[i][:, :], in1=xb[i][:], op=ALU.add)
                em.dma('sp', 'yo%d' % i, xo[t * 128:(t + 1) * 128, :], yb[i][:], ['yb%d' % i], [])
        em.finish()
    return nc


def build_pout2(cfg):
    S, D, KC, E = cfg.S, cfg.D, cfg.KC, cfg.E
    TL = S // cfg.NC
    TT = TL // 128
    nc = bass.Bass("TRN2", target_bir_lowering=False)
    oTs = nc.dram_tensor("oTs", [D, TL], BF16, kind="ExternalInput").ap()
    wo = nc.dram_tensor("wo", [D, D], F32, kind="ExternalInput").ap()
    xs = nc.dram_tensor("xs", [TL, D], F32, kind="ExternalInput").ap()
    gn = nc.dram_tensor("gn", [D], F32, kind="ExternalInput").ap()
    rw = nc.dram_tensor("rw", [D, E], F32, kind="ExternalInput").ap()
    xm = nc.dram_tensor("xm", [TL, D], F32, kind="ExternalOutput").ap()
    hbo = nc.dram_tensor("hb", [TL, D], BF16, kind="ExternalOutput").ap()
    affo = nc.dram_tensor("aff", [TL, E], F32, kind="ExternalOutput").ap()
    with ExitStack() as es:
        em = Em(nc, es)
        idf, idb, onesb, onesf = _consts(em, es, nc)
        epsb = _sb(es, nc, 'epsb', [128, 1], F32)
        em.op('pool', 'memset', [], ['epsb'], epsb[:], EPS)
        with ExitStack() as pes:
            oT = _sb(pes, nc, 'oT', [128, KC, TL], BF16)
            em.dma('sp', 'ol', oT[:], oTs.rearrange("(kc p) s -> p kc s", p=128), [], ['oT'])
            Wc = [_sb(pes, nc, 'Wc%d' % i, [128, KC, 512], BF16) for i in range(2)]
            xb = [_sb(pes, nc, 'xb%d' % i, [128, 512], F32) for i in range(2)]
            yb = [_sb(pes, nc, 'yb%d' % i, [128, 512], F32) for i in range(2)]
            pp = [_ps(pes, nc, 'pp%d' % i, [128, 512], F32) for i in range(2)]

            def loadW(cg):
                wi = cg % 2
                for kc in range(KC):
                    em.dma('pool', 'wl%d' % wi, Wc[wi][:, kc, :], wo[kc * 128:(kc + 1) * 128, cg * 512:(cg + 1) * 512], [], ['Wc%d' % wi], new_group=(kc == 0))
            loadW(0)
            n = 0
            for cg in range(D // 512):
                if cg + 1 < D // 512:
                    loadW(cg + 1)
                wi = cg % 2
                for tt in range(TT):
                    i = n % 2
                    n += 1
                    em.dma('sp', 'xb%d' % i, xb[i][:], xs[tt * 128:(tt + 1) * 128, cg * 512:(cg + 1) * 512], [], ['xb%d' % i])
                    em.mm(pp[i][:, :], [(oT[:, kc, tt * 128:(tt + 1) * 128], Wc[wi][:, kc, :]) for kc in range(KC)], ['oT', 'Wc%d' % wi], ['pp%d' % i])
                    em.op('dve', 'tensor_tensor', ['pp%d' % i, 'xb%d' % i], ['yb%d' % i], out=yb[i][:], in0=pp[i][:, :], in1=xb[i][:], op=ALU.add)
                    em.dma('sp', 'yo%d' % i, xm[tt * 128:(tt + 1) * 128, cg * 512:(cg + 1) * 512], yb[i][:], ['yb%d' % i], [])
        em.barrier()
        with ExitStack() as pes:
            gb = _sb(pes, nc, 'gb', [128, D], F32)
            em.dma('sp', 'c0', gb[:], gn.rearrange("(o n) -> o n", o=1).partition_broadcast(128), [], ['gb'])
            rws = _sb(pes, nc, 'rws', [128, KC, E], F32)
            em.dma('sp', 'c1', rws[:], rw.rearrange("(kc p) e -> p kc e", p=128), [], ['rws'])
            xt = [_sb(pes, nc, 'xt%d' % i, [128, D], F32) for i in range(2)]
            h32 = _sb(pes, nc, 'h32', [128, D], F32)
            hb = [_sb(pes, nc, 'hb%d' % i, [128, D], BF16) for i in range(2)]
            hT32 = _sb(pes, nc, 'hT32', [128, KC, 128], F32)
            ss = _sb(pes, nc, 'ss', [128, 8], F32)
            rs = _sb(pes, nc, 'rs', [128, 8], F32)
            tmp = _sb(pes, nc, 'tmp', [128, 8], F32)
            ex = _sb(pes, nc, 'ex', [128, E], F32)
            af = [_sb(pes, nc, 'af%d' % i, [128, E], F32) for i in range(2)]
            ptf = [_ps(pes, nc, 'ptf%d' % i, [128, 4, 128], F32) for i in range(2)]
            pl = _ps(pes, nc, 'pl', [128, 512], F32)
            em.dma('sp', 'xin0', xt[0][:], xm[0:128, :], [], ['xt0'])
            for t in range(TT):
                i = t % 2
                if t + 1 < TT:
                    em.dma('sp', 'xin%d' % (1 - i), xt[1 - i][:], xm[(t + 1) * 128:(t + 2) * 128, :], [], ['xt%d' % (1 - i)])
                xk = 'xt%d' % i
                em.op('act', 'activation', [xk], ['h32', 'ss'], out=h32[:], in_=xt[i][:], func=AF.Square, accum_out=ss[:, 0:1])
                _rstd(em, nc, ss[:, 0:1], rs[:, 0:1], tmp[:, 0:1], D, 'ss', 'rs', 'tmp', epsb[:, 0:1])
                em.op('dve', 'scalar_tensor_tensor', [xk, 'rs', 'gb'], ['h32'], out=h32[:], in0=xt[i][:], scalar=rs[:, 0:1], in1=gb[:], op0=ALU.mult, op1=ALU.mult)
                em.op('act', 'copy', ['h32'], ['hb%d' % i], out=hb[i][:], in_=h32[:])
                em.dma('sp', 'hout%d' % i, hbo[t * 128:(t + 1) * 128, :], hb[i][:], ['hb%d' % i], [])
                for b in range(KC // 4):
                    pk = 'ptf%d' % (b % 2)
                    p = ptf[b % 2]
                    em.mm_multi([(p[:, j, :], h32[:, (b * 4 + j) * 128:(b * 4 + j + 1) * 128], idf[:], True) for j in range(4)], ['h32', 'idf'], [pk])
                    em.op('act', 'copy', [pk], ['hT32'], out=hT32[:, b * 4:(b + 1) * 4, :], in_=p[:, :, :])
                em.mm(pl[:, 0:E], [(hT32[:, kc, :], rws[:, kc, :]) for kc in range(KC)], ['hT32', 'rws'], ['pl'])
                em.op('dve', 'tensor_reduce', ['pl'], ['ss'], out=ss[:, 1:2], in_=pl[:, 0:E], axis=AX.X, op=ALU.max)
                em.op('dve', 'tensor_scalar', ['ss'], ['ss'], out=ss[:, 2:3], in0=ss[:, 1:2], scalar1=-1.0, scalar2=None, op0=ALU.mult)
                em.op('act', 'activation', ['pl', 'ss'], ['ex', 'ss'], out=ex[:], in_=pl[:, 0:E], func=AF.Exp, bias=ss[:, 2:3], accum_out=ss[:, 3:4])
                em.op('dve', 'reciprocal', ['ss'], ['ss'], out=ss[:, 4:5], in_=ss[:, 3:4])
                em.op('dve', 'tensor_scalar', ['ex', 'ss'], ['af%d' % i], out=af[i][:], in0=ex[:], scalar1=ss[:, 4:5], scalar2=None, op0=ALU.mult)
                em.dma('sp', 'aout%d' % i, affo[t * 128:(t + 1) * 128, :], af[i][:], ['af%d' % i], [])
        em.finish()
    return nc


def build_pfin(cfg):
    S, D = cfg.S, cfg.D
    TL = S // cfg.NC
    nc = bass.Bass("TRN2", target_bir_lowering=False)
    x = nc.dram_tensor("x", [TL, D], F32, kind="ExternalInput").ap()
    g = nc.dram_tensor("g", [D], F32, kind="ExternalInput").ap()
    y = nc.dram_tensor("y", [TL, D], F32, kind="ExternalOutput").ap()
    with ExitStack() as es:
        em = Em(nc, es)
        epsb = _sb(es, nc, 'epsb', [128, 1], F32)
        em.op('pool', 'memset', [], ['epsb'], epsb[:], EPS)
        gb = _sb(es, nc, 'gb', [128, D], F32)
        em.dma('sp', 'c0', gb[:], g.rearrange("(o n) -> o n", o=1).partition_broadcast(128), [], ['gb'])
        xt = [_sb(es, nc, 'xt%d' % i, [128, D], F32) for i in range(2)]
        yt = [_sb(es, nc, 'yt%d' % i, [128, D], F32) for i in range(2)]
        ss = _sb(es, nc, 'ss', [128, 2], F32)
        rs = _sb(es, nc, 'rs', [128, 2], F32)
        tmp = _sb(es, nc, 'tmp', [128, 2], F32)
        for t in range(TL // 128):
            i = t % 2
            em.dma('sp', 'x%d' % i, xt[i][:], x[t * 128:(t + 1) * 128, :], [], ['xt%d' % i])
            em.op('act', 'activation', ['xt%d' % i], ['yt%d' % i, 'ss%d' % i], out=yt[i][:], in_=xt[i][:], func=AF.Square, accum_out=ss[:, i:i + 1])
            _rstd(em, nc, ss[:, i:i + 1], rs[:, i:i + 1], tmp[:, i:i + 1], D, 'ss%d' % i, 'rs%d' % i, 'tmp%d' % i, epsb[:, 0:1])
            em.op('dve', 'scalar_tensor_tensor', ['xt%d' % i, 'rs%d' % i, 'gb'], ['yt%d' % i], out=yt[i][:], in0=xt[i][:], scalar=rs[:, i:i + 1], in1=gb[:],
                  op0=ALU.mult, op1=ALU.mult)
            em.dma('sp', 'y%d' % i, y[t * 128:(t + 1) * 128, :], yt[i][:], ['yt%d' % i], [])
        em.finish()
    return nc


def build_moe(cfg, NIT=30, ext=True):
    S, D, KC, NT, E, FF, FC, CAP, CT = cfg.S, cfg.D, cfg.KC, cfg.NT, cfg.E, cfg.FF, cfg.FC, cfg.CAP, cfg.CT
    J = S // 128
    nc = bass.Bass("TRN2", target_bir_lowering=False)
    if ext:
        hs = nc.dram_tensor("hs", [S, D], BF16, kind="ExternalInput").ap()
        Ain = nc.dram_tensor("A", [128, 2, J], F32, kind="ExternalInput").ap()
    else:
        x = nc.dram_tensor("x", [S, D], F32, kind="ExternalInput").ap()
        gn = nc.dram_tensor("gn", [D], F32, kind="ExternalInput").ap()
        rw = nc.dram_tensor("rw", [D, E], F32, kind="ExternalInput").ap()
    wg = nc.dram_tensor("wg", [2, D, FF], F32, kind="ExternalInput").ap()
    wu = nc.dram_tensor("wu", [2, D, FF], F32, kind="ExternalInput").ap()
    wd = nc.dram_tensor("wd", [2, FF, D], F32, kind="ExternalInput").ap()
    y = nc.dram_tensor("y", [2, CAP, D], F32, kind="ExternalOutput").ap()
    idxo = nc.dram_tensor("idx", [2, 128, CT], I32, kind="ExternalOutput").ap()
    if not ext:
        hs = nc.dram_tensor("hs_scr", [S, D], BF16).ap()
        affd = nc.dram_tensor("aff_scr", [2, S], F32).ap()
    with ExitStack() as es:
        em = Em(nc, es)
        idf, idb, onesb, onesf = _consts(em, es, nc)
        epsb = _sb(es, nc, 'epsb', [128, 1], F32)
        em.op('pool', 'memset', [], ['epsb'], epsb[:], EPS)
        A = _sb(es, nc, 'A', [128, 2, J], F32)
        idxi = _sb(es, nc, 'idxi', [128, 2, CT], I32)
        gate = _sb(es, nc, 'gate', [128, 2, CT], F32)
        if ext:
            em.dma('sp', 'affi', A[:], Ain[:, :, :], [], ['A'])
        if not ext:
            with ExitStack() as pes:
                gb = _sb(pes, nc, 'gb', [128, D], F32)
                em.dma('sp', 'c0', gb[:], gn.rearrange("(o n) -> o n", o=1).partition_broadcast(128), [], ['gb'])
                rws = _sb(pes, nc, 'rws', [128, KC, E], F32)
                em.dma('sp', 'c1', rws[:], rw.rearrange("(kc p) e -> p kc e", p=128), [], ['rws'])
                xt = _sb(pes, nc, 'xt', [128, D], F32)
                h32 = _sb(pes, nc, 'h32', [128, D], F32)
                hb = _sb(pes, nc, 'hb', [128, D], BF16)
                hT32 = _sb(pes, nc, 'hT32', [128, KC, 128], F32)
                ss = _sb(pes, nc, 'ss', [128, 8], F32)
                rs = _sb(pes, nc, 'rs', [128, 8], F32)
                tmp = _sb(pes, nc, 'tmp', [128, 8], F32)
                ex = _sb(pes, nc, 'ex', [128, E], F32)
                aff2 = _sb(pes, nc, 'aff2', [128, 2], F32)
                affT = _sb(pes, nc, 'affT', [2, S], F32)
                ptf = [_ps(pes, nc, 'ptf%d' % i, [128, 4, 128], F32) for i in range(2)]
                pl = _ps(pes, nc, 'pl', [128, 512], F32)
                pa = _ps(pes, nc, 'pa', [128, 512], F32)
                for t in range(NT):
                    em.dma('sp', 'xin', xt[:], x[t * 128:(t + 1) * 128, :], [], ['xt'])
                    em.op('act', 'activation', ['xt'], ['h32', 'ss'], out=h32[:], in_=xt[:], func=AF.Square, accum_out=ss[:, 0:1])
                    _rstd(em, nc, ss[:, 0:1], rs[:, 0:1], tmp[:, 0:1], D, 'ss', 'rs', 'tmp', epsb[:, 0:1])
                    em.op('dve', 'scalar_tensor_tensor', ['xt', 'rs', 'gb'], ['h32'], out=h32[:], in0=xt[:], scalar=rs[:, 0:1], in1=gb[:], op0=ALU.mult, op1=ALU.mult)
                    em.op('act', 'copy', ['h32'], ['hb'], out=hb[:], in_=h32[:])
                    em.dma('sp', 'hout', hs[t * 128:(t + 1) * 128, :], hb[:], ['hb'], ['hs'])
                    for b in range(KC // 4):
                        pk = 'ptf%d' % (b % 2)
                        p = ptf[b % 2]
                        em.mm_multi([(p[:, j, :], h32[:, (b * 4 + j) * 128:(b * 4 + j + 1) * 128], idf[:], True) for j in range(4)], ['h32', 'idf'], [pk])
                        em.op('act', 'copy', [pk], ['hT32'], out=hT32[:, b * 4:(b + 1) * 4, :], in_=p[:, :, :])
                    em.mm(pl[:, 0:E], [(hT32[:, kc, :], rws[:, kc, :]) for kc in range(KC)], ['hT32', 'rws'], ['pl'])
                    em.op('dve', 'tensor_reduce', ['pl'], ['ss'], out=ss[:, 1:2], in_=pl[:, 0:E], axis=AX.X, op=ALU.max)
                    em.op('dve', 'tensor_scalar', ['ss'], ['ss'], out=ss[:, 2:3], in0=ss[:, 1:2], scalar1=-1.0, scalar2=None, op0=ALU.mult)
                    em.op('act', 'activation', ['pl', 'ss'], ['ex', 'ss'], out=ex[:], in_=pl[:, 0:E], func=AF.Exp, bias=ss[:, 2:3], accum_out=ss[:, 3:4])
                    em.op('dve', 'reciprocal', ['ss'], ['ss'], out=ss[:, 4:5], in_=ss[:, 3:4])
                    em.op('dve', 'tensor_scalar', ['ex', 'ss'], ['aff2'], out=aff2[:], in0=ex[:, 0:2], scalar1=ss[:, 4:5], scalar2=None, op0=ALU.mult)
                    em.mm_multi([(pa[0:2, 0:128], aff2[:, 0:2], idf[:], True)], ['aff2', 'idf'], ['pa'])
                    em.op('act', 'copy', ['pa'], ['affT'], out=affT[:, t * 128:(t + 1) * 128], in_=pa[0:2, 0:128])
                em.dma('sp', 'affo', affd[:, :], affT[:], ['affT'], ['affd'])
                em.dma('sp', 'affi', A[:], affd.rearrange("e (p j) -> p e j", j=J), ['affd'], ['A'])
        em.barrier()
        with ExitStack() as pes:
            lo = _sb(pes, nc, 'lo', [128, 2], F32)
            hi = _sb(pes, nc, 'hi', [128, 2], F32)
            mid = _sb(pes, nc, 'mid', [128, 2], F32)
            cnt = _sb(pes, nc, 'cnt', [128, 2], F32)
            gem = _sb(pes, nc, 'gem', [128, 2], U32)
            ltm = _sb(pes, nc, 'ltm', [128, 2], U32)
            cmpb = _sb(pes, nc, 'cmpb', [128, 2, J], F32)
            cs = _sb(pes, nc, 'cs', [128, 2, J], F32)
            pos = _sb(pes, nc, 'pos', [128, 2, J], F32)
            onesJ = _sb(pes, nc, 'onesJ', [128, J], F32)
            off = _sb(pes, nc, 'off', [128, 2], F32)
            totp = _sb(pes, nc, 'totp', [128, 2], F32)
            U = _sb(pes, nc, 'U', [128, 128], F32)
            iot = _sb(pes, nc, 'iot', [128, CAP], F32)
            rhsv = _sb(pes, nc, 'rhsv', [128, 2, J, 2], F32)
            OH = [_sb(pes, nc, 'OH%d' % i, [128, CAP], F32) for i in range(2)]
            res4 = _sb(pes, nc, 'res4', [128, CT, 2], F32)
            pc = _ps(pes, nc, 'pc', [128, 512], F32)
            pacc = _ps(pes, nc, 'pacc', [128, CT, 512], F32) if CT <= 7 else None
            if pacc is None:
                pacc = _ps(pes, nc, 'pacc', [128, 7, 512], F32)
            em.op('pool', 'memset', [], ['lo'], lo[:], 0.0)
            em.op('pool', 'memset', [], ['hi'], hi[:], 1.0)
            em.op('pool', 'memset', [], ['onesJ'], onesJ[:], 1.0)
            em.op('pool', 'memset', [], ['U'], U[:], 1.0)
            em.op('pool', 'affine_select', ['U'], ['U'], out=U[:], in_=U[:], pattern=[[1, 128]], compare_op=ALU.is_gt, fill=0.0, base=0, channel_multiplier=-1)
            em.op('pool', 'iota', [], ['iot'], iot[:], pattern=[[1, CAP]], base=0, channel_multiplier=0, allow_small_or_imprecise_dtypes=True)
            for e in range(2):
                em.op('pool', 'iota', [], ['rhsv'], rhsv[:, e, :, 0], pattern=[[1, J]], base=0, channel_multiplier=J, allow_small_or_imprecise_dtypes=True)
                em.op('dve', 'tensor_copy', ['A', 'rhsv'], ['rhsv'], out=rhsv[:, e, :, 1], in_=A[:, e, :])
            for it in range(NIT):
                em.op('dve', 'tensor_tensor', ['lo', 'hi'], ['mid'], out=mid[:], in0=lo[:], in1=hi[:], op=ALU.add)
                em.op('dve', 'tensor_scalar', ['mid'], ['mid'], out=mid[:], in0=mid[:], scalar1=0.5, scalar2=None, op0=ALU.mult)
                for e in range(2):
                    em.op('dve', 'tensor_scalar', ['A', 'mid'], ['cmpb', 'cnt'], out=cmpb[:, e, :], in0=A[:, e, :], scalar1=mid[:, e:e + 1], scalar2=0.0,
                          op0=ALU.is_gt, op1=ALU.add, accum_out=cnt[:, e:e + 1])
                em.mm(pc[:, 0:2], [(onesf[:], cnt[:])], ['cnt', 'onesf'], ['pc'])
                em.op('dve', 'tensor_scalar', ['pc'], ['gem'], out=gem[:], in0=pc[:, 0:2], scalar1=float(CAP), scalar2=None, op0=ALU.is_ge)
                em.op('dve', 'tensor_scalar', ['pc'], ['ltm'], out=ltm[:], in0=pc[:, 0:2], scalar1=float(CAP), scalar2=None, op0=ALU.is_lt)
                em.op('dve', 'copy_predicated', ['gem', 'mid'], ['lo'], out=lo[:], mask=gem[:], data=mid[:])
                em.op('dve', 'copy_predicated', ['ltm', 'mid'], ['hi'], out=hi[:], mask=ltm[:], data=mid[:])
            for e in range(2):
                em.op('dve', 'tensor_scalar', ['A', 'lo'], ['cmpb'], out=cmpb[:, e, :], in0=A[:, e, :], scalar1=lo[:, e:e + 1], scalar2=None, op0=ALU.is_gt)
                em.op('dve', 'tensor_tensor_scan', ['cmpb', 'onesJ'], ['cs'], out=cs[:, e, :], data0=onesJ[:], data1=cmpb[:, e, :], initial=0.0, op0=ALU.mult, op1=ALU.add)
                em.op('dve', 'tensor_copy', ['cs'], ['totp'], out=totp[:, e:e + 1], in_=cs[:, e, J - 1:J])
            em.mm(pc[:, 0:2], [(U[:], totp[:])], ['totp', 'U'], ['pc'])
            em.op('act', 'copy', ['pc'], ['off'], out=off[:], in_=pc[:, 0:2])
            for e in range(2):
                em.op('dve', 'tensor_scalar', ['cs', 'off'], ['pos'], out=pos[:, e, :], in0=cs[:, e, :], scalar1=off[:, e:e + 1], scalar2=None, op0=ALU.add)
                em.op('dve', 'tensor_tensor', ['pos', 'cmpb'], ['pos'], out=pos[:, e, :], in0=pos[:, e, :], in1=cmpb[:, e, :], op=ALU.mult)
                em.op('dve', 'tensor_scalar', ['pos'], ['pos'], out=pos[:, e, :], in0=pos[:, e, :], scalar1=-1.0, scalar2=None, op0=ALU.add)
            for e in range(2):
                for c0 in range(0, CT, 7):
                    cts = list(range(c0, min(CT, c0 + 7)))
                    for j in range(J):
                        i = j % 2
                        em.op('dve', 'tensor_scalar', ['iot', 'pos'], ['OH%d' % i], out=OH[i][:], in0=iot[:], scalar1=pos[:, e, j:j + 1], scalar2=None, op0=ALU.is_equal)
                        first, last = j == 0, j == J - 1
                        em._deps('pe', ['OH%d' % i, 'rhsv'], ['pacc'] if first else [])
                        ins = None
                        for ct in cts:
                            ins = nc.tensor.matmul(pacc[:, ct - c0, 0:2], OH[i][:, ct * 128:(ct + 1) * 128], rhsv[:, e, j, :], start=first, stop=last)
                        em.ecnt['pe'] += 1
                        ins.then_inc(em.esem['pe'], 1)
                        ev = ('s_pe', em.esem['pe'], em.ecnt['pe'], 'pe')
                        em._record(ev, ['OH%d' % i, 'rhsv'], ['pacc'] if (first or last) else [])
                    em.op('act', 'copy', ['pacc'], ['res4'], out=res4[:, c0:c0 + len(cts), :], in_=pacc[:, 0:len(cts), 0:2])
                em.op('dve', 'tensor_copy', ['res4'], ['idxi'], out=idxi[:, e, :], in_=res4[:, :, 0])
                em.op('dve', 'tensor_copy', ['res4'], ['gate'], out=gate[:, e, :], in_=res4[:, :, 1])
            em.dma('sp', 'idxo', idxo.rearrange("e p c -> p e c"), idxi[:], ['idxi'], [])
        em.barrier()
        xinT = _sb(es, nc, 'xinT', [128, KC, CAP], BF16)
        actT = _sb(es, nc, 'actT', [128, FC, CAP], BF16)
        for e in range(2):
            with ExitStack() as pes:
                xg = [_sb(pes, nc, 'xg%d' % i, [128, D], BF16) for i in range(2)]
                Wgh = _sb(pes, nc, 'Wgh', [128, KC, 512], BF16)
                Wuh = _sb(pes, nc, 'Wuh', [128, KC, 512], BF16)
                sg = _sb(pes, nc, 'sg', [128, 512], F32)
                ptr = [_ps(pes, nc, 'ptr%d' % i, [128, 8, 128], BF16) for i in range(2)]
                pg = _ps(pes, nc, 'pg', [128, 512], F32)
                pu = _ps(pes, nc, 'pu', [128, 512], F32)
                for ct in range(CT):
                    i = ct % 2
                    em.dma('pool', 'xg%d' % i, xg[i][:], hs[:, :], ['hs', 'idxi'], ['xg%d' % i],
                           indirect=dict(out_offset=None, in_offset=bass.IndirectOffsetOnAxis(ap=idxi[:, e, ct:ct + 1], axis=0)))
                    nb = max(1, KC // 8)
                    per = min(8, KC)
                    for b in range(nb):
                        pk = 'ptr%d' % (b % 2)
                        em.mm_multi([(ptr[b % 2][:, j, :], xg[i][:, (b * per + j) * 128:(b * per + j + 1) * 128], idb[:], True) for j in range(per)], ['xg%d' % i, 'idb'], [pk])
                        em.op('act', 'copy', [pk], ['xinT'], out=xinT[:, b * per:(b + 1) * per, ct * 128:(ct + 1) * 128], in_=ptr[b % 2][:, 0:per, :])
                for (f0, nfc) in [(0, 4), (4, FC - 4)]:
                    for kc in range(KC):
                        em.dma('pool', 'wgl', Wgh[:, kc, 0:nfc * 128], wg[e, kc * 128:(kc + 1) * 128, f0 * 128:(f0 + nfc) * 128], [], ['Wgh'], new_group=(kc == 0))
                        em.dma('pool', 'wul', Wuh[:, kc, 0:nfc * 128], wu[e, kc * 128:(kc + 1) * 128, f0 * 128:(f0 + nfc) * 128], [], ['Wuh'], new_group=(kc == 0))
                    for fcl in range(nfc):
                        for th in range(CAP // 512):
                            em.mm(pg[:, :], [(Wgh[:, kc, fcl * 128:(fcl + 1) * 128], xinT[:, kc, th * 512:(th + 1) * 512]) for kc in range(KC)], ['Wgh', 'xinT'], ['pg'])
                            em.mm(pu[:, :], [(Wuh[:, kc, fcl * 128:(fcl + 1) * 128], xinT[:, kc, th * 512:(th + 1) * 512]) for kc in range(KC)], ['Wuh', 'xinT'], ['pu'])
                            em.op('act', 'activation', ['pg'], ['sg'], out=sg[:], in_=pg[:, :], func=AF.Silu)
                            em.op('dve', 'tensor_tensor', ['pu', 'sg'], ['actT'], out=actT[:, f0 + fcl, th * 512:(th + 1) * 512], in0=pu[:, :], in1=sg[:], op=ALU.mult)
            em.barrier()
            with ExitStack() as pes:
                Wd = _sb(pes, nc, 'Wd', [128, FC, D], BF16)
                for fc in range(FC):
                    em.dma('pool', 'wdl', Wd[:, fc, :], wd[e, fc * 128:(fc + 1) * 128, :], [], ['Wd'], new_group=(fc == 0))
                ysb = [_sb(pes, nc, 'ysb%d' % i, [128, D], F32) for i in range(2)]
                py = [_ps(pes, nc, 'py%d' % i, [128, 512], F32) for i in range(2)]
                n = 0
                for tt in range(CT):
                    b = tt % 2
                    for cg in range(D // 512):
                        i = n % 2
                        n += 1
                        em.mm(py[i][:, :], [(actT[:, fc, tt * 128:(tt + 1) * 128], Wd[:, fc, cg * 512:(cg + 1) * 512]) for fc in range(FC)], ['actT', 'Wd'], ['py%d' % i])
                        em.op('dve', 'tensor_scalar', ['py%d' % i, 'gate'], ['ysb%d' % b], out=ysb[b][:, cg * 512:(cg + 1) * 512], in0=py[i][:, :], scalar1=gate[:, e, tt:tt + 1],
                              scalar2=None, op0=ALU.mult)
                    em.dma('sp', 'yo%d' % b, y[e, tt * 128:(tt + 1) * 128, :], ysb[b][:], ['ysb%d' % b], [])
            em.barrier()
        em.finish()
    return nc


def build_pcomb(cfg):
    S, E, CAP, CT = cfg.S, cfg.E, cfg.CAP, cfg.CT
    nc = bass.Bass("TRN2", target_bir_lowering=False)
    xm = nc.dram_tensor("xm", [S, 512], F32, kind="ExternalInput").ap()
    ys = nc.dram_tensor("ys", [E, CAP, 512], F32, kind="ExternalInput").ap()
    idx = nc.dram_tensor("idx", [E, 128, CT], I32, kind="ExternalInput").ap()
    xo = nc.dram_tensor("xo", [S, 512], F32, kind="ExternalOutput").ap()
    with ExitStack() as es:
        em = Em(nc, es)
        ii = _sb(es, nc, 'ii', [128, E, CT], I32)
        em.dma('sp', 'c0', ii[:], idx.rearrange("e p c -> p e c"), [], ['ii'])
        em.dma('sp', 'cp', xo[:, :], xm[:, :], [], ['xo'])
        yb = [_sb(es, nc, 'yb%d' % i, [128, 512], F32) for i in range(4)]
        n = 0
        for e in range(E):
            for ct in range(CT):
                i = n % 4
                n += 1
                em.dma('sp', 'yl%d' % i, yb[i][:], ys[e, ct * 128:(ct + 1) * 128, :], [], ['yb%d' % i])
                em.dma('pool', 'sc', xo[:, :], yb[i][:], ['yb%d' % i, 'ii', 'xo'], ['xo'] if ct == CT - 1 else [], new_group=(ct == 0),
                       indirect=dict(out_offset=bass.IndirectOffsetOnAxis(ap=ii[:, e, ct:ct + 1], axis=0), in_offset=None, compute_op=ALU.add))
        em.finish()
    return nc


def moe_inputs(cfg, x, gn, rw, wg, wu, wd):
    maps = []
    x = np.ascontiguousarray(x, dtype=np.float32)
    for c in range(cfg.NC):
        mine = [2 * c, 2 * c + 1]
        perm = mine + [e for e in range(cfg.E) if e not in mine]
        maps.append({"x": x, "gn": np.ascontiguousarray(gn), "rw": np.ascontiguousarray(rw[:, perm]),
                     "wg": np.ascontiguousarray(wg[mine]), "wu": np.ascontiguousarray(wu[mine]), "wd": np.ascontiguousarray(wd[mine])})
    return maps


_PROGS = {}


def _prog(name, fn):
    if name not in _PROGS:
        _PROGS[name] = fn()
    return _PROGS[name]


_PROF = []


def _run(nc, maps, n, tag=''):
    import os
    if os.environ.get('KPROF'):
        r = run_bass_kernel_spmd(nc, maps, core_ids=list(range(n)), trace=True)
        _PROF.append((tag, r.exec_time_ns))
        print('KPROF', tag, r.exec_time_ns, flush=True)
        return r.results
    return run_bass_kernel_spmd(nc, maps, core_ids=list(range(n))).results


def forward(cfg, inp):
    NC, S, D = cfg.NC, cfg.S, cfg.D
    x = np.ascontiguousarray(np.asarray(inp['x'], dtype=np.float32).reshape(S, D))
    for i in range(cfg.DEPTH):
        j = i // 2
        if i % 2 == 0:
            nc = _prog('gqa', lambda: build_att(cfg, 'gqa'))
            maps = att_inputs(cfg, 'gqa', x, inp['attn_norm'][i], np.asarray(inp['gqa_w_in'][j]),
                              dict(qg=np.asarray(inp['gqa_q_norm'][j]), kg=np.asarray(inp['gqa_k_norm'][j])))
            w_out = np.asarray(inp['gqa_w_out'][j])
        else:
            li = 0.8 - 0.6 * math.exp(-0.3 * i)
            nc = _prog('diff%d' % i, lambda: build_att(cfg, 'diff', lambda_init=li))
            maps = att_inputs(cfg, 'diff', x, inp['attn_norm'][i], np.asarray(inp['diff_w_in'][j]),
                              dict(lq1=np.asarray(inp['diff_lambda_q1'][j]), lk1=np.asarray(inp['diff_lambda_k1'][j]),
                                   lq2=np.asarray(inp['diff_lambda_q2'][j]), lk2=np.asarray(inp['diff_lambda_k2'][j]),
                                   subg=np.asarray(inp['diff_sub_norm'][j]), rel_bias=np.asarray(inp['rel_bias'])))
            w_out = np.asarray(inp['diff_w_out'][j])
        res = _run(nc, maps, NC, 'att%d' % i)
        oTf = np.ascontiguousarray(np.concatenate([np.asarray(r['oT']) for r in res], axis=0))
        TLc = S // NC
        J = S // 128
        nc = _prog('pout2', lambda: build_pout2(cfg))
        gn2 = np.ascontiguousarray(np.asarray(inp['ffn_norm'][i], dtype=np.float32))
        rwf = np.ascontiguousarray(np.asarray(inp['router_w'][i], dtype=np.float32))
        w_out = np.ascontiguousarray(w_out)
        maps = [{"oTs": np.ascontiguousarray(oTf[:, c * TLc:(c + 1) * TLc]), "wo": w_out, "xs": np.ascontiguousarray(x[c * TLc:(c + 1) * TLc]),
                 "gn": gn2, "rw": rwf} for c in range(NC)]
        res = _run(nc, maps, NC, 'pout%d' % i)
        xm = np.ascontiguousarray(np.concatenate([np.asarray(r['xm']) for r in res], axis=0))
        hbf = np.ascontiguousarray(np.concatenate([np.asarray(r['hb']) for r in res], axis=0))
        aff = np.concatenate([np.asarray(r['aff']) for r in res], axis=0)
        nc = _prog('moe', lambda: build_moe(cfg))
        wg_, wu_, wd_ = np.asarray(inp['expert_w_gate'][i]), np.asarray(inp['expert_w_up'][i]), np.asarray(inp['expert_w_down'][i])
        maps = []
        for c in range(NC):
            A_c = np.ascontiguousarray(aff[:, 2 * c:2 * c + 2].T.reshape(2, 128, J).transpose(1, 0, 2))
            maps.append({"hs": hbf, "A": A_c, "wg": np.ascontiguousarray(wg_[2 * c:2 * c + 2]), "wu": np.ascontiguousarray(wu_[2 * c:2 * c + 2]),
                         "wd": np.ascontiguousarray(wd_[2 * c:2 * c + 2])})
        res = _run(nc, maps, NC, 'moe%d' % i)
        ys = np.concatenate([np.asarray(r['y']) for r in res], axis=0)
        idx = np.ascontiguousarray(np.concatenate([np.asarray(r['idx']) for r in res], axis=0))
        nc = _prog('pcomb', lambda: build_pcomb(cfg))
        maps = [{"xm": np.ascontiguousarray(xm[:, c * 512:(c + 1) * 512]), "ys": np.ascontiguousarray(ys[:, :, c * 512:(c + 1) * 512]), "idx": idx} for c in range(NC)]
        res = _run(nc, maps, NC, 'pcomb%d' % i)
        x = np.ascontiguousarray(np.concatenate([np.asarray(r['xo']) for r in res], axis=1))
    nc = _prog('pfin', lambda: build_pfin(cfg))
    TL = S // NC
    maps = [{"x": np.ascontiguousarray(x[c * TL:(c + 1) * TL]), "g": np.ascontiguousarray(np.asarray(inp['final_norm'], dtype=np.float32))} for c in range(NC)]
    res = _run(nc, maps, NC)
    out = np.concatenate([np.asarray(r['y']) for r in res], axis=0)
    return out.reshape(1, S, D).astype(np.float32)


def kernel(**inputs):
    cfg = Cfg()
    return forward(cfg, inputs)
```

```python
import math
from contextlib import ExitStack
import numpy as np
import concourse.bass as bass
import concourse.mybir as mybir
from concourse.bass_utils import run_bass_kernel_spmd

F32 = mybir.dt.float32
BF16 = mybir.dt.bfloat16
I32 = mybir.dt.int32
U32 = mybir.dt.uint32
AF = mybir.ActivationFunctionType
ALU = mybir.AluOpType
AX = mybir.AxisListType

EPS = 1e-6


class Cfg:
    def __init__(self, NC=8, S=8192, D=4096, E=16, FF=896, GRID_W=64, DEPTH=4):
        self.NC, self.S, self.D, self.E, self.FF, self.GRID_W, self.DEPTH = NC, S, D, E, FF, GRID_W, DEPTH
        self.KC = D // 128
        self.NT = S // 128
        self.CAP = 2 * S // E
        self.CT = self.CAP // 128
        self.FC = FF // 128
        self.QH = 4 * NC
        self.KVH = NC
        self.DH = 2 * NC
        self.CW = D // NC
        assert self.CW == 512 and E == 2 * NC


class Em:
    def __init__(self, nc, es):
        self.nc, self.es = nc, es
        self.E = {'pe': nc.tensor, 'act': nc.scalar, 'dve': nc.vector, 'pool': nc.gpsimd, 'sp': nc.sync}
        self.esem = {e: es.enter_context(nc.semaphore('s_' + e)) for e in ['pe', 'act', 'dve', 'pool']}
        self.ecnt = {e: 0 for e in self.esem}
        self.waited = {e: {} for e in self.E}
        self.dsem = {}
        self.res = {}
        self.limit = None
        self.ncalls = 0

    def _wait(self, eng, evs):
        need = {}
        for (name, sem, val, src) in evs:
            if src == 'pe' and eng == 'pe':
                continue
            if val <= 0:
                continue
            if need.get(name, (None, 0))[1] < val:
                need[name] = (sem, val)
        for name, (sem, val) in need.items():
            if self.waited[eng].get(name, 0) >= val:
                continue
            self.E[eng].wait_ge(sem, val)
            self.waited[eng][name] = val

    def _deps(self, eng, reads, writes):
        evs = []
        for r in reads:
            st = self.res.get(r)
            if st and st['w']:
                evs.append(st['w'])
        for w in writes:
            st = self.res.get(w)
            if st:
                if st['w']:
                    evs.append(st['w'])
                evs.extend(st['r'].values())
        self._wait(eng, evs)

    def _record(self, ev, reads, writes):
        for r in reads:
            st = self.res.setdefault(r, {'w': None, 'r': {}})
            old = st['r'].get(ev[0])
            if old is None or old[2] < ev[2]:
                st['r'][ev[0]] = ev
        for w in writes:
            self.res[w] = {'w': ev, 'r': {}}

    def _lim(self):
        self.ncalls += 1
        return self.limit is not None and self.ncalls > self.limit

    def op(self, eng, fn, reads, writes, *args, **kw):
        if self._lim():
            return None
        self._deps(eng, reads, writes)
        ins = getattr(self.E[eng], fn)(*args, **kw)
        self.ecnt[eng] += 1
        ins.then_inc(self.esem[eng], 1)
        ev = ('s_' + eng, self.esem[eng], self.ecnt[eng], eng)
        self._record(ev, reads, writes)
        return ins

    def mm(self, out, pairs, reads, writes, transpose=False):
        if self._lim():
            return
        self._deps('pe', reads, writes)
        n = len(pairs)
        ins = None
        for i, (a, b) in enumerate(pairs):
            if transpose:
                ins = self.nc.tensor.transpose(out=out, in_=a, identity=b)
            else:
                ins = self.nc.tensor.matmul(out, a, b, start=(i == 0), stop=(i == n - 1))
        self.ecnt['pe'] += 1
        ins.then_inc(self.esem['pe'], 1)
        ev = ('s_pe', self.esem['pe'], self.ecnt['pe'], 'pe')
        self._record(ev, reads, writes)

    def mm_multi(self, items, reads, writes):
        if self._lim():
            return
        self._deps('pe', reads, writes)
        ins = None
        for (o, a, b, tr) in items:
            if tr:
                ins = self.nc.tensor.transpose(out=o, in_=a, identity=b)
            else:
                ins = self.nc.tensor.matmul(o, a, b, start=True, stop=True)
        self.ecnt['pe'] += 1
        ins.then_inc(self.esem['pe'], 1)
        ev = ('s_pe', self.esem['pe'], self.ecnt['pe'], 'pe')
        self._record(ev, reads, writes)

    def _dkey(self, key):
        if key not in self.dsem:
            self.dsem[key] = [self.es.enter_context(self.nc.semaphore('d_' + key)), 0]
        return self.dsem[key]

    def dma(self, eng, key, out, in_, reads, writes, new_group=True, indirect=None, **kw):
        if self._lim():
            return
        ent = self._dkey(key)
        if new_group:
            self._wait(eng, [('d_' + key, ent[0], ent[1], 'dma')])
        self._deps(eng, reads, writes)
        if indirect is None:
            ins = self.E[eng].dma_start(out=out, in_=in_, **kw)
        else:
            ins = self.nc.gpsimd.indirect_dma_start(out=out, in_=in_, **indirect, **kw)
        ent[1] += 16
        ins.then_inc(ent[0], 16)
        ev = ('d_' + key, ent[0], ent[1], 'dma')
        self._record(ev, reads, writes)

    def barrier(self):
        evs = [('s_' + e, self.esem[e], self.ecnt[e], 'x') for e in self.esem]
        evs += [('d_' + k, v[0], v[1], 'dma') for k, v in self.dsem.items()]
        for eng in self.E:
            self._wait(eng, evs)
        self.res = {}

    def finish(self):
        evs = [('s_' + e, self.esem[e], self.ecnt[e], 'x') for e in self.esem]
        evs += [('d_' + k, v[0], v[1], 'dma') for k, v in self.dsem.items()]
        self._wait('sp', evs)


_UNIQ = [0]


def _sb(es, nc, name, shape, dt):
    _UNIQ[0] += 1
    return es.enter_context(nc.sbuf_tensor('%s_%d' % (name, _UNIQ[0]), list(shape), dt))


def _ps(es, nc, name, shape, dt):
    _UNIQ[0] += 1
    return es.enter_context(nc.psum_tensor('%s_%d' % (name, _UNIQ[0]), list(shape), dt))


def _consts(em, es, nc):
    idf = _sb(es, nc, 'idf', [128, 128], F32)
    idb = _sb(es, nc, 'idb', [128, 128], BF16)
    onesb = _sb(es, nc, 'onesb', [128, 128], BF16)
    onesf = _sb(es, nc, 'onesf', [128, 128], F32)
    em.op('pool', 'memset', [], ['idf'], idf[:], 0.0)
    em.op('pool', 'affine_select', ['idf'], ['idf'], out=idf[:], in_=idf[:], pattern=[[-1, 128]],
          compare_op=ALU.not_equal, fill=1.0, base=0, channel_multiplier=1)
    em.op('dve', 'tensor_copy', ['idf'], ['idb'], out=idb[:], in_=idf[:])
    em.op('pool', 'memset', [], ['onesb'], onesb[:], 1.0)
    em.op('pool', 'memset', [], ['onesf'], onesf[:], 1.0)
    return idf, idb, onesb, onesf


def _rstd(em, nc, ss, rs, tmp, n, key_ss, key_rs, key_tmp, epsb):
    em.op('act', 'activation', [key_ss, 'epsb'], [key_tmp], out=tmp, in_=ss, func=AF.Sqrt, bias=epsb, scale=1.0 / n)
    em.op('dve', 'reciprocal', [key_tmp], [key_rs], out=rs, in_=tmp)


def _norm_transpose_tile(em, nc, cfg, t, x_ap, bufs):
    KC, D, NT = cfg.KC, cfg.D, cfg.NT
    xts, hbf, hTs, gcol, ss, rs, tmp, epsb, idb, pst = bufs
    nx = len(xts)
    if t == 0:
        em.dma('sp', 'xin0', xts[0][:], x_ap[0:128, :], [], ['xt0'])
    if nx > 1 and t + 1 < NT:
        b = (t + 1) % nx
        em.dma('sp', 'xin%d' % b, xts[b][:], x_ap[(t + 1) * 128:(t + 2) * 128, :], [], ['xt%d' % b])
    elif nx == 1 and t > 0:
        em.dma('sp', 'xin0', xts[0][:], x_ap[t * 128:(t + 1) * 128, :], [], ['xt0'])
    xk = 'xt%d' % (t % nx)
    xt = xts[t % nx]
    par = t % len(hTs)
    hT, hk = hTs[par], 'hT%d' % par
    c = par
    sk, rk, tk = 'ss%d' % c, 'rs%d' % c, 'tmp%d' % c
    em.op('act', 'activation', [xk], ['hbf', sk], out=hbf[:], in_=xt[:], func=AF.Square, accum_out=ss[:, c:c + 1])
    _rstd(em, nc, ss[:, c:c + 1], rs[:, c:c + 1], tmp[:, c:c + 1], D, sk, rk, tk, epsb[:, 0:1])
    em.op('dve', 'tensor_scalar', [xk, rk], ['hbf'], out=hbf[:], in0=xt[:], scalar1=rs[:, c:c + 1], scalar2=None, op0=ALU.mult)
    nb = KC // 8 if KC >= 8 else 1
    per = min(8, KC)
    for b in range(nb):
        pk = 'pst%d' % (b % 2)
        p = pst[b % 2]
        items = [(p[:, j, :], hbf[:, (b * per + j) * 128:(b * per + j + 1) * 128], idb[:], True) for j in range(per)]
        em.mm_multi(items, ['hbf', 'idb'], [pk])
        g_b = gcol[:, b * per:(b + 1) * per]
        em.op('dve', 'tensor_tensor', [pk, 'gcol'], [hk], out=hT[:, b * per:(b + 1) * per, :], in0=p[:, 0:per, :],
              in1=_bc_last(g_b, 128), op=ALU.mult)
    return hT, hk


def _bc_last(ap2d, n):
    t = ap2d
    return bass.AP(tensor=t.tensor, offset=t.offset, ap=list(t.ap) + [[0, n]])


def _bc_mid(ap2d, m):
    t = ap2d
    a = list(t.ap)
    return bass.AP(tensor=t.tensor, offset=t.offset, ap=[a[0], [0, m]] + a[1:])


def build_att(cfg, kind, lambda_init=0.0, stop=None, acc_mode='dve', rope_pool=False):
    S, D, KC, NT = cfg.S, cfg.D, cfg.KC, cfg.NT
    gqa = kind == 'gqa'
    NP = 1 if gqa else 2
    nc = bass.Bass("TRN2", target_bir_lowering=False)
    x = nc.dram_tensor("x", [S, D], F32, kind="ExternalInput").ap()
    gn = nc.dram_tensor("gn", [D], F32, kind="ExternalInput").ap()
    w = nc.dram_tensor("w", [NP, D, 768], F32, kind="ExternalInput").ap()
    if gqa:
        qkg = nc.dram_tensor("qkg", [5 * 128], F32, kind="ExternalInput").ap()
        rope = nc.dram_tensor("rope", [S, 128], F32, kind="ExternalInput").ap()
    else:
        lamv = nc.dram_tensor("lam", [4 * 128], F32, kind="ExternalInput").ap()
        subg = nc.dram_tensor("subg", [256], F32, kind="ExternalInput").ap()
        rb = nc.dram_tensor("rb", [64], F32, kind="ExternalInput").ap()
        bmap = nc.dram_tensor("bmap", [128, 384], F32, kind="ExternalInput").ap()
    oT = nc.dram_tensor("oT", [512, S], BF16, kind="ExternalOutput").ap()
    scale = 128 ** -0.5

    with ExitStack() as es:
        em = Em(nc, es)
        idf, idb, onesb, onesf = _consts(em, es, nc)
        epsb = _sb(es, nc, 'epsb', [128, 1], F32)
        em.op('pool', 'memset', [], ['epsb'], epsb[:], EPS)
        gcol = _sb(es, nc, 'gcol', [128, KC], F32)
        em.dma('sp', 'c0', gcol[:], gn.rearrange("(kc p) -> p kc", p=128), [], ['gcol'], allow_slow_non_contiguous=True)
        QT = _sb(es, nc, 'QT', [128, 4, S], BF16)
        if gqa:
            KT = _sb(es, nc, 'KT', [128, S], BF16)
            V = _sb(es, nc, 'V', [128, NT, 128], BF16)
        else:
            V = _sb(es, nc, 'V', [128, NT, 256], BF16)
        if gqa:
            gq = _sb(es, nc, 'gq', [128, 5, 128], F32)
            em.dma('sp', 'c1', gq[:], qkg.rearrange("(h d) -> h d", d=128).partition_broadcast(128), [], ['gq'])
        else:
            lam4 = _sb(es, nc, 'lam4', [128, 4, 128], F32)
            em.dma('sp', 'c1', lam4[:], lamv.rearrange("(h d) -> h d", d=128).partition_broadcast(128), [], ['lam4'])
            sgc = _sb(es, nc, 'sgc', [128, 2], F32)
            em.dma('sp', 'c2', sgc[:], subg.rearrange("(h p) -> p h", p=128), [], ['sgc'], allow_slow_non_contiguous=True)
            rbb = _sb(es, nc, 'rbb', [128, 64], F32)
            em.dma('sp', 'c3', rbb[:], rb.rearrange("(o n) -> o n", o=1).partition_broadcast(128), [], ['rbb'])
            bm = _sb(es, nc, 'bm', [128, 384], F32)
            em.dma('sp', 'c4', bm[:], bmap[:, :], [], ['bm'])
            lp = _sb(es, nc, 'lp', [128, 2, 128], F32)
            ls = _sb(es, nc, 'ls', [128, 4], F32)
            em.op('dve', 'tensor_tensor', ['lam4'], ['lp'], out=lp[:], in0=lam4[:, 0:4:2, :], in1=lam4[:, 1:4:2, :], op=ALU.mult)
            em.op('dve', 'tensor_reduce', ['lp'], ['ls'], out=ls[:, 0:2], in_=lp[:], axis=AX.X, op=ALU.add)
            em.op('act', 'activation', ['ls'], ['ls'], out=ls[:, 0:2], in_=ls[:, 0:2], func=AF.Exp)
            em.op('dve', 'tensor_tensor', ['ls'], ['ls'], out=ls[:, 2:3], in0=ls[:, 1:2], in1=ls[:, 0:1], op=ALU.subtract)
            em.op('dve', 'tensor_scalar', ['ls'], ['ls'], out=ls[:, 3:4], in0=ls[:, 2:3], scalar1=-float(lambda_init), scalar2=None, op0=ALU.add)
            neglam = ls[:, 3:4]
            BT = _sb(es, nc, 'BT', [128, 2, 384], F32)
            btmp = _sb(es, nc, 'btmp', [128, 384], F32)
            em.op('pool', 'memset', [], ['BT'], BT[:], 0.0)
            for h in range(2):
                for b in range(32):
                    em.op('dve', 'tensor_scalar', ['bm', 'rbb'], ['btmp'], out=btmp[:], in0=bm[:], scalar1=float(b),
                          scalar2=rbb[:, b * 2 + h:b * 2 + h + 1], op0=ALU.is_equal, op1=ALU.mult)
                    em.op('dve', 'tensor_tensor', ['btmp', 'BT'], ['BT'], out=BT[:, h, :], in0=BT[:, h, :], in1=btmp[:], op=ALU.add)

        for ps_i in range(NP):
            if stop == 'consts':
                break
            em.barrier()
            with ExitStack() as pes:
                W = _sb(pes, nc, 'W', [128, KC, 768], BF16)
                for kc in range(KC):
                    em.dma('pool', 'wl', W[:, kc, :], w[ps_i, kc * 128:(kc + 1) * 128, :], [], ['W'], new_group=(kc == 0))
                xt = [_sb(pes, nc, 'xt%d' % q_, [128, D], F32) for q_ in range(1)]
                hbf = _sb(pes, nc, 'hbf', [128, D], BF16)
                hTs = [_sb(pes, nc, 'hT%d' % q_, [128, KC, 128], BF16) for q_ in range(2)]
                ss = _sb(pes, nc, 'ss', [128, 8], F32)
                rs = _sb(pes, nc, 'rs', [128, 8], F32)
                tmp = _sb(pes, nc, 'tmp', [128, 8], F32)
                pst = [_ps(pes, nc, 'pst%d' % i, [128, 8, 128], BF16) for i in range(2)]
                ppA = [_ps(pes, nc, 'ppA%d' % i, [128, 512], F32) for i in range(2)]
                ppB = [_ps(pes, nc, 'ppB%d' % i, [128, 512], F32) for i in range(2)]
                ptr = _ps(pes, nc, 'ptr', [128, 8, 128], BF16)
                if gqa:
                    sq = [_sb(pes, nc, 'sq%d' % q_, [128, 5, 128], F32) for q_ in range(2)]
                    qn = [_sb(pes, nc, 'qn%d' % q_, [128, 5, 128], F32) for q_ in range(2)]
                    qr = [_sb(pes, nc, 'qr%d' % q_, [128, 5, 128], BF16) for q_ in range(2)]
                    rt = [_sb(pes, nc, 'rt%d' % q_, [128, 128], F32) for q_ in range(2)]
                    t1 = [_sb(pes, nc, 't1%d' % q_, [128, 5, 2, 32], F32) for q_ in range(2)]
                    t2 = [_sb(pes, nc, 't2%d' % q_, [128, 5, 2, 32], F32) for q_ in range(2)]
                    ssh = _sb(pes, nc, 'ssh', [128, 16], F32)
                    rsh = _sb(pes, nc, 'rsh', [128, 16], F32)
                    tmh = _sb(pes, nc, 'tmh', [128, 16], F32)
                else:
                    qb16 = [_sb(pes, nc, 'qb16%d' % q_, [128, 4, 128], BF16) for q_ in range(2)]
                bufs = (xt, hbf, hTs, gcol, ss, rs, tmp, epsb, idb, pst)
                def front(t):
                    c = t % 2
                    hT, hk = _norm_transpose_tile(em, nc, cfg, t, x, bufs)
                    pA, pB, kA, kB = ppA[c], ppB[c], 'ppA%d' % c, 'ppB%d' % c
                    em.mm(pA[:, :], [(hT[:, kc, :], W[:, kc, 0:512]) for kc in range(KC)], [hk, 'W'], [kA])
                    em.mm(pB[:, 0:256], [(hT[:, kc, :], W[:, kc, 512:768]) for kc in range(KC)], [hk, 'W'], [kB])

                def mid(t):
                    c = t % 2
                    pA, pB, kA, kB = ppA[c], ppB[c], 'ppA%d' % c, 'ppB%d' % c
                    if gqa:
                        sqk, qnk, qrk, rtk, t1k, t2k = 'sq%d' % c, 'qn%d' % c, 'qr%d' % c, 'rt%d' % c, 't1%d' % c, 't2%d' % c
                        sshk, rshk, tmhk = 'ssh%d' % c, 'rsh%d' % c, 'tmh%d' % c
                        o8 = c * 8
                        em.dma('sp', 'rt%d' % c, rt[c][:], rope[t * 128:(t + 1) * 128, :], [], [rtk])
                        em.op('act', 'activation', [kA], [sqk], out=sq[c][:, 0:4, :], in_=pA[:, :].rearrange("p (h d) -> p h d", d=128), func=AF.Square)
                        em.op('act', 'activation', [kB], [sqk], out=sq[c][:, 4, :], in_=pB[:, 0:128], func=AF.Square)
                        em.op('act', 'copy', [kB], [('V', t)], out=V[:, t, :], in_=pB[:, 128:256])
                        em.op('dve', 'tensor_reduce', [sqk], [sshk], out=ssh[:, o8:o8 + 5], in_=sq[c][:], axis=AX.X, op=ALU.add)
                        _rstd(em, nc, ssh[:, o8:o8 + 5], rsh[:, o8:o8 + 5], tmh[:, o8:o8 + 5], 128, sshk, rshk, tmhk, epsb[:, 0:1])
                        em.op('dve', 'tensor_tensor', [kA, rshk], [qnk], out=qn[c][:, 0:4, :], in0=pA[:, :].rearrange("p (h d) -> p h d", d=128),
                              in1=_bc_last(rsh[:, o8:o8 + 4], 128), op=ALU.mult)
                        em.op('dve', 'tensor_tensor', [kB, rshk], [qnk], out=qn[c][:, 4, :], in0=pB[:, 0:128],
                              in1=rsh[:, o8 + 4:o8 + 5].to_broadcast([128, 128]), op=ALU.mult)
                        em.op('dve', 'tensor_tensor', [qnk, 'gq'], [qnk], out=qn[c][:], in0=qn[c][:], in1=gq[:], op=ALU.mult)
                        qv = qn[c][:].rearrange("p h (hh ab i) -> p h hh ab i", hh=2, ab=2)
                        qo = qr[c][:].rearrange("p h (hh ab i) -> p h hh ab i", hh=2, ab=2)
                        A_, B_ = qv[:, :, :, 0, :], qv[:, :, :, 1, :]
                        C_ = _bc_mid(rt[c][:, 0:64], 5).rearrange("p h (hh i) -> p h hh i", hh=2)
                        S_ = _bc_mid(rt[c][:, 64:128], 5).rearrange("p h (hh i) -> p h hh i", hh=2)
                        em.op('dve', 'tensor_tensor', [qnk, rtk], [t1k], out=t1[c][:], in0=A_, in1=C_, op=ALU.mult)
                        em.op('pool' if rope_pool else 'dve', 'tensor_tensor', [qnk, rtk], [t2k], out=t2[c][:], in0=B_, in1=S_, op=ALU.mult)
                        em.op('dve', 'tensor_tensor', [t1k, t2k], [qrk], out=qo[:, :, :, 0, :], in0=t1[c][:], in1=t2[c][:], op=ALU.subtract)
                        em.op('dve', 'tensor_tensor', [qnk, rtk], [t1k], out=t1[c][:], in0=B_, in1=C_, op=ALU.mult)
                        em.op('pool' if rope_pool else 'dve', 'tensor_tensor', [qnk, rtk], [t2k], out=t2[c][:], in0=A_, in1=S_, op=ALU.mult)
                        em.op('dve', 'tensor_tensor', [t1k, t2k], [qrk], out=qo[:, :, :, 1, :], in0=t1[c][:], in1=t2[c][:], op=ALU.add)
                    else:
                        qk_ = 'qb16%d' % c
                        em.op('act', 'copy', [kA], [qk_], out=qb16[c][:], in_=pA[:, :].rearrange("p (h d) -> p h d", d=128))
                        em.op('act', 'copy', [kB], [('V', t)], out=V[:, t, :], in_=pB[:, 0:256])

                def back(t):
                    c = t % 2
                    if gqa:
                        qrk = 'qr%d' % c
                        em.mm_multi([(ptr[:, j, :], qr[c][:, j, :], idb[:], True) for j in range(5)], [qrk, 'idb'], ['ptr'])
                        em.op('act', 'copy', ['ptr'], [('QT', t)], out=QT[:, :, t * 128:(t + 1) * 128], in_=ptr[:, 0:4, :])
                        em.op('act', 'copy', ['ptr'], [('KT', t)], out=KT[:, t * 128:(t + 1) * 128], in_=ptr[:, 4, :])
                    else:
                        qk_ = 'qb16%d' % c
                        em.mm_multi([(ptr[:, j, :], qb16[c][:, j, :], idb[:], True) for j in range(4)], [qk_, 'idb'], ['ptr'])
                        em.op('act', 'copy', ['ptr'], [('QT', t)], out=QT[:, :, t * 128:(t + 1) * 128], in_=ptr[:, 0:4, :])

                front(0)
                for t in range(NT):
                    if t + 1 < NT:
                        front(t + 1)
                    mid(t)
                    back(t)
            if stop in ('tile0', 'proj', 'nt') or isinstance(stop, int):
                break
            em.barrier()
            with ExitStack() as pes:
                NB = 3 if gqa else 2
                pS = [_ps(pes, nc, 'pS%d' % i, [128, 512], F32) for i in range(NB)]
                pT = [_sb(pes, nc, 'pT%d' % i, [128, 512], BF16) for i in range(NB)]
                if gqa:
                    pO = [[_ps(pes, nc, 'pO%d' % a, [128, 512], F32)] for a in range(2)]
                else:
                    pO = [[_ps(pes, nc, 'pO%d_%d' % (a, b), [128, 512], F32) for b in range(2)] for a in range(2)]
                pL = [_ps(pes, nc, 'pL%d' % a, [128, 512], F32) for a in range(2)]
                rl = _sb(pes, nc, 'rl', [128, 512], F32)
                accs = {e_: [_sb(pes, nc, 'acc%s%d' % (e_, a_), [128, 512], F32) for a_ in range(2)] for e_ in ('dve', 'pool')}
                if gqa:
                    steps = [(qb, 0, kt) for qb in range(NT) for kt in range(NT)]
                else:
                    h = ps_i
                    o1 = _sb(pes, nc, 'o1', [128, 2, 512], F32)
                    o2 = _sb(pes, nc, 'o2', [128, 2, 512], F32)
                    sq2 = _sb(pes, nc, 'sq2', [128, 2, 512], BF16)
                    sbs = [_sb(pes, nc, 'sbias%d' % q_, [128, 128], F32) for q_ in range(4)]
                    steps = [(qb, m, kt) for qb in range(S // 512) for m in range(2) for kt in range(NT)]

                def qkeys(qb):
                    return [('QT', qb)] if gqa else [('QT', j) for j in range(qb * 4, qb * 4 + 4)]

                def emit_S(s):
                    qb, m, kt = steps[s]
                    i = s % NB
                    if gqa:
                        em.mm(pS[i][:, :].rearrange("p (h q) -> p h q", h=4), [(KT[:, kt * 128:(kt + 1) * 128], QT[:, :, qb * 128:(qb + 1) * 128])],
                              [('QT', qb), ('KT', kt)], ['pS%d' % i])
                    else:
                        em.mm(pS[i][:, :], [(QT[:, 2 + m, kt * 128:(kt + 1) * 128], QT[:, m, qb * 512:(qb + 1) * 512])],
                              qkeys(qb) + [('QT', kt)], ['pS%d' % i, 'pSd%d' % i])

                def emit_exp(s):
                    qb, m, kt = steps[s]
                    i = s % NB
                    if gqa:
                        em.op('act', 'activation', ['pS%d' % i], ['pT%d' % i], out=pT[i][:], in_=pS[i][:, :], func=AF.Exp, scale=scale)
                        return
                    cls = []
                    for j in range(4):
                        dlt = kt - (qb * 4 + j)
                        cls.append('lo' if dlt < -1 else ('hi' if dlt > 1 else dlt))
                    near = [j for j in range(4) if cls[j] not in ('lo', 'hi')]
                    for ni, j in enumerate(near):
                        c = cls[j]
                        em.op('dve', 'scalar_tensor_tensor', ['pS%d' % i, 'BT'], ['sbias%d' % ni, 'pSd%d' % i], out=sbs[ni][:], in0=pS[i][:, j * 128:(j + 1) * 128],
                              scalar=scale, in1=BT[:, h, (c + 1) * 128:(c + 2) * 128], op0=ALU.mult, op1=ALU.add)
                    for ni, j in enumerate(near):
                        em.op('act', 'activation', ['sbias%d' % ni], ['pT%d' % i], out=pT[i][:, j * 128:(j + 1) * 128], in_=sbs[ni][:], func=AF.Exp)
                    j = 0
                    while j < 4:
                        c = cls[j]
                        if c in ('lo', 'hi'):
                            j2 = j
                            while j2 < 4 and cls[j2] == c:
                                j2 += 1
                            bcol = (15 if c == 'lo' else 31) * 2 + h
                            em.op('act', 'activation', ['pS%d' % i, 'pSd%d' % i, 'rbb'], ['pT%d' % i], out=pT[i][:, j * 128:j2 * 128], in_=pS[i][:, j * 128:j2 * 128],
                                  func=AF.Exp, scale=scale, bias=rbb[:, bcol:bcol + 1])
                            j = j2
                        else:
                            j += 1

                def emit_PV(s):
                    qb, m, kt = steps[s]
                    i = s % NB
                    a = (qb * (1 if gqa else 2) + m) % 2
                    first, last = kt == 0, kt == NT - 1
                    okeys = ['pO%d_%d' % (a, b) for b in range(len(pO[a]))]
                    rd = ['pT%d' % i, ('V', kt)]
                    em._deps('pe', rd, okeys if first else [])
                    if gqa:
                        ins = nc.tensor.matmul(pO[a][0][:, :], V[:, kt, :], pT[i][:], start=first, stop=last)
                    else:
                        nc.tensor.matmul(pO[a][0][:, :], V[:, kt, 0:128], pT[i][:], start=first, stop=last)
                        ins = nc.tensor.matmul(pO[a][1][:, :], V[:, kt, 128:256], pT[i][:], start=first, stop=last)
                    em.ecnt['pe'] += 1
                    ins.then_inc(em.esem['pe'], 1)
                    ev = ('s_pe', em.esem['pe'], em.ecnt['pe'], 'pe')
                    em._record(ev, rd, okeys if (first or last) else [])
                    eng = 'dve' if (kt % 2 == 0 or acc_mode == 'dve') else 'pool'
                    ak = 'acc%s%d' % (eng, a)
                    ac = accs[eng][a]
                    if kt < (1 if acc_mode == 'dve' else 2):
                        em.op(eng, 'tensor_copy', ['pT%d' % i], [ak], out=ac[:], in_=pT[i][:])
                    else:
                        em.op(eng, 'tensor_tensor', ['pT%d' % i, ak], [ak], out=ac[:], in0=ac[:], in1=pT[i][:], op=ALU.add)
                    if not last:
                        return
                    if acc_mode == 'dve':
                        em.mm(pL[a][:, :], [(onesf[:], accs['dve'][a][:])], ['accdve%d' % a, 'onesf'], ['pL%d' % a])
                    else:
                        em.mm(pL[a][:, :], [(onesf[:], accs['dve'][a][:]), (onesf[:], accs['pool'][a][:])], ['accdve%d' % a, 'accpool%d' % a, 'onesf'], ['pL%d' % a])
                    okeys = okeys + ['pL%d' % a]
                    em.op('dve', 'reciprocal', ['pL%d' % a], ['rl'], out=rl[:], in_=pL[a][:, :])
                    if gqa:
                        em.op('dve', 'tensor_tensor', [okeys[0], 'rl'], [('QT', qb)], out=QT[:, :, qb * 128:(qb + 1) * 128],
                              in0=pO[a][0][:, :].rearrange("p (h q) -> p h q", h=4), in1=rl[:].rearrange("p (h q) -> p h q", h=4), op=ALU.mult)
                        return
                    om, ok = (o1, 'o1') if m == 0 else (o2, 'o2')
                    for b in range(2):
                        em.op('dve', 'tensor_tensor', [okeys[b], 'rl'], [ok], out=om[:, b, :], in0=pO[a][b][:, :], in1=rl[:], op=ALU.mult)
                    if m == 0:
                        return
                    em.op('dve', 'scalar_tensor_tensor', ['o1', 'o2', 'ls'], ['o1'], out=o1[:], in0=o2[:], scalar=neglam, in1=o1[:], op0=ALU.mult, op1=ALU.add)
                    em.op('act', 'activation', ['o1'], ['sq2'], out=sq2[:], in_=o1[:], func=AF.Square)
                    em.mm(pL[a][:, :], [(onesb[:], sq2[:, 0, :]), (onesb[:], sq2[:, 1, :])], ['sq2', 'onesb'], ['pL%d' % a])
                    em.op('act', 'activation', ['pL%d' % a, 'epsb'], ['rl'], out=rl[:], in_=pL[a][:, :], func=AF.Sqrt, bias=epsb[:, 0:1], scale=1.0 / 256)
                    em.op('dve', 'reciprocal', ['rl'], ['rl'], out=rl[:], in_=rl[:])
                    for hf in range(2):
                        em.op('dve', 'scalar_tensor_tensor', ['o1', 'rl', 'sgc'], ['o2'], out=o2[:, hf, :], in0=o1[:, hf, :], scalar=sgc[:, hf:hf + 1], in1=rl[:],
                              op0=ALU.mult, op1=ALU.mult)
                    em.op('dve', 'tensor_scalar', ['o2'], qkeys(qb), out=QT[:, 0:2, qb * 512:(qb + 1) * 512], in0=o2[:],
                          scalar1=float(1.0 - lambda_init), scalar2=None, op0=ALU.mult)

                emit_S(0)
                for s in range(len(steps)):
                    if s + 1 < len(steps):
                        emit_S(s + 1)
                    emit_exp(s)
                    emit_PV(s)
                if gqa:
                    em.dma('sp', 'oout', oT.rearrange("(h d) s -> d h s", d=128), QT[:], [('QT', qb) for qb in range(NT)], [])
                else:
                    em.dma('sp', 'oout', oT[h * 256:(h + 1) * 256, :].rearrange("(c d) s -> d c s", d=128), QT[:, 0:2, :], [('QT', j) for j in range(NT)], [])
        em.finish()
    return nc


def rope_table(cfg):
    S = cfg.S
    pos = np.arange(S)
    row = (pos // cfg.GRID_W).astype(np.float32)
    col = (pos % cfg.GRID_W).astype(np.float32)
    half = 64
    freq = (10000.0 ** (-np.arange(0, half, 2, dtype=np.float32) / half)).astype(np.float32)
    ar = row[:, None] * freq[None, :]
    ac = col[:, None] * freq[None, :]
    return np.ascontiguousarray(np.concatenate([np.cos(ar), np.cos(ac), np.sin(ar), np.sin(ac)], axis=1).astype(np.float32))


def bucket_map():
    nb, max_exact, max_distance = 16, 8, 128
    k = np.arange(128)[:, None]
    q = np.arange(128)[None, :]
    out = np.zeros((128, 384), np.float32)
    for d in (-1, 0, 1):
        rel = k - q + 128 * d
        n = np.abs(rel)
        large = max_exact + (np.log(np.maximum(n, 1).astype(np.float32) / max_exact) / math.log(max_distance / max_exact) * (nb - max_exact)).astype(np.int32)
        large = np.minimum(large, nb - 1)
        b = np.where(rel > 0, nb, 0) + np.where(n < max_exact, n, large)
        out[:, (d + 1) * 128:(d + 2) * 128] = b
    return out


def att_inputs(cfg, kind, x, gn, w_in, extra):
    NC = cfg.NC
    maps = []
    x = np.ascontiguousarray(x, dtype=np.float32)
    for c in range(NC):
        m = {"x": x, "gn": np.ascontiguousarray(gn, dtype=np.float32)}
        if kind == 'gqa':
            qd = cfg.QH * 128
            kd = cfg.KVH * 128
            cols = np.concatenate([np.arange(512 * c, 512 * (c + 1)), qd + np.arange(128 * c, 128 * (c + 1)),
                                   qd + kd + np.arange(128 * c, 128 * (c + 1))])
            m["w"] = np.ascontiguousarray(w_in[:, cols][None])
            m["qkg"] = np.ascontiguousarray(np.concatenate([np.tile(extra['qg'], 4), extra['kg']]).astype(np.float32))
            m["rope"] = rope_table(cfg)
        else:
            qk = cfg.DH * 256
            ws = []
            for hh in (2 * c, 2 * c + 1):
                cols = np.concatenate([hh * 256 + np.arange(256), qk + hh * 256 + np.arange(256), 2 * qk + hh * 256 + np.arange(256)])
                ws.append(w_in[:, cols])
            m["w"] = np.ascontiguousarray(np.stack(ws))
            m["lam"] = np.ascontiguousarray(np.concatenate([extra['lq1'], extra['lk1'], extra['lq2'], extra['lk2']]).astype(np.float32))
            m["subg"] = np.ascontiguousarray(extra['subg'], dtype=np.float32)
            m["rb"] = np.ascontiguousarray(extra['rel_bias'][:, 2 * c:2 * c + 2].reshape(-1), dtype=np.float32)
            m["bmap"] = bucket_map()
        maps.append(m)
    return maps


def build_pout(cfg):
    S, D, KC, NT = cfg.S, cfg.D, cfg.KC, cfg.NT
    nc = bass.Bass("TRN2", target_bir_lowering=False)
    oTf = nc.dram_tensor("oTf", [D, S], BF16, kind="ExternalInput").ap()
    wo = nc.dram_tensor("wo", [D, 512], F32, kind="ExternalInput").ap()
    xc = nc.dram_tensor("xc", [S, 512], F32, kind="ExternalInput").ap()
    xo = nc.dram_tensor("xo", [S, 512], F32, kind="ExternalOutput").ap()
    with ExitStack() as es:
        em = Em(nc, es)
        W = _sb(es, nc, 'W', [128, KC, 512], BF16)
        for kc in range(KC):
            em.dma('pool', 'wl', W[:, kc, :], wo[kc * 128:(kc + 1) * 128, :], [], ['W'], new_group=(kc == 0))
        ob = [_sb(es, nc, 'ob%d' % i, [128, KC, 512], BF16) for i in range(2)]
        xb = [_sb(es, nc, 'xb%d' % i, [128, 512], F32) for i in range(2)]
        yb = [_sb(es, nc, 'yb%d' % i, [128, 512], F32) for i in range(2)]
        pp = [_ps(es, nc, 'pp%d' % i, [128, 512], F32) for i in range(2)]
        oview = oTf.rearrange("(kc p) s -> p kc s", p=128)
        n = 0
        for tb in range(S // 512):
            bi = tb % 2
            em.dma('sp', 'ob%d' % bi, ob[bi][:], oview[:, :, tb * 512:(tb + 1) * 512], [], ['ob%d' % bi])
            for j in range(4):
                t = tb * 4 + j
                i = n % 2
                n += 1
                em.dma('sp', 'xb%d' % i, xb[i][:], xc[t * 128:(t + 1) * 128, :], [], ['xb%d' % i])
                em.mm(pp[i][:, :], [(ob[bi][:, kc, j * 128:(j + 1) * 128], W[:, kc, :]) for kc in range(KC)], ['ob%d' % bi, 'W'], ['pp%d' % i])
                em.op('dve', 'tensor_tensor', ['pp%d' % i, 'xb%d' % i], ['yb%d' % i], out=yb[i][:], in0=pp[i][:, :], in1=xb[i][:], op=ALU.add)
                em.dma('sp', 'yo%d' % i, xo[t * 128:(t + 1) * 128, :], yb[i][:], ['yb%d' % i], [])
        em.finish()
    return nc


def build_pout2(cfg):
    S, D, KC, E = cfg.S, cfg.D, cfg.KC, cfg.E
    TL = S // cfg.NC
    TT = TL // 128
    nc = bass.Bass("TRN2", target_bir_lowering=False)
    oTs = nc.dram_tensor("oTs", [D, TL], BF16, kind="ExternalInput").ap()
    wo = nc.dram_tensor("wo", [D, D], F32, kind="ExternalInput").ap()
    xs = nc.dram_tensor("xs", [TL, D], F32, kind="ExternalInput").ap()
    gn = nc.dram_tensor("gn", [D], F32, kind="ExternalInput").ap()
    rw = nc.dram_tensor("rw", [D, E], F32, kind="ExternalInput").ap()
    xm = nc.dram_tensor("xm", [TL, D], F32, kind="ExternalOutput").ap()
    hbo = nc.dram_tensor("hb", [TL, D], BF16, kind="ExternalOutput").ap()
    affo = nc.dram_tensor("aff", [TL, E], F32, kind="ExternalOutput").ap()
    with ExitStack() as es:
        em = Em(nc, es)
        idf, idb, onesb, onesf = _consts(em, es, nc)
        epsb = _sb(es, nc, 'epsb', [128, 1], F32)
        em.op('pool', 'memset', [], ['epsb'], epsb[:], EPS)
        with ExitStack() as pes:
            oT = _sb(pes, nc, 'oT', [128, KC, TL], BF16)
            em.dma('sp', 'ol', oT[:], oTs.rearrange("(kc p) s -> p kc s", p=128), [], ['oT'])
            Wc = [_sb(pes, nc, 'Wc%d' % i, [128, KC, 512], BF16) for i in range(2)]
            xb = [_sb(pes, nc, 'xb%d' % i, [128, 512], F32) for i in range(2)]
            yb = [_sb(pes, nc, 'yb%d' % i, [128, 512], F32) for i in range(2)]
            pp = [_ps(pes, nc, 'pp%d' % i, [128, 512], F32) for i in range(2)]

            def loadW(cg):
                wi = cg % 2
                for kc in range(KC):
                    em.dma('pool', 'wl%d' % wi, Wc[wi][:, kc, :], wo[kc * 128:(kc + 1) * 128, cg * 512:(cg + 1) * 512], [], ['Wc%d' % wi], new_group=(kc == 0))
            loadW(0)
            n = 0
            for cg in range(D // 512):
                if cg + 1 < D // 512:
                    loadW(cg + 1)
                wi = cg % 2
                for tt in range(TT):
                    i = n % 2
                    n += 1
                    em.dma('sp', 'xb%d' % i, xb[i][:], xs[tt * 128:(tt + 1) * 128, cg * 512:(cg + 1) * 512], [], ['xb%d' % i])
                    em.mm(pp[i][:, :], [(oT[:, kc, tt * 128:(tt + 1) * 128], Wc[wi][:, kc, :]) for kc in range(KC)], ['oT', 'Wc%d' % wi], ['pp%d' % i])
                    em.op('dve', 'tensor_tensor', ['pp%d' % i, 'xb%d' % i], ['yb%d' % i], out=yb[i][:], in0=pp[i][:, :], in1=xb[i][:], op=ALU.add)
                    em.dma('sp', 'yo%d' % i, xm[tt * 128:(tt + 1) * 128, cg * 512:(cg + 1) * 512], yb[i][:], ['yb%d' % i], [])
        em.barrier()
        with ExitStack() as pes:
            gb = _sb(pes, nc, 'gb', [128, D], F32)
            em.dma('sp', 'c0', gb[:], gn.rearrange("(o n) -> o n", o=1).partition_broadcast(128), [], ['gb'])
            rws = _sb(pes, nc, 'rws', [128, KC, E], F32)
            em.dma('sp', 'c1', rws[:], rw.rearrange("(kc p) e -> p kc e", p=128), [], ['rws'])
            xt = [_sb(pes, nc, 'xt%d' % i, [128, D], F32) for i in range(2)]
            h32 = _sb(pes, nc, 'h32', [128, D], F32)
            hb = [_sb(pes, nc, 'hb%d' % i, [128, D], BF16) for i in range(2)]
            hT32 = _sb(pes, nc, 'hT32', [128, KC, 128], F32)
            ss = _sb(pes, nc, 'ss', [128, 8], F32)
            rs = _sb(pes, nc, 'rs', [128, 8], F32)
            tmp = _sb(pes, nc, 'tmp', [128, 8], F32)
            ex = _sb(pes, nc, 'ex', [128, E], F32)
            af = [_sb(pes, nc, 'af%d' % i, [128, E], F32) for i in range(2)]
            ptf = [_ps(pes, nc, 'ptf%d' % i, [128, 4, 128], F32) for i in range(2)]
            pl = _ps(pes, nc, 'pl', [128, 512], F32)
            em.dma('sp', 'xin0', xt[0][:], xm[0:128, :], [], ['xt0'])
            for t in range(TT):
                i = t % 2
                if t + 1 < TT:
                    em.dma('sp', 'xin%d' % (1 - i), xt[1 - i][:], xm[(t + 1) * 128:(t + 2) * 128, :], [], ['xt%d' % (1 - i)])
                xk = 'xt%d' % i
                em.op('act', 'activation', [xk], ['h32', 'ss'], out=h32[:], in_=xt[i][:], func=AF.Square, accum_out=ss[:, 0:1])
                _rstd(em, nc, ss[:, 0:1], rs[:, 0:1], tmp[:, 0:1], D, 'ss', 'rs', 'tmp', epsb[:, 0:1])
                em.op('dve', 'scalar_tensor_tensor', [xk, 'rs', 'gb'], ['h32'], out=h32[:], in0=xt[i][:], scalar=rs[:, 0:1], in1=gb[:], op0=ALU.mult, op1=ALU.mult)
                em.op('act', 'copy', ['h32'], ['hb%d' % i], out=hb[i][:], in_=h32[:])
                em.dma('sp', 'hout%d' % i, hbo[t * 128:(t + 1) * 128, :], hb[i][:], ['hb%d' % i], [])
                for b in range(KC // 4):
                    pk = 'ptf%d' % (b % 2)
                    p = ptf[b % 2]
                    em.mm_multi([(p[:, j, :], h32[:, (b * 4 + j) * 128:(b * 4 + j + 1) * 128], idf[:], True) for j in range(4)], ['h32', 'idf'], [pk])
                    em.op('act', 'copy', [pk], ['hT32'], out=hT32[:, b * 4:(b + 1) * 4, :], in_=p[:, :, :])
                em.mm(pl[:, 0:E], [(hT32[:, kc, :], rws[:, kc, :]) for kc in range(KC)], ['hT32', 'rws'], ['pl'])
                em.op('dve', 'tensor_reduce', ['pl'], ['ss'], out=ss[:, 1:2], in_=pl[:, 0:E], axis=AX.X, op=ALU.max)
                em.op('dve', 'tensor_scalar', ['ss'], ['ss'], out=ss[:, 2:3], in0=ss[:, 1:2], scalar1=-1.0, scalar2=None, op0=ALU.mult)
                em.op('act', 'activation', ['pl', 'ss'], ['ex', 'ss'], out=ex[:], in_=pl[:, 0:E], func=AF.Exp, bias=ss[:, 2:3], accum_out=ss[:, 3:4])
                em.op('dve', 'reciprocal', ['ss'], ['ss'], out=ss[:, 4:5], in_=ss[:, 3:4])
                em.op('dve', 'tensor_scalar', ['ex', 'ss'], ['af%d' % i], out=af[i][:], in0=ex[:], scalar1=ss[:, 4:5], scalar2=None, op0=ALU.mult)
                em.dma('sp', 'aout%d' % i, affo[t * 128:(t + 1) * 128, :], af[i][:], ['af%d' % i], [])
        em.finish()
    return nc


def build_pfin(cfg):
    S, D = cfg.S, cfg.D
    TL = S // cfg.NC
    nc = bass.Bass("TRN2", target_bir_lowering=False)
    x = nc.dram_tensor("x", [TL, D], F32, kind="ExternalInput").ap()
    g = nc.dram_tensor("g", [D], F32, kind="ExternalInput").ap()
    y = nc.dram_tensor("y", [TL, D], F32, kind="ExternalOutput").ap()
    with ExitStack() as es:
        em = Em(nc, es)
        epsb = _sb(es, nc, 'epsb', [128, 1], F32)
        em.op('pool', 'memset', [], ['epsb'], epsb[:], EPS)
        gb = _sb(es, nc, 'gb', [128, D], F32)
        em.dma('sp', 'c0', gb[:], g.rearrange("(o n) -> o n", o=1).partition_broadcast(128), [], ['gb'])
        xt = [_sb(es, nc, 'xt%d' % i, [128, D], F32) for i in range(2)]
        yt = [_sb(es, nc, 'yt%d' % i, [128, D], F32) for i in range(2)]
        ss = _sb(es, nc, 'ss', [128, 2], F32)
        rs = _sb(es, nc, 'rs', [128, 2], F32)
        tmp = _sb(es, nc, 'tmp', [128, 2], F32)
        for t in range(TL // 128):
            i = t % 2
            em.dma('sp', 'x%d' % i, xt[i][:], x[t * 128:(t + 1) * 128, :], [], ['xt%d' % i])
            em.op('act', 'activation', ['xt%d' % i], ['yt%d' % i, 'ss%d' % i], out=yt[i][:], in_=xt[i][:], func=AF.Square, accum_out=ss[:, i:i + 1])
            _rstd(em, nc, ss[:, i:i + 1], rs[:, i:i + 1], tmp[:, i:i + 1], D, 'ss%d' % i, 'rs%d' % i, 'tmp%d' % i, epsb[:, 0:1])
            em.op('dve', 'scalar_tensor_tensor', ['xt%d' % i, 'rs%d' % i, 'gb'], ['yt%d' % i], out=yt[i][:], in0=xt[i][:], scalar=rs[:, i:i + 1], in1=gb[:],
                  op0=ALU.mult, op1=ALU.mult)
            em.dma('sp', 'y%d' % i, y[t * 128:(t + 1) * 128, :], yt[i][:], ['yt%d' % i], [])
        em.finish()
    return nc


def build_moe(cfg, NIT=30, ext=True):
    S, D, KC, NT, E, FF, FC, CAP, CT = cfg.S, cfg.D, cfg.KC, cfg.NT, cfg.E, cfg.FF, cfg.FC, cfg.CAP, cfg.CT
    J = S // 128
    nc = bass.Bass("TRN2", target_bir_lowering=False)
    if ext:
        hs = nc.dram_tensor("hs", [S, D], BF16, kind="ExternalInput").ap()
        Ain = nc.dram_tensor("A", [128, 2, J], F32, kind="ExternalInput").ap()
    else:
        x = nc.dram_tensor("x", [S, D], F32, kind="ExternalInput").ap()
        gn = nc.dram_tensor("gn", [D], F32, kind="ExternalInput").ap()
        rw = nc.dram_tensor("rw", [D, E], F32, kind="ExternalInput").ap()
    wg = nc.dram_tensor("wg", [2, D, FF], F32, kind="ExternalInput").ap()
    wu = nc.dram_tensor("wu", [2, D, FF], F32, kind="ExternalInput").ap()
    wd = nc.dram_tensor("wd", [2, FF, D], F32, kind="ExternalInput").ap()
    y = nc.dram_tensor("y", [2, CAP, D], F32, kind="ExternalOutput").ap()
    idxo = nc.dram_tensor("idx", [2, 128, CT], I32, kind="ExternalOutput").ap()
    if not ext:
        hs = nc.dram_tensor("hs_scr", [S, D], BF16).ap()
        affd = nc.dram_tensor("aff_scr", [2, S], F32).ap()
    with ExitStack() as es:
        em = Em(nc, es)
        idf, idb, onesb, onesf = _consts(em, es, nc)
        epsb = _sb(es, nc, 'epsb', [128, 1], F32)
        em.op('pool', 'memset', [], ['epsb'], epsb[:], EPS)
        A = _sb(es, nc, 'A', [128, 2, J], F32)
        idxi = _sb(es, nc, 'idxi', [128, 2, CT], I32)
        gate = _sb(es, nc, 'gate', [128, 2, CT], F32)
        if ext:
            em.dma('sp', 'affi', A[:], Ain[:, :, :], [], ['A'])
        if not ext:
            with ExitStack() as pes:
                gb = _sb(pes, nc, 'gb', [128, D], F32)
                em.dma('sp', 'c0', gb[:], gn.rearrange("(o n) -> o n", o=1).partition_broadcast(128), [], ['gb'])
                rws = _sb(pes, nc, 'rws', [128, KC, E], F32)
                em.dma('sp', 'c1', rws[:], rw.rearrange("(kc p) e -> p kc e", p=128), [], ['rws'])
                xt = _sb(pes, nc, 'xt', [128, D], F32)
                h32 = _sb(pes, nc, 'h32', [128, D], F32)
                hb = _sb(pes, nc, 'hb', [128, D], BF16)
                hT32 = _sb(pes, nc, 'hT32', [128, KC, 128], F32)
                ss = _sb(pes, nc, 'ss', [128, 8], F32)
                rs = _sb(pes, nc, 'rs', [128, 8], F32)
                tmp = _sb(pes, nc, 'tmp', [128, 8], F32)
                ex = _sb(pes, nc, 'ex', [128, E], F32)
                aff2 = _sb(pes, nc, 'aff2', [128, 2], F32)
                affT = _sb(pes, nc, 'affT', [2, S], F32)
                ptf = [_ps(pes, nc, 'ptf%d' % i, [128, 4, 128], F32) for i in range(2)]
                pl = _ps(pes, nc, 'pl', [128, 512], F32)
                pa = _ps(pes, nc, 'pa', [128, 512], F32)
                for t in range(NT):
                    em.dma('sp', 'xin', xt[:], x[t * 128:(t + 1) * 128, :], [], ['xt'])
                    em.op('act', 'activation', ['xt'], ['h32', 'ss'], out=h32[:], in_=xt[:], func=AF.Square, accum_out=ss[:, 0:1])
                    _rstd(em, nc, ss[:, 0:1], rs[:, 0:1], tmp[:, 0:1], D, 'ss', 'rs', 'tmp', epsb[:, 0:1])
                    em.op('dve', 'scalar_tensor_tensor', ['xt', 'rs', 'gb'], ['h32'], out=h32[:], in0=xt[:], scalar=rs[:, 0:1], in1=gb[:], op0=ALU.mult, op1=ALU.mult)
                    em.op('act', 'copy', ['h32'], ['hb'], out=hb[:], in_=h32[:])
                    em.dma('sp', 'hout', hs[t * 128:(t + 1) * 128, :], hb[:], ['hb'], ['hs'])
                    for b in range(KC // 4):
                        pk = 'ptf%d' % (b % 2)
                        p = ptf[b % 2]
                        em.mm_multi([(p[:, j, :], h32[:, (b * 4 + j) * 128:(b * 4 + j + 1) * 128], idf[:], True) for j in range(4)], ['h32', 'idf'], [pk])
                        em.op('act', 'copy', [pk], ['hT32'], out=hT32[:, b * 4:(b + 1) * 4, :], in_=p[:, :, :])
                    em.mm(pl[:, 0:E], [(hT32[:, kc, :], rws[:, kc, :]) for kc in range(KC)], ['hT32', 'rws'], ['pl'])
                    em.op('dve', 'tensor_reduce', ['pl'], ['ss'], out=ss[:, 1:2], in_=pl[:, 0:E], axis=AX.X, op=ALU.max)
                    em.op('dve', 'tensor_scalar', ['ss'], ['ss'], out=ss[:, 2:3], in0=ss[:, 1:2], scalar1=-1.0, scalar2=None, op0=ALU.mult)
                    em.op('act', 'activation', ['pl', 'ss'], ['ex', 'ss'], out=ex[:], in_=pl[:, 0:E], func=AF.Exp, bias=ss[:, 2:3], accum_out=ss[:, 3:4])
                    em.op('dve', 'reciprocal', ['ss'], ['ss'], out=ss[:, 4:5], in_=ss[:, 3:4])
                    em.op('dve', 'tensor_scalar', ['ex', 'ss'], ['aff2'], out=aff2[:], in0=ex[:, 0:2], scalar1=ss[:, 4:5], scalar2=None, op0=ALU.mult)
                    em.mm_multi([(pa[0:2, 0:128], aff2[:, 0:2], idf[:], True)], ['aff2', 'idf'], ['pa'])
                    em.op('act', 'copy', ['pa'], ['affT'], out=affT[:, t * 128:(t + 1) * 128], in_=pa[0:2, 0:128])
                em.dma('sp', 'affo', affd[:, :], affT[:], ['affT'], ['affd'])
                em.dma('sp', 'affi', A[:], affd.rearrange("e (p j) -> p e j", j=J), ['affd'], ['A'])
        em.barrier()
        with ExitStack() as pes:
            lo = _sb(pes, nc, 'lo', [128, 2], F32)
            hi = _sb(pes, nc, 'hi', [128, 2], F32)
            mid = _sb(pes, nc, 'mid', [128, 2], F32)
            cnt = _sb(pes, nc, 'cnt', [128, 2], F32)
            gem = _sb(pes, nc, 'gem', [128, 2], U32)
            ltm = _sb(pes, nc, 'ltm', [128, 2], U32)
            cmpb = _sb(pes, nc, 'cmpb', [128, 2, J], F32)
            cs = _sb(pes, nc, 'cs', [128, 2, J], F32)
            pos = _sb(pes, nc, 'pos', [128, 2, J], F32)
            onesJ = _sb(pes, nc, 'onesJ', [128, J], F32)
            off = _sb(pes, nc, 'off', [128, 2], F32)
            totp = _sb(pes, nc, 'totp', [128, 2], F32)
            U = _sb(pes, nc, 'U', [128, 128], F32)
            iot = _sb(pes, nc, 'iot', [128, CAP], F32)
            rhsv = _sb(pes, nc, 'rhsv', [128, 2, J, 2], F32)
            OH = [_sb(pes, nc, 'OH%d' % i, [128, CAP], F32) for i in range(2)]
            res4 = _sb(pes, nc, 'res4', [128, CT, 2], F32)
            pc = _ps(pes, nc, 'pc', [128, 512], F32)
            pacc = _ps(pes, nc, 'pacc', [128, CT, 512], F32) if CT <= 7 else None
            if pacc is None:
                pacc = _ps(pes, nc, 'pacc', [128, 7, 512], F32)
            em.op('pool', 'memset', [], ['lo'], lo[:], 0.0)
            em.op('pool', 'memset', [], ['hi'], hi[:], 1.0)
            em.op('pool', 'memset', [], ['onesJ'], onesJ[:], 1.0)
            em.op('pool', 'memset', [], ['U'], U[:], 1.0)
            em.op('pool', 'affine_select', ['U'], ['U'], out=U[:], in_=U[:], pattern=[[1, 128]], compare_op=ALU.is_gt, fill=0.0, base=0, channel_multiplier=-1)
            em.op('pool', 'iota', [], ['iot'], iot[:], pattern=[[1, CAP]], base=0, channel_multiplier=0, allow_small_or_imprecise_dtypes=True)
            for e in range(2):
                em.op('pool', 'iota', [], ['rhsv'], rhsv[:, e, :, 0], pattern=[[1, J]], base=0, channel_multiplier=J, allow_small_or_imprecise_dtypes=True)
                em.op('dve', 'tensor_copy', ['A', 'rhsv'], ['rhsv'], out=rhsv[:, e, :, 1], in_=A[:, e, :])
            for it in range(NIT):
                em.op('dve', 'tensor_tensor', ['lo', 'hi'], ['mid'], out=mid[:], in0=lo[:], in1=hi[:], op=ALU.add)
                em.op('dve', 'tensor_scalar', ['mid'], ['mid'], out=mid[:], in0=mid[:], scalar1=0.5, scalar2=None, op0=ALU.mult)
                for e in range(2):
                    em.op('dve', 'tensor_scalar', ['A', 'mid'], ['cmpb', 'cnt'], out=cmpb[:, e, :], in0=A[:, e, :], scalar1=mid[:, e:e + 1], scalar2=0.0,
                          op0=ALU.is_gt, op1=ALU.add, accum_out=cnt[:, e:e + 1])
                em.mm(pc[:, 0:2], [(onesf[:], cnt[:])], ['cnt', 'onesf'], ['pc'])
                em.op('dve', 'tensor_scalar', ['pc'], ['gem'], out=gem[:], in0=pc[:, 0:2], scalar1=float(CAP), scalar2=None, op0=ALU.is_ge)
                em.op('dve', 'tensor_scalar', ['pc'], ['ltm'], out=ltm[:], in0=pc[:, 0:2], scalar1=float(CAP), scalar2=None, op0=ALU.is_lt)
                em.op('dve', 'copy_predicated', ['gem', 'mid'], ['lo'], out=lo[:], mask=gem[:], data=mid[:])
                em.op('dve', 'copy_predicated', ['ltm', 'mid'], ['hi'], out=hi[:], mask=ltm[:], data=mid[:])
            for e in range(2):
                em.op('dve', 'tensor_scalar', ['A', 'lo'], ['cmpb'], out=cmpb[:, e, :], in0=A[:, e, :], scalar1=lo[:, e:e + 1], scalar2=None, op0=ALU.is_gt)
                em.op('dve', 'tensor_tensor_scan', ['cmpb', 'onesJ'], ['cs'], out=cs[:, e, :], data0=onesJ[:], data1=cmpb[:, e, :], initial=0.0, op0=ALU.mult, op1=ALU.add)
                em.op('dve', 'tensor_copy', ['cs'], ['totp'], out=totp[:, e:e + 1], in_=cs[:, e, J - 1:J])
            em.mm(pc[:, 0:2], [(U[:], totp[:])], ['totp', 'U'], ['pc'])
            em.op('act', 'copy', ['pc'], ['off'], out=off[:], in_=pc[:, 0:2])
            for e in range(2):
                em.op('dve', 'tensor_scalar', ['cs', 'off'], ['pos'], out=pos[:, e, :], in0=cs[:, e, :], scalar1=off[:, e:e + 1], scalar2=None, op0=ALU.add)
                em.op('dve', 'tensor_tensor', ['pos', 'cmpb'], ['pos'], out=pos[:, e, :], in0=pos[:, e, :], in1=cmpb[:, e, :], op=ALU.mult)
                em.op('dve', 'tensor_scalar', ['pos'], ['pos'], out=pos[:, e, :], in0=pos[:, e, :], scalar1=-1.0, scalar2=None, op0=ALU.add)
            for e in range(2):
                for c0 in range(0, CT, 7):
                    cts = list(range(c0, min(CT, c0 + 7)))
                    for j in range(J):
                        i = j % 2
                        em.op('dve', 'tensor_scalar', ['iot', 'pos'], ['OH%d' % i], out=OH[i][:], in0=iot[:], scalar1=pos[:, e, j:j + 1], scalar2=None, op0=ALU.is_equal)
                        first, last = j == 0, j == J - 1
                        em._deps('pe', ['OH%d' % i, 'rhsv'], ['pacc'] if first else [])
                        ins = None
                        for ct in cts:
                            ins = nc.tensor.matmul(pacc[:, ct - c0, 0:2], OH[i][:, ct * 128:(ct + 1) * 128], rhsv[:, e, j, :], start=first, stop=last)
                        em.ecnt['pe'] += 1
                        ins.then_inc(em.esem['pe'], 1)
                        ev = ('s_pe', em.esem['pe'], em.ecnt['pe'], 'pe')
                        em._record(ev, ['OH%d' % i, 'rhsv'], ['pacc'] if (first or last) else [])
                    em.op('act', 'copy', ['pacc'], ['res4'], out=res4[:, c0:c0 + len(cts), :], in_=pacc[:, 0:len(cts), 0:2])
                em.op('dve', 'tensor_copy', ['res4'], ['idxi'], out=idxi[:, e, :], in_=res4[:, :, 0])
                em.op('dve', 'tensor_copy', ['res4'], ['gate'], out=gate[:, e, :], in_=res4[:, :, 1])
            em.dma('sp', 'idxo', idxo.rearrange("e p c -> p e c"), idxi[:], ['idxi'], [])
        em.barrier()
        xinT = _sb(es, nc, 'xinT', [128, KC, CAP], BF16)
        actT = _sb(es, nc, 'actT', [128, FC, CAP], BF16)
        for e in range(2):
            with ExitStack() as pes:
                xg = [_sb(pes, nc, 'xg%d' % i, [128, D], BF16) for i in range(2)]
                Wgh = _sb(pes, nc, 'Wgh', [128, KC, 512], BF16)
                Wuh = _sb(pes, nc, 'Wuh', [128, KC, 512], BF16)
                sg = _sb(pes, nc, 'sg', [128, 512], F32)
                ptr = [_ps(pes, nc, 'ptr%d' % i, [128, 8, 128], BF16) for i in range(2)]
                pg = _ps(pes, nc, 'pg', [128, 512], F32)
                pu = _ps(pes, nc, 'pu', [128, 512], F32)
                for ct in range(CT):
                    i = ct % 2
                    em.dma('pool', 'xg%d' % i, xg[i][:], hs[:, :], ['hs', 'idxi'], ['xg%d' % i],
                           indirect=dict(out_offset=None, in_offset=bass.IndirectOffsetOnAxis(ap=idxi[:, e, ct:ct + 1], axis=0)))
                    nb = max(1, KC // 8)
                    per = min(8, KC)
                    for b in range(nb):
                        pk = 'ptr%d' % (b % 2)
                        em.mm_multi([(ptr[b % 2][:, j, :], xg[i][:, (b * per + j) * 128:(b * per + j + 1) * 128], idb[:], True) for j in range(per)], ['xg%d' % i, 'idb'], [pk])
                        em.op('act', 'copy', [pk], ['xinT'], out=xinT[:, b * per:(b + 1) * per, ct * 128:(ct + 1) * 128], in_=ptr[b % 2][:, 0:per, :])
                for (f0, nfc) in [(0, 4), (4, FC - 4)]:
                    for kc in range(KC):
                        em.dma('pool', 'wgl', Wgh[:, kc, 0:nfc * 128], wg[e, kc * 128:(kc + 1) * 128, f0 * 128:(f0 + nfc) * 128], [], ['Wgh'], new_group=(kc == 0))
                        em.dma('pool', 'wul', Wuh[:, kc, 0:nfc * 128], wu[e, kc * 128:(kc + 1) * 128, f0 * 128:(f0 + nfc) * 128], [], ['Wuh'], new_group=(kc == 0))
                    for fcl in range(nfc):
                        for th in range(CAP // 512):
                            em.mm(pg[:, :], [(Wgh[:, kc, fcl * 128:(fcl + 1) * 128], xinT[:, kc, th * 512:(th + 1) * 512]) for kc in range(KC)], ['Wgh', 'xinT'], ['pg'])
                            em.mm(pu[:, :], [(Wuh[:, kc, fcl * 128:(fcl + 1) * 128], xinT[:, kc, th * 512:(th + 1) * 512]) for kc in range(KC)], ['Wuh', 'xinT'], ['pu'])
                            em.op('act', 'activation', ['pg'], ['sg'], out=sg[:], in_=pg[:, :], func=AF.Silu)
                            em.op('dve', 'tensor_tensor', ['pu', 'sg'], ['actT'], out=actT[:, f0 + fcl, th * 512:(th + 1) * 512], in0=pu[:, :], in1=sg[:], op=ALU.mult)
            em.barrier()
            with ExitStack() as pes:
                Wd = _sb(pes, nc, 'Wd', [128, FC, D], BF16)
                for fc in range(FC):
                    em.dma('pool', 'wdl', Wd[:, fc, :], wd[e, fc * 128:(fc + 1) * 128, :], [], ['Wd'], new_group=(fc == 0))
                ysb = [_sb(pes, nc, 'ysb%d' % i, [128, D], F32) for i in range(2)]
                py = [_ps(pes, nc, 'py%d' % i, [128, 512], F32) for i in range(2)]
                n = 0
                for tt in range(CT):
                    b = tt % 2
                    for cg in range(D // 512):
                        i = n % 2
                        n += 1
                        em.mm(py[i][:, :], [(actT[:, fc, tt * 128:(tt + 1) * 128], Wd[:, fc, cg * 512:(cg + 1) * 512]) for fc in range(FC)], ['actT', 'Wd'], ['py%d' % i])
                        em.op('dve', 'tensor_scalar', ['py%d' % i, 'gate'], ['ysb%d' % b], out=ysb[b][:, cg * 512:(cg + 1) * 512], in0=py[i][:, :], scalar1=gate[:, e, tt:tt + 1],
                              scalar2=None, op0=ALU.mult)
                    em.dma('sp', 'yo%d' % b, y[e, tt * 128:(tt + 1) * 128, :], ysb[b][:], ['ysb%d' % b], [])
            em.barrier()
        em.finish()
    return nc


def build_pcomb(cfg):
    S, E, CAP, CT = cfg.S, cfg.E, cfg.CAP, cfg.CT
    nc = bass.Bass("TRN2", target_bir_lowering=False)
    xm = nc.dram_tensor("xm", [S, 512], F32, kind="ExternalInput").ap()
    ys = nc.dram_tensor("ys", [E, CAP, 512], F32, kind="ExternalInput").ap()
    idx = nc.dram_tensor("idx", [E, 128, CT], I32, kind="ExternalInput").ap()
    xo = nc.dram_tensor("xo", [S, 512], F32, kind="ExternalOutput").ap()
    with ExitStack() as es:
        em = Em(nc, es)
        ii = _sb(es, nc, 'ii', [128, E, CT], I32)
        em.dma('sp', 'c0', ii[:], idx.rearrange("e p c -> p e c"), [], ['ii'])
        em.dma('sp', 'cp', xo[:, :], xm[:, :], [], ['xo'])
        yb = [_sb(es, nc, 'yb%d' % i, [128, 512], F32) for i in range(4)]
        n = 0
        for e in range(E):
            for ct in range(CT):
                i = n % 4
                n += 1
                em.dma('sp', 'yl%d' % i, yb[i][:], ys[e, ct * 128:(ct + 1) * 128, :], [], ['yb%d' % i])
                em.dma('pool', 'sc', xo[:, :], yb[i][:], ['yb%d' % i, 'ii', 'xo'], ['xo'] if ct == CT - 1 else [], new_group=(ct == 0),
                       indirect=dict(out_offset=bass.IndirectOffsetOnAxis(ap=ii[:, e, ct:ct + 1], axis=0), in_offset=None, compute_op=ALU.add))
        em.finish()
    return nc


def moe_inputs(cfg, x, gn, rw, wg, wu, wd):
    maps = []
    x = np.ascontiguousarray(x, dtype=np.float32)
    for c in range(cfg.NC):
        mine = [2 * c, 2 * c + 1]
        perm = mine + [e for e in range(cfg.E) if e not in mine]
        maps.append({"x": x, "gn": np.ascontiguousarray(gn), "rw": np.ascontiguousarray(rw[:, perm]),
                     "wg": np.ascontiguousarray(wg[mine]), "wu": np.ascontiguousarray(wu[mine]), "wd": np.ascontiguousarray(wd[mine])})
    return maps


_PROGS = {}


def _prog(name, fn):
    if name not in _PROGS:
        _PROGS[name] = fn()
    return _PROGS[name]


_PROF = []


def _run(nc, maps, n, tag=''):
    import os
    if os.environ.get('KPROF'):
        r = run_bass_kernel_spmd(nc, maps, core_ids=list(range(n)), trace=True)
        _PROF.append((tag, r.exec_time_ns))
        print('KPROF', tag, r.exec_time_ns, flush=True)
        return r.results
    return run_bass_kernel_spmd(nc, maps, core_ids=list(range(n))).results


def forward(cfg, inp):
    NC, S, D = cfg.NC, cfg.S, cfg.D
    x = np.ascontiguousarray(np.asarray(inp['x'], dtype=np.float32).reshape(S, D))
    for i in range(cfg.DEPTH):
        j = i // 2
        if i % 2 == 0:
            nc = _prog('gqa', lambda: build_att(cfg, 'gqa'))
            maps = att_inputs(cfg, 'gqa', x, inp['attn_norm'][i], np.asarray(inp['gqa_w_in'][j]),
                              dict(qg=np.asarray(inp['gqa_q_norm'][j]), kg=np.asarray(inp['gqa_k_norm'][j])))
            w_out = np.asarray(inp['gqa_w_out'][j])
        else:
            li = 0.8 - 0.6 * math.exp(-0.3 * i)
            nc = _prog('diff%d' % i, lambda: build_att(cfg, 'diff', lambda_init=li))
            maps = att_inputs(cfg, 'diff', x, inp['attn_norm'][i], np.asarray(inp['diff_w_in'][j]),
                              dict(lq1=np.asarray(inp['diff_lambda_q1'][j]), lk1=np.asarray(inp['diff_lambda_k1'][j]),
                                   lq2=np.asarray(inp['diff_lambda_q2'][j]), lk2=np.asarray(inp['diff_lambda_k2'][j]),
                                   subg=np.asarray(inp['diff_sub_norm'][j]), rel_bias=np.asarray(inp['rel_bias'])))
            w_out = np.asarray(inp['diff_w_out'][j])
        res = _run(nc, maps, NC, 'att%d' % i)
        oTf = np.ascontiguousarray(np.concatenate([np.asarray(r['oT']) for r in res], axis=0))
        TLc = S // NC
        J = S // 128
        nc = _prog('pout2', lambda: build_pout2(cfg))
        gn2 = np.ascontiguousarray(np.asarray(inp['ffn_norm'][i], dtype=np.float32))
        rwf = np.ascontiguousarray(np.asarray(inp['router_w'][i], dtype=np.float32))
        w_out = np.ascontiguousarray(w_out)
        maps = [{"oTs": np.ascontiguousarray(oTf[:, c * TLc:(c + 1) * TLc]), "wo": w_out, "xs": np.ascontiguousarray(x[c * TLc:(c + 1) * TLc]),
                 "gn": gn2, "rw": rwf} for c in range(NC)]
        res = _run(nc, maps, NC, 'pout%d' % i)
        xm = np.ascontiguousarray(np.concatenate([np.asarray(r['xm']) for r in res], axis=0))
        hbf = np.ascontiguousarray(np.concatenate([np.asarray(r['hb']) for r in res], axis=0))
        aff = np.concatenate([np.asarray(r['aff']) for r in res], axis=0)
        nc = _prog('moe', lambda: build_moe(cfg))
        wg_, wu_, wd_ = np.asarray(inp['expert_w_gate'][i]), np.asarray(inp['expert_w_up'][i]), np.asarray(inp['expert_w_down'][i])
        maps = []
        for c in range(NC):
            A_c = np.ascontiguousarray(aff[:, 2 * c:2 * c + 2].T.reshape(2, 128, J).transpose(1, 0, 2))
            maps.append({"hs": hbf, "A": A_c, "wg": np.ascontiguousarray(wg_[2 * c:2 * c + 2]), "wu": np.ascontiguousarray(wu_[2 * c:2 * c + 2]),
                         "wd": np.ascontiguousarray(wd_[2 * c:2 * c + 2])})
        res = _run(nc, maps, NC, 'moe%d' % i)
        ys = np.concatenate([np.asarray(r['y']) for r in res], axis=0)
        idx = np.ascontiguousarray(np.concatenate([np.asarray(r['idx']) for r in res], axis=0))
        nc = _prog('pcomb', lambda: build_pcomb(cfg))
        maps = [{"xm": np.ascontiguousarray(xm[:, c * 512:(c + 1) * 512]), "ys": np.ascontiguousarray(ys[:, :, c * 512:(c + 1) * 512]), "idx": idx} for c in range(NC)]
        res = _run(nc, maps, NC, 'pcomb%d' % i)
        x = np.ascontiguousarray(np.concatenate([np.asarray(r['xo']) for r in res], axis=1))
    nc = _prog('pfin', lambda: build_pfin(cfg))
    TL = S // NC
    maps = [{"x": np.ascontiguousarray(x[c * TL:(c + 1) * TL]), "g": np.ascontiguousarray(np.asarray(inp['final_norm'], dtype=np.float32))} for c in range(NC)]
    res = _run(nc, maps, NC)
    out = np.concatenate([np.asarray(r['y']) for r in res], axis=0)
    return out.reshape(1, S, D).astype(np.float32)


def kernel(**inputs):
    cfg = Cfg()
    return forward(cfg, inputs)
```

```python
import math
from contextlib import ExitStack
import numpy as np
import concourse.bass as bass
import concourse.mybir as mybir
from concourse.bass_utils import run_bass_kernel_spmd

F32 = mybir.dt.float32
BF16 = mybir.dt.bfloat16
I32 = mybir.dt.int32
U32 = mybir.dt.uint32
AF = mybir.ActivationFunctionType
ALU = mybir.AluOpType
AX = mybir.AxisListType

EPS = 1e-6


class Cfg:
    def __init__(self, NC=8, S=8192, D=4096, E=16, FF=896, GRID_W=64, DEPTH=4):
        self.NC, self.S, self.D, self.E, self.FF, self.GRID_W, self.DEPTH = NC, S, D, E, FF, GRID_W, DEPTH
        self.KC = D // 128
        self.NT = S // 128
        self.CAP = 2 * S // E
        self.CT = self.CAP // 128
        self.FC = FF // 128
        self.QH = 4 * NC
        self.KVH = NC
        self.DH = 2 * NC
        self.CW = D // NC
        assert self.CW == 512 and E == 2 * NC


class Em:
    def __init__(self, nc, es):
        self.nc, self.es = nc, es
        self.E = {'pe': nc.tensor, 'act': nc.scalar, 'dve': nc.vector, 'pool': nc.gpsimd, 'sp': nc.sync}
        self.esem = {e: es.enter_context(nc.semaphore('s_' + e)) for e in ['pe', 'act', 'dve', 'pool']}
        self.ecnt = {e: 0 for e in self.esem}
        self.waited = {e: {} for e in self.E}
        self.dsem = {}
        self.res = {}
        self.limit = None
        self.ncalls = 0

    def _wait(self, eng, evs):
        need = {}
        for (name, sem, val, src) in evs:
            if src == 'pe' and eng == 'pe':
                continue
            if val <= 0:
                continue
            if need.get(name, (None, 0))[1] < val:
                need[name] = (sem, val)
        for name, (sem, val) in need.items():
            if self.waited[eng].get(name, 0) >= val:
                continue
            self.E[eng].wait_ge(sem, val)
            self.waited[eng][name] = val

    def _deps(self, eng, reads, writes):
        evs = []
        for r in reads:
            st = self.res.get(r)
            if st and st['w']:
                evs.append(st['w'])
        for w in writes:
            st = self.res.get(w)
            if st:
                if st['w']:
                    evs.append(st['w'])
                evs.extend(st['r'].values())
        self._wait(eng, evs)

    def _record(self, ev, reads, writes):
        for r in reads:
            st = self.res.setdefault(r, {'w': None, 'r': {}})
            old = st['r'].get(ev[0])
            if old is None or old[2] < ev[2]:
                st['r'][ev[0]] = ev
        for w in writes:
            self.res[w] = {'w': ev, 'r': {}}

    def _lim(self):
        self.ncalls += 1
        return self.limit is not None and self.ncalls > self.limit

    def op(self, eng, fn, reads, writes, *args, **kw):
        if self._lim():
            return None
        self._deps(eng, reads, writes)
        ins = getattr(self.E[eng], fn)(*args, **kw)
        self.ecnt[eng] += 1
        ins.then_inc(self.esem[eng], 1)
        ev = ('s_' + eng, self.esem[eng], self.ecnt[eng], eng)
        self._record(ev, reads, writes)
        return ins

    def mm(self, out, pairs, reads, writes, transpose=False):
        if self._lim():
            return
        self._deps('pe', reads, writes)
        n = len(pairs)
        ins = None
        for i, (a, b) in enumerate(pairs):
            if transpose:
                ins = self.nc.tensor.transpose(out=out, in_=a, identity=b)
            else:
                ins = self.nc.tensor.matmul(out, a, b, start=(i == 0), stop=(i == n - 1))
        self.ecnt['pe'] += 1
        ins.then_inc(self.esem['pe'], 1)
        ev = ('s_pe', self.esem['pe'], self.ecnt['pe'], 'pe')
        self._record(ev, reads, writes)

    def mm_multi(self, items, reads, writes):
        if self._lim():
            return
        self._deps('pe', reads, writes)
        ins = None
        for (o, a, b, tr) in items:
            if tr:
                ins = self.nc.tensor.transpose(out=o, in_=a, identity=b)
            else:
                ins = self.nc.tensor.matmul(o, a, b, start=True, stop=True)
        self.ecnt['pe'] += 1
        ins.then_inc(self.esem['pe'], 1)
        ev = ('s_pe', self.esem['pe'], self.ecnt['pe'], 'pe')
        self._record(ev, reads, writes)

    def _dkey(self, key):
        if key not in self.dsem:
            self.dsem[key] = [self.es.enter_context(self.nc.semaphore('d_' + key)), 0]
        return self.dsem[key]

    def dma(self, eng, key, out, in_, reads, writes, new_group=True, indirect=None, **kw):
        if self._lim():
            return
        ent = self._dkey(key)
        if new_group:
            self._wait(eng, [('d_' + key, ent[0], ent[1], 'dma')])
        self._deps(eng, reads, writes)
        if indirect is None:
            ins = self.E[eng].dma_start(out=out, in_=in_, **kw)
        else:
            ins = self.nc.gpsimd.indirect_dma_start(out=out, in_=in_, **indirect, **kw)
        ent[1] += 16
        ins.then_inc(ent[0], 16)
        ev = ('d_' + key, ent[0], ent[1], 'dma')
        self._record(ev, reads, writes)

    def barrier(self):
        evs = [('s_' + e, self.esem[e], self.ecnt[e], 'x') for e in self.esem]
        evs += [('d_' + k, v[0], v[1], 'dma') for k, v in self.dsem.items()]
        for eng in self.E:
            self._wait(eng, evs)
        self.res = {}

    def finish(self):
        evs = [('s_' + e, self.esem[e], self.ecnt[e], 'x') for e in self.esem]
        evs += [('d_' + k, v[0], v[1], 'dma') for k, v in self.dsem.items()]
        self._wait('sp', evs)


_UNIQ = [0]


def _sb(es, nc, name, shape, dt):
    _UNIQ[0] += 1
    return es.enter_context(nc.sbuf_tensor('%s_%d' % (name, _UNIQ[0]), list(shape), dt))


def _ps(es, nc, name, shape, dt):
    _UNIQ[0] += 1
    return es.enter_context(nc.psum_tensor('%s_%d' % (name, _UNIQ[0]), list(shape), dt))


def _consts(em, es, nc):
    idf = _sb(es, nc, 'idf', [128, 128], F32)
    idb = _sb(es, nc, 'idb', [128, 128], BF16)
    onesb = _sb(es, nc, 'onesb', [128, 128], BF16)
    onesf = _sb(es, nc, 'onesf', [128, 128], F32)
    em.op('pool', 'memset', [], ['idf'], idf[:], 0.0)
    em.op('pool', 'affine_select', ['idf'], ['idf'], out=idf[:], in_=idf[:], pattern=[[-1, 128]],
          compare_op=ALU.not_equal, fill=1.0, base=0, channel_multiplier=1)
    em.op('dve', 'tensor_copy', ['idf'], ['idb'], out=idb[:], in_=idf[:])
    em.op('pool', 'memset', [], ['onesb'], onesb[:], 1.0)
    em.op('pool', 'memset', [], ['onesf'], onesf[:], 1.0)
    return idf, idb, onesb, onesf


def _rstd(em, nc, ss, rs, tmp, n, key_ss, key_rs, key_tmp, epsb):
    em.op('act', 'activation', [key_ss, 'epsb'], [key_tmp], out=tmp, in_=ss, func=AF.Sqrt, bias=epsb, scale=1.0 / n)
    em.op('dve', 'reciprocal', [key_tmp], [key_rs], out=rs, in_=tmp)


def _norm_transpose_tile(em, nc, cfg, t, x_ap, bufs):
    KC, D, NT = cfg.KC, cfg.D, cfg.NT
    xts, hbf, hTs, gcol, ss, rs, tmp, epsb, idb, pst = bufs
    nx = len(xts)
    if t == 0:
        em.dma('sp', 'xin0', xts[0][:], x_ap[0:128, :], [], ['xt0'])
    if nx > 1 and t + 1 < NT:
        b = (t + 1) % nx
        em.dma('sp', 'xin%d' % b, xts[b][:], x_ap[(t + 1) * 128:(t + 2) * 128, :], [], ['xt%d' % b])
    elif nx == 1 and t > 0:
        em.dma('sp', 'xin0', xts[0][:], x_ap[t * 128:(t + 1) * 128, :], [], ['xt0'])
    xk = 'xt%d' % (t % nx)
    xt = xts[t % nx]
    par = t % len(hTs)
    hT, hk = hTs[par], 'hT%d' % par
    c = par
    sk, rk, tk = 'ss%d' % c, 'rs%d' % c, 'tmp%d' % c
    em.op('act', 'activation', [xk], ['hbf', sk], out=hbf[:], in_=xt[:], func=AF.Square, accum_out=ss[:, c:c + 1])
    _rstd(em, nc, ss[:, c:c + 1], rs[:, c:c + 1], tmp[:, c:c + 1], D, sk, rk, tk, epsb[:, 0:1])
    em.op('dve', 'tensor_scalar', [xk, rk], ['hbf'], out=hbf[:], in0=xt[:], scalar1=rs[:, c:c + 1], scalar2=None, op0=ALU.mult)
    nb = KC // 8 if KC >= 8 else 1
    per = min(8, KC)
    for b in range(nb):
        pk = 'pst%d' % (b % 2)
        p = pst[b % 2]
        items = [(p[:, j, :], hbf[:, (b * per + j) * 128:(b * per + j + 1) * 128], idb[:], True) for j in range(per)]
        em.mm_multi(items, ['hbf', 'idb'], [pk])
        g_b = gcol[:, b * per:(b + 1) * per]
        em.op('dve', 'tensor_tensor', [pk, 'gcol'], [hk], out=hT[:, b * per:(b + 1) * per, :], in0=p[:, 0:per, :],
              in1=_bc_last(g_b, 128), op=ALU.mult)
    return hT, hk


def _bc_last(ap2d, n):
    t = ap2d
    return bass.AP(tensor=t.tensor, offset=t.offset, ap=list(t.ap) + [[0, n]])


def _bc_mid(ap2d, m):
    t = ap2d
    a = list(t.ap)
    return bass.AP(tensor=t.tensor, offset=t.offset, ap=[a[0], [0, m]] + a[1:])


def build_att(cfg, kind, lambda_init=0.0, stop=None, acc_mode=None, rope_pool=False, nb_diff=3):
    S, D, KC, NT = cfg.S, cfg.D, cfg.KC, cfg.NT
    gqa = kind == 'gqa'
    NP = 1 if gqa else 2
    if acc_mode is None:
        acc_mode = 'dve' if gqa else 'split'
    nc = bass.Bass("TRN2", target_bir_lowering=False)
    x = nc.dram_tensor("x", [S, D], F32, kind="ExternalInput").ap()
    gn = nc.dram_tensor("gn", [D], F32, kind="ExternalInput").ap()
    w = nc.dram_tensor("w", [NP, D, 768], F32, kind="ExternalInput").ap()
    if gqa:
        qkg = nc.dram_tensor("qkg", [5 * 128], F32, kind="ExternalInput").ap()
        rope = nc.dram_tensor("rope", [S, 128], F32, kind="ExternalInput").ap()
    else:
        lamv = nc.dram_tensor("lam", [4 * 128], F32, kind="ExternalInput").ap()
        subg = nc.dram_tensor("subg", [256], F32, kind="ExternalInput").ap()
        rb = nc.dram_tensor("rb", [64], F32, kind="ExternalInput").ap()
        bmap = nc.dram_tensor("bmap", [128, 384], F32, kind="ExternalInput").ap()
    oT = nc.dram_tensor("oT", [512, S], BF16, kind="ExternalOutput").ap()
    scale = 128 ** -0.5

    with ExitStack() as es:
        em = Em(nc, es)
        idf, idb, onesb, onesf = _consts(em, es, nc)
        epsb = _sb(es, nc, 'epsb', [128, 1], F32)
        em.op('pool', 'memset', [], ['epsb'], epsb[:], EPS)
        gcol = _sb(es, nc, 'gcol', [128, KC], F32)
        em.dma('sp', 'c0', gcol[:], gn.rearrange("(kc p) -> p kc", p=128), [], ['gcol'], allow_slow_non_contiguous=True)
        QT = _sb(es, nc, 'QT', [128, 4, S], BF16)
        if gqa:
            KT = _sb(es, nc, 'KT', [128, S], BF16)
            V = _sb(es, nc, 'V', [128, NT, 128], BF16)
        else:
            V = _sb(es, nc, 'V', [128, NT, 256], BF16)
        if gqa:
            gq = _sb(es, nc, 'gq', [128, 5, 128], F32)
            em.dma('sp', 'c1', gq[:], qkg.rearrange("(h d) -> h d", d=128).partition_broadcast(128), [], ['gq'])
        else:
            lam4 = _sb(es, nc, 'lam4', [128, 4, 128], F32)
            em.dma('sp', 'c1', lam4[:], lamv.rearrange("(h d) -> h d", d=128).partition_broadcast(128), [], ['lam4'])
            sgc = _sb(es, nc, 'sgc', [128, 2], F32)
            em.dma('sp', 'c2', sgc[:], subg.rearrange("(h p) -> p h", p=128), [], ['sgc'], allow_slow_non_contiguous=True)
            rbb = _sb(es, nc, 'rbb', [128, 64], F32)
            em.dma('sp', 'c3', rbb[:], rb.rearrange("(o n) -> o n", o=1).partition_broadcast(128), [], ['rbb'])
            bm = _sb(es, nc, 'bm', [128, 384], F32)
            em.dma('sp', 'c4', bm[:], bmap[:, :], [], ['bm'])
            lp = _sb(es, nc, 'lp', [128, 2, 128], F32)
            ls = _sb(es, nc, 'ls', [128, 4], F32)
            em.op('dve', 'tensor_tensor', ['lam4'], ['lp'], out=lp[:], in0=lam4[:, 0:4:2, :], in1=lam4[:, 1:4:2, :], op=ALU.mult)
            em.op('dve', 'tensor_reduce', ['lp'], ['ls'], out=ls[:, 0:2], in_=lp[:], axis=AX.X, op=ALU.add)
            em.op('act', 'activation', ['ls'], ['ls'], out=ls[:, 0:2], in_=ls[:, 0:2], func=AF.Exp)
            em.op('dve', 'tensor_tensor', ['ls'], ['ls'], out=ls[:, 2:3], in0=ls[:, 1:2], in1=ls[:, 0:1], op=ALU.subtract)
            em.op('dve', 'tensor_scalar', ['ls'], ['ls'], out=ls[:, 3:4], in0=ls[:, 2:3], scalar1=-float(lambda_init), scalar2=None, op0=ALU.add)
            neglam = ls[:, 3:4]
            BT = _sb(es, nc, 'BT', [128, 2, 384], F32)
            btmp = _sb(es, nc, 'btmp', [128, 384], F32)
            em.op('pool', 'memset', [], ['BT'], BT[:], 0.0)
            for h in range(2):
                for b in range(32):
                    em.op('dve', 'tensor_scalar', ['bm', 'rbb'], ['btmp'], out=btmp[:], in0=bm[:], scalar1=float(b),
                          scalar2=rbb[:, b * 2 + h:b * 2 + h + 1], op0=ALU.is_equal, op1=ALU.mult)
                    em.op('dve', 'tensor_tensor', ['btmp', 'BT'], ['BT'], out=BT[:, h, :], in0=BT[:, h, :], in1=btmp[:], op=ALU.add)

        for ps_i in range(NP):
            if stop == 'consts':
                break
            em.barrier()
            with ExitStack() as pes:
                W = _sb(pes, nc, 'W', [128, KC, 768], BF16)
                for kc in range(KC):
                    em.dma('pool', 'wl', W[:, kc, :], w[ps_i, kc * 128:(kc + 1) * 128, :], [], ['W'], new_group=(kc == 0))
                xt = [_sb(pes, nc, 'xt%d' % q_, [128, D], F32) for q_ in range(1)]
                hbf = _sb(pes, nc, 'hbf', [128, D], BF16)
                hTs = [_sb(pes, nc, 'hT%d' % q_, [128, KC, 128], BF16) for q_ in range(2)]
                ss = _sb(pes, nc, 'ss', [128, 8], F32)
                rs = _sb(pes, nc, 'rs', [128, 8], F32)
                tmp = _sb(pes, nc, 'tmp', [128, 8], F32)
                pst = [_ps(pes, nc, 'pst%d' % i, [128, 8, 128], BF16) for i in range(2)]
                ppA = [_ps(pes, nc, 'ppA%d' % i, [128, 512], F32) for i in range(2)]
                ppB = [_ps(pes, nc, 'ppB%d' % i, [128, 512], F32) for i in range(2)]
                ptr = _ps(pes, nc, 'ptr', [128, 8, 128], BF16)
                if gqa:
                    sq = [_sb(pes, nc, 'sq%d' % q_, [128, 5, 128], F32) for q_ in range(2)]
                    qn = [_sb(pes, nc, 'qn%d' % q_, [128, 5, 128], F32) for q_ in range(2)]
                    qr = [_sb(pes, nc, 'qr%d' % q_, [128, 5, 128], BF16) for q_ in range(2)]
                    rt = [_sb(pes, nc, 'rt%d' % q_, [128, 128], F32) for q_ in range(2)]
                    t1 = [_sb(pes, nc, 't1%d' % q_, [128, 5, 2, 32], F32) for q_ in range(2)]
                    t2 = [_sb(pes, nc, 't2%d' % q_, [128, 5, 2, 32], F32) for q_ in range(2)]
                    ssh = _sb(pes, nc, 'ssh', [128, 16], F32)
                    rsh = _sb(pes, nc, 'rsh', [128, 16], F32)
                    tmh = _sb(pes, nc, 'tmh', [128, 16], F32)
                else:
                    qb16 = [_sb(pes, nc, 'qb16%d' % q_, [128, 4, 128], BF16) for q_ in range(2)]
                bufs = (xt, hbf, hTs, gcol, ss, rs, tmp, epsb, idb, pst)
                def front(t):
                    c = t % 2
                    hT, hk = _norm_transpose_tile(em, nc, cfg, t, x, bufs)
                    pA, pB, kA, kB = ppA[c], ppB[c], 'ppA%d' % c, 'ppB%d' % c
                    em.mm(pA[:, :], [(hT[:, kc, :], W[:, kc, 0:512]) for kc in range(KC)], [hk, 'W'], [kA])
                    em.mm(pB[:, 0:256], [(hT[:, kc, :], W[:, kc, 512:768]) for kc in range(KC)], [hk, 'W'], [kB])

                def mid(t):
                    c = t % 2
                    pA, pB, kA, kB = ppA[c], ppB[c], 'ppA%d' % c, 'ppB%d' % c
                    if gqa:
                        sqk, qnk, qrk, rtk, t1k, t2k = 'sq%d' % c, 'qn%d' % c, 'qr%d' % c, 'rt%d' % c, 't1%d' % c, 't2%d' % c
                        sshk, rshk, tmhk = 'ssh%d' % c, 'rsh%d' % c, 'tmh%d' % c
                        o8 = c * 8
                        em.dma('sp', 'rt%d' % c, rt[c][:], rope[t * 128:(t + 1) * 128, :], [], [rtk])
                        em.op('act', 'activation', [kA], [sqk], out=sq[c][:, 0:4, :], in_=pA[:, :].rearrange("p (h d) -> p h d", d=128), func=AF.Square)
                        em.op('act', 'activation', [kB], [sqk], out=sq[c][:, 4, :], in_=pB[:, 0:128], func=AF.Square)
                        em.op('act', 'copy', [kB], [('V', t)], out=V[:, t, :], in_=pB[:, 128:256])
                        em.op('dve', 'tensor_reduce', [sqk], [sshk], out=ssh[:, o8:o8 + 5], in_=sq[c][:], axis=AX.X, op=ALU.add)
                        _rstd(em, nc, ssh[:, o8:o8 + 5], rsh[:, o8:o8 + 5], tmh[:, o8:o8 + 5], 128, sshk, rshk, tmhk, epsb[:, 0:1])
                        em.op('dve', 'tensor_tensor', [kA, rshk], [qnk], out=qn[c][:, 0:4, :], in0=pA[:, :].rearrange("p (h d) -> p h d", d=128),
                              in1=_bc_last(rsh[:, o8:o8 + 4], 128), op=ALU.mult)
                        em.op('dve', 'tensor_tensor', [kB, rshk], [qnk], out=qn[c][:, 4, :], in0=pB[:, 0:128],
                              in1=rsh[:, o8 + 4:o8 + 5].to_broadcast([128, 128]), op=ALU.mult)
                        em.op('dve', 'tensor_tensor', [qnk, 'gq'], [qnk], out=qn[c][:], in0=qn[c][:], in1=gq[:], op=ALU.mult)
                        qv = qn[c][:].rearrange("p h (hh ab i) -> p h hh ab i", hh=2, ab=2)
                        qo = qr[c][:].rearrange("p h (hh ab i) -> p h hh ab i", hh=2, ab=2)
                        A_, B_ = qv[:, :, :, 0, :], qv[:, :, :, 1, :]
                        C_ = _bc_mid(rt[c][:, 0:64], 5).rearrange("p h (hh i) -> p h hh i", hh=2)
                        S_ = _bc_mid(rt[c][:, 64:128], 5).rearrange("p h (hh i) -> p h hh i", hh=2)
                        em.op('dve', 'tensor_tensor', [qnk, rtk], [t1k], out=t1[c][:], in0=A_, in1=C_, op=ALU.mult)
                        em.op('pool' if rope_pool else 'dve', 'tensor_tensor', [qnk, rtk], [t2k], out=t2[c][:], in0=B_, in1=S_, op=ALU.mult)
                        em.op('dve', 'tensor_tensor', [t1k, t2k], [qrk], out=qo[:, :, :, 0, :], in0=t1[c][:], in1=t2[c][:], op=ALU.subtract)
                        em.op('dve', 'tensor_tensor', [qnk, rtk], [t1k], out=t1[c][:], in0=B_, in1=C_, op=ALU.mult)
                        em.op('pool' if rope_pool else 'dve', 'tensor_tensor', [qnk, rtk], [t2k], out=t2[c][:], in0=A_, in1=S_, op=ALU.mult)
                        em.op('dve', 'tensor_tensor', [t1k, t2k], [qrk], out=qo[:, :, :, 1, :], in0=t1[c][:], in1=t2[c][:], op=ALU.add)
                    else:
                        qk_ = 'qb16%d' % c
                        em.op('act', 'copy', [kA], [qk_], out=qb16[c][:], in_=pA[:, :].rearrange("p (h d) -> p h d", d=128))
                        em.op('act', 'copy', [kB], [('V', t)], out=V[:, t, :], in_=pB[:, 0:256])

                def back(t):
                    c = t % 2
                    if gqa:
                        qrk = 'qr%d' % c
                        em.mm_multi([(ptr[:, j, :], qr[c][:, j, :], idb[:], True) for j in range(5)], [qrk, 'idb'], ['ptr'])
                        em.op('act', 'copy', ['ptr'], [('QT', t)], out=QT[:, :, t * 128:(t + 1) * 128], in_=ptr[:, 0:4, :])
                        em.op('act', 'copy', ['ptr'], [('KT', t)], out=KT[:, t * 128:(t + 1) * 128], in_=ptr[:, 4, :])
                    else:
                        qk_ = 'qb16%d' % c
                        em.mm_multi([(ptr[:, j, :], qb16[c][:, j, :], idb[:], True) for j in range(4)], [qk_, 'idb'], ['ptr'])
                        em.op('act', 'copy', ['ptr'], [('QT', t)], out=QT[:, :, t * 128:(t + 1) * 128], in_=ptr[:, 0:4, :])

                front(0)
                for t in range(NT):
                    if t + 1 < NT:
                        front(t + 1)
                    mid(t)
                    back(t)
            if stop in ('tile0', 'proj', 'nt') or isinstance(stop, int):
                break
            em.barrier()
            with ExitStack() as pes:
                NB = 3 if gqa else nb_diff
                pS = [_ps(pes, nc, 'pS%d' % i, [128, 512], F32) for i in range(NB)]
                pT = [_sb(pes, nc, 'pT%d' % i, [128, 512], BF16) for i in range(NB)]
                if gqa:
                    pO = [[_ps(pes, nc, 'pO%d' % a, [128, 512], F32)] for a in range(2)]
                else:
                    pO = [[_ps(pes, nc, 'pO%d_%d' % (a, b), [128, 512], F32) for b in range(2)] for a in range(2)]
                if gqa or NB == 2:
                    pL = [_ps(pes, nc, 'pL%d' % a, [128, 512], F32) for a in range(2)]
                else:
                    pL0 = _ps(pes, nc, 'pL0', [128, 512], F32)
                    pL = [pL0, pL0]
                pla = (lambda a_: a_) if (gqa or NB == 2) else (lambda a_: 0)
                rl = _sb(pes, nc, 'rl', [128, 512], F32)
                accs = {e_: [_sb(pes, nc, 'acc%s%d' % (e_, a_), [128, 512], F32) for a_ in range(2)] for e_ in ('dve', 'pool')}
                if gqa:
                    steps = [(qb, 0, kt) for qb in range(NT) for kt in range(NT)]
                else:
                    h = ps_i
                    o1 = _sb(pes, nc, 'o1', [128, 2, 512], F32)
                    o2 = _sb(pes, nc, 'o2', [128, 2, 512], F32)
                    sq2 = _sb(pes, nc, 'sq2', [128, 2, 512], BF16)
                    sbs = [_sb(pes, nc, 'sbias%d' % q_, [128, 128], F32) for q_ in range(4)]
                    steps = [(qb, m, kt) for qb in range(S // 512) for m in range(2) for kt in range(NT)]

                def qkeys(qb):
                    return [('QT', qb)] if gqa else [('QT', j) for j in range(qb * 4, qb * 4 + 4)]

                def emit_S(s):
                    qb, m, kt = steps[s]
                    i = s % NB
                    if gqa:
                        em.mm(pS[i][:, :].rearrange("p (h q) -> p h q", h=4), [(KT[:, kt * 128:(kt + 1) * 128], QT[:, :, qb * 128:(qb + 1) * 128])],
                              [('QT', qb), ('KT', kt)], ['pS%d' % i])
                    else:
                        em.mm(pS[i][:, :], [(QT[:, 2 + m, kt * 128:(kt + 1) * 128], QT[:, m, qb * 512:(qb + 1) * 512])],
                              qkeys(qb) + [('QT', kt)], ['pS%d' % i, 'pSd%d' % i])

                def emit_exp(s):
                    qb, m, kt = steps[s]
                    i = s % NB
                    if gqa:
                        em.op('act', 'activation', ['pS%d' % i], ['pT%d' % i], out=pT[i][:], in_=pS[i][:, :], func=AF.Exp, scale=scale)
                        return
                    cls = []
                    for j in range(4):
                        dlt = kt - (qb * 4 + j)
                        cls.append('lo' if dlt < -1 else ('hi' if dlt > 1 else dlt))
                    near = [j for j in range(4) if cls[j] not in ('lo', 'hi')]
                    for ni, j in enumerate(near):
                        c = cls[j]
                        em.op('dve', 'scalar_tensor_tensor', ['pS%d' % i, 'BT'], ['sbias%d' % ni, 'pSd%d' % i], out=sbs[ni][:], in0=pS[i][:, j * 128:(j + 1) * 128],
                              scalar=scale, in1=BT[:, h, (c + 1) * 128:(c + 2) * 128], op0=ALU.mult, op1=ALU.add)
                    for ni, j in enumerate(near):
                        em.op('act', 'activation', ['sbias%d' % ni], ['pT%d' % i], out=pT[i][:, j * 128:(j + 1) * 128], in_=sbs[ni][:], func=AF.Exp)
                    j = 0
                    while j < 4:
                        c = cls[j]
                        if c in ('lo', 'hi'):
                            j2 = j
                            while j2 < 4 and cls[j2] == c:
                                j2 += 1
                            bcol = (15 if c == 'lo' else 31) * 2 + h
                            em.op('act', 'activation', ['pS%d' % i, 'pSd%d' % i, 'rbb'], ['pT%d' % i], out=pT[i][:, j * 128:j2 * 128], in_=pS[i][:, j * 128:j2 * 128],
                                  func=AF.Exp, scale=scale, bias=rbb[:, bcol:bcol + 1])
                            j = j2
                        else:
                            j += 1

                def emit_PV(s):
                    qb, m, kt = steps[s]
                    i = s % NB
                    a = (qb * (1 if gqa else 2) + m) % 2
                    first, last = kt == 0, kt == NT - 1
                    okeys = ['pO%d_%d' % (a, b) for b in range(len(pO[a]))]
                    rd = ['pT%d' % i, ('V', kt)]
                    em._deps('pe', rd, okeys if first else [])
                    if gqa:
                        ins = nc.tensor.matmul(pO[a][0][:, :], V[:, kt, :], pT[i][:], start=first, stop=last)
                    else:
                        nc.tensor.matmul(pO[a][0][:, :], V[:, kt, 0:128], pT[i][:], start=first, stop=last)
                        ins = nc.tensor.matmul(pO[a][1][:, :], V[:, kt, 128:256], pT[i][:], start=first, stop=last)
                    em.ecnt['pe'] += 1
                    ins.then_inc(em.esem['pe'], 1)
                    ev = ('s_pe', em.esem['pe'], em.ecnt['pe'], 'pe')
                    em._record(ev, rd, okeys if (first or last) else [])
                    eng = 'dve' if (kt % 2 == 0 or acc_mode == 'dve') else 'pool'
                    ak = 'acc%s%d' % (eng, a)
                    ac = accs[eng][a]
                    if kt < (1 if acc_mode == 'dve' else 2):
                        em.op(eng, 'tensor_copy', ['pT%d' % i], [ak], out=ac[:], in_=pT[i][:])
                    else:
                        em.op(eng, 'tensor_tensor', ['pT%d' % i, ak], [ak], out=ac[:], in0=ac[:], in1=pT[i][:], op=ALU.add)
                    if not last:
                        return
                    if acc_mode == 'dve':
                        em.mm(pL[a][:, :], [(onesf[:], accs['dve'][a][:])], ['accdve%d' % a, 'onesf'], ['pL%d' % pla(a)])
                    else:
                        em.mm(pL[a][:, :], [(onesf[:], accs['dve'][a][:]), (onesf[:], accs['pool'][a][:])], ['accdve%d' % a, 'accpool%d' % a, 'onesf'], ['pL%d' % pla(a)])
                    okeys = okeys + ['pL%d' % pla(a)]
                    em.op('dve', 'reciprocal', ['pL%d' % pla(a)], ['rl'], out=rl[:], in_=pL[a][:, :])
                    if gqa:
                        em.op('dve', 'tensor_tensor', [okeys[0], 'rl'], [('QT', qb)], out=QT[:, :, qb * 128:(qb + 1) * 128],
                              in0=pO[a][0][:, :].rearrange("p (h q) -> p h q", h=4), in1=rl[:].rearrange("p (h q) -> p h q", h=4), op=ALU.mult)
                        return
                    om, ok = (o1, 'o1') if m == 0 else (o2, 'o2')
                    for b in range(2):
                        em.op('dve', 'tensor_tensor', [okeys[b], 'rl'], [ok], out=om[:, b, :], in0=pO[a][b][:, :], in1=rl[:], op=ALU.mult)
                    if m == 0:
                        return
                    em.op('dve', 'scalar_tensor_tensor', ['o1', 'o2', 'ls'], ['o1'], out=o1[:], in0=o2[:], scalar=neglam, in1=o1[:], op0=ALU.mult, op1=ALU.add)
                    em.op('act', 'activation', ['o1'], ['sq2'], out=sq2[:], in_=o1[:], func=AF.Square)
                    em.mm(pL[a][:, :], [(onesb[:], sq2[:, 0, :]), (onesb[:], sq2[:, 1, :])], ['sq2', 'onesb'], ['pL%d' % pla(a)])
                    em.op('act', 'activation', ['pL%d' % pla(a), 'epsb'], ['rl'], out=rl[:], in_=pL[a][:, :], func=AF.Sqrt, bias=epsb[:, 0:1], scale=1.0 / 256)
                    em.op('dve', 'reciprocal', ['rl'], ['rl'], out=rl[:], in_=rl[:])
                    for hf in range(2):
                        em.op('dve', 'scalar_tensor_tensor', ['o1', 'rl', 'sgc'], ['o2'], out=o2[:, hf, :], in0=o1[:, hf, :], scalar=sgc[:, hf:hf + 1], in1=rl[:],
                              op0=ALU.mult, op1=ALU.mult)
                    em.op('dve', 'tensor_scalar', ['o2'], qkeys(qb), out=QT[:, 0:2, qb * 512:(qb + 1) * 512], in0=o2[:],
                          scalar1=float(1.0 - lambda_init), scalar2=None, op0=ALU.mult)

                emit_S(0)
                for s in range(len(steps)):
                    if s + 1 < len(steps):
                        emit_S(s + 1)
                    emit_exp(s)
                    emit_PV(s)
                if gqa:
                    em.dma('sp', 'oout', oT.rearrange("(h d) s -> d h s", d=128), QT[:], [('QT', qb) for qb in range(NT)], [])
                else:
                    em.dma('sp', 'oout', oT[h * 256:(h + 1) * 256, :].rearrange("(c d) s -> d c s", d=128), QT[:, 0:2, :], [('QT', j) for j in range(NT)], [])
        em.finish()
    return nc


def rope_table(cfg):
    S = cfg.S
    pos = np.arange(S)
    row = (pos // cfg.GRID_W).astype(np.float32)
    col = (pos % cfg.GRID_W).astype(np.float32)
    half = 64
    freq = (10000.0 ** (-np.arange(0, half, 2, dtype=np.float32) / half)).astype(np.float32)
    ar = row[:, None] * freq[None, :]
    ac = col[:, None] * freq[None, :]
    return np.ascontiguousarray(np.concatenate([np.cos(ar), np.cos(ac), np.sin(ar), np.sin(ac)], axis=1).astype(np.float32))


def bucket_map():
    nb, max_exact, max_distance = 16, 8, 128
    k = np.arange(128)[:, None]
    q = np.arange(128)[None, :]
    out = np.zeros((128, 384), np.float32)
    for d in (-1, 0, 1):
        rel = k - q + 128 * d
        n = np.abs(rel)
        large = max_exact + (np.log(np.maximum(n, 1).astype(np.float32) / max_exact) / math.log(max_distance / max_exact) * (nb - max_exact)).astype(np.int32)
        large = np.minimum(large, nb - 1)
        b = np.where(rel > 0, nb, 0) + np.where(n < max_exact, n, large)
        out[:, (d + 1) * 128:(d + 2) * 128] = b
    return out


def att_inputs(cfg, kind, x, gn, w_in, extra):
    NC = cfg.NC
    maps = []
    x = np.ascontiguousarray(x, dtype=np.float32)
    for c in range(NC):
        m = {"x": x, "gn": np.ascontiguousarray(gn, dtype=np.float32)}
        if kind == 'gqa':
            qd = cfg.QH * 128
            kd = cfg.KVH * 128
            cols = np.concatenate([np.arange(512 * c, 512 * (c + 1)), qd + np.arange(128 * c, 128 * (c + 1)),
                                   qd + kd + np.arange(128 * c, 128 * (c + 1))])
            m["w"] = np.ascontiguousarray(w_in[:, cols][None])
            m["qkg"] = np.ascontiguousarray(np.concatenate([np.tile(extra['qg'], 4), extra['kg']]).astype(np.float32))
            m["rope"] = rope_table(cfg)
        else:
            qk = cfg.DH * 256
            ws = []
            for hh in (2 * c, 2 * c + 1):
                cols = np.concatenate([hh * 256 + np.arange(256), qk + hh * 256 + np.arange(256), 2 * qk + hh * 256 + np.arange(256)])
                ws.append(w_in[:, cols])
            m["w"] = np.ascontiguousarray(np.stack(ws))
            m["lam"] = np.ascontiguousarray(np.concatenate([extra['lq1'], extra['lk1'], extra['lq2'], extra['lk2']]).astype(np.float32))
            m["subg"] = np.ascontiguousarray(extra['subg'], dtype=np.float32)
            m["rb"] = np.ascontiguousarray(extra['rel_bias'][:, 2 * c:2 * c + 2].reshape(-1), dtype=np.float32)
            m["bmap"] = bucket_map()
        maps.append(m)
    return maps


def build_pout(cfg):
    S, D, KC, NT = cfg.S, cfg.D, cfg.KC, cfg.NT
    nc = bass.Bass("TRN2", target_bir_lowering=False)
    oTf = nc.dram_tensor("oTf", [D, S], BF16, kind="ExternalInput").ap()
    wo = nc.dram_tensor("wo", [D, 512], F32, kind="ExternalInput").ap()
    xc = nc.dram_tensor("xc", [S, 512], F32, kind="ExternalInput").ap()
    xo = nc.dram_tensor("xo", [S, 512], F32, kind="ExternalOutput").ap()
    with ExitStack() as es:
        em = Em(nc, es)
        W = _sb(es, nc, 'W', [128, KC, 512], BF16)
        for kc in range(KC):
            em.dma('pool', 'wl', W[:, kc, :], wo[kc * 128:(kc + 1) * 128, :], [], ['W'], new_group=(kc == 0))
        ob = [_sb(es, nc, 'ob%d' % i, [128, KC, 512], BF16) for i in range(2)]
        xb = [_sb(es, nc, 'xb%d' % i, [128, 512], F32) for i in range(2)]
        yb = [_sb(es, nc, 'yb%d' % i, [128, 512], F32) for i in range(2)]
        pp = [_ps(es, nc, 'pp%d' % i, [128, 512], F32) for i in range(2)]
        oview = oTf.rearrange("(kc p) s -> p kc s", p=128)
        n = 0
        for tb in range(S // 512):
            bi = tb % 2
            em.dma('sp', 'ob%d' % bi, ob[bi][:], oview[:, :, tb * 512:(tb + 1) * 512], [], ['ob%d' % bi])
            for j in range(4):
                t = tb * 4 + j
                i = n % 2
                n += 1
                em.dma('sp', 'xb%d' % i, xb[i][:], xc[t * 128:(t + 1) * 128, :], [], ['xb%d' % i])
                em.mm(pp[i][:, :], [(ob[bi][:, kc, j * 128:(j + 1) * 128], W[:, kc, :]) for kc in range(KC)], ['ob%d' % bi, 'W'], ['pp%d' % i])
                em.op('dve', 'tensor_tensor', ['pp%d' % i, 'xb%d' % i], ['yb%d' % i], out=yb[i][:], in0=pp[i][:, :], in1=xb[i][:], op=ALU.add)
                em.dma('sp', 'yo%d' % i, xo[t * 128:(t + 1) * 128, :], yb[i][:], ['yb%d' % i], [])
        em.finish()
    return nc


def build_pout2(cfg):
    S, D, KC, E = cfg.S, cfg.D, cfg.KC, cfg.E
    TL = S // cfg.NC
    TT = TL // 128
    nc = bass.Bass("TRN2", target_bir_lowering=False)
    oTs = nc.dram_tensor("oTs", [D, TL], BF16, kind="ExternalInput").ap()
    wo = nc.dram_tensor("wo", [D, D], F32, kind="ExternalInput").ap()
    xs = nc.dram_tensor("xs", [TL, D], F32, kind="ExternalInput").ap()
    gn = nc.dram_tensor("gn", [D], F32, kind="ExternalInput").ap()
    rw = nc.dram_tensor("rw", [D, E], F32, kind="ExternalInput").ap()
    xm = nc.dram_tensor("xm", [TL, D], F32, kind="ExternalOutput").ap()
    hbo = nc.dram_tensor("hb", [TL, D], BF16, kind="ExternalOutput").ap()
    affo = nc.dram_tensor("aff", [TL, E], F32, kind="ExternalOutput").ap()
    with ExitStack() as es:
        em = Em(nc, es)
        idf, idb, onesb, onesf = _consts(em, es, nc)
        epsb = _sb(es, nc, 'epsb', [128, 1], F32)
        em.op('pool', 'memset', [], ['epsb'], epsb[:], EPS)
        with ExitStack() as pes:
            oT = _sb(pes, nc, 'oT', [128, KC, TL], BF16)
            em.dma('sp', 'ol', oT[:], oTs.rearrange("(kc p) s -> p kc s", p=128), [], ['oT'])
            Wc = [_sb(pes, nc, 'Wc%d' % i, [128, KC, 512], BF16) for i in range(2)]
            xb = [_sb(pes, nc, 'xb%d' % i, [128, 512], F32) for i in range(2)]
            yb = [_sb(pes, nc, 'yb%d' % i, [128, 512], F32) for i in range(2)]
            pp = [_ps(pes, nc, 'pp%d' % i, [128, 512], F32) for i in range(2)]

            def loadW(cg):
                wi = cg % 2
                for kc in range(KC):
                    em.dma('pool', 'wl%d' % wi, Wc[wi][:, kc, :], wo[kc * 128:(kc + 1) * 128, cg * 512:(cg + 1) * 512], [], ['Wc%d' % wi], new_group=(kc == 0))
            loadW(0)
            n = 0
            for cg in range(D // 512):
                if cg + 1 < D // 512:
                    loadW(cg + 1)
                wi = cg % 2
                for tt in range(TT):
                    i = n % 2
                    n += 1
                    em.dma('sp', 'xb%d' % i, xb[i][:], xs[tt * 128:(tt + 1) * 128, cg * 512:(cg + 1) * 512], [], ['xb%d' % i])
                    em.mm(pp[i][:, :], [(oT[:, kc, tt * 128:(tt + 1) * 128], Wc[wi][:, kc, :]) for kc in range(KC)], ['oT', 'Wc%d' % wi], ['pp%d' % i])
                    em.op('dve', 'tensor_tensor', ['pp%d' % i, 'xb%d' % i], ['yb%d' % i], out=yb[i][:], in0=pp[i][:, :], in1=xb[i][:], op=ALU.add)
                    em.dma('sp', 'yo%d' % i, xm[tt * 128:(tt + 1) * 128, cg * 512:(cg + 1) * 512], yb[i][:], ['yb%d' % i], [])
        em.barrier()
        with ExitStack() as pes:
            gb = _sb(pes, nc, 'gb', [128, D], F32)
            em.dma('sp', 'c0', gb[:], gn.rearrange("(o n) -> o n", o=1).partition_broadcast(128), [], ['gb'])
            rws = _sb(pes, nc, 'rws', [128, KC, E], F32)
            em.dma('sp', 'c1', rws[:], rw.rearrange("(kc p) e -> p kc e", p=128), [], ['rws'])
            xt = [_sb(pes, nc, 'xt%d' % i, [128, D], F32) for i in range(2)]
            h32 = _sb(pes, nc, 'h32', [128, D], F32)
            hb = [_sb(pes, nc, 'hb%d' % i, [128, D], BF16) for i in range(2)]
            hT32 = _sb(pes, nc, 'hT32', [128, KC, 128], F32)
            ss = _sb(pes, nc, 'ss', [128, 8], F32)
            rs = _sb(pes, nc, 'rs', [128, 8], F32)
            tmp = _sb(pes, nc, 'tmp', [128, 8], F32)
            ex = _sb(pes, nc, 'ex', [128, E], F32)
            af = [_sb(pes, nc, 'af%d' % i, [128, E], F32) for i in range(2)]
            ptf = [_ps(pes, nc, 'ptf%d' % i, [128, 4, 128], F32) for i in range(2)]
            pl = _ps(pes, nc, 'pl', [128, 512], F32)
            em.dma('sp', 'xin0', xt[0][:], xm[0:128, :], [], ['xt0'])
            for t in range(TT):
                i = t % 2
                if t + 1 < TT:
                    em.dma('sp', 'xin%d' % (1 - i), xt[1 - i][:], xm[(t + 1) * 128:(t + 2) * 128, :], [], ['xt%d' % (1 - i)])
                xk = 'xt%d' % i
                em.op('act', 'activation', [xk], ['h32', 'ss'], out=h32[:], in_=xt[i][:], func=AF.Square, accum_out=ss[:, 0:1])
                _rstd(em, nc, ss[:, 0:1], rs[:, 0:1], tmp[:, 0:1], D, 'ss', 'rs', 'tmp', epsb[:, 0:1])
                em.op('dve', 'scalar_tensor_tensor', [xk, 'rs', 'gb'], ['h32'], out=h32[:], in0=xt[i][:], scalar=rs[:, 0:1], in1=gb[:], op0=ALU.mult, op1=ALU.mult)
                em.op('act', 'copy', ['h32'], ['hb%d' % i], out=hb[i][:], in_=h32[:])
                em.dma('sp', 'hout%d' % i, hbo[t * 128:(t + 1) * 128, :], hb[i][:], ['hb%d' % i], [])
                for b in range(KC // 4):
                    pk = 'ptf%d' % (b % 2)
                    p = ptf[b % 2]
                    em.mm_multi([(p[:, j, :], h32[:, (b * 4 + j) * 128:(b * 4 + j + 1) * 128], idf[:], True) for j in range(4)], ['h32', 'idf'], [pk])
                    em.op('act', 'copy', [pk], ['hT32'], out=hT32[:, b * 4:(b + 1) * 4, :], in_=p[:, :, :])
                em.mm(pl[:, 0:E], [(hT32[:, kc, :], rws[:, kc, :]) for kc in range(KC)], ['hT32', 'rws'], ['pl'])
                em.op('dve', 'tensor_reduce', ['pl'], ['ss'], out=ss[:, 1:2], in_=pl[:, 0:E], axis=AX.X, op=ALU.max)
                em.op('dve', 'tensor_scalar', ['ss'], ['ss'], out=ss[:, 2:3], in0=ss[:, 1:2], scalar1=-1.0, scalar2=None, op0=ALU.mult)
                em.op('act', 'activation', ['pl', 'ss'], ['ex', 'ss'], out=ex[:], in_=pl[:, 0:E], func=AF.Exp, bias=ss[:, 2:3], accum_out=ss[:, 3:4])
                em.op('dve', 'reciprocal', ['ss'], ['ss'], out=ss[:, 4:5], in_=ss[:, 3:4])
                em.op('dve', 'tensor_scalar', ['ex', 'ss'], ['af%d' % i], out=af[i][:], in0=ex[:], scalar1=ss[:, 4:5], scalar2=None, op0=ALU.mult)
                em.dma('sp', 'aout%d' % i, affo[t * 128:(t + 1) * 128, :], af[i][:], ['af%d' % i], [])
        em.finish()
    return nc


def build_pfin(cfg):
    S, D = cfg.S, cfg.D
    TL = S // cfg.NC
    nc = bass.Bass("TRN2", target_bir_lowering=False)
    x = nc.dram_tensor("x", [TL, D], F32, kind="ExternalInput").ap()
    g = nc.dram_tensor("g", [D], F32, kind="ExternalInput").ap()
    y = nc.dram_tensor("y", [TL, D], F32, kind="ExternalOutput").ap()
    with ExitStack() as es:
        em = Em(nc, es)
        epsb = _sb(es, nc, 'epsb', [128, 1], F32)
        em.op('pool', 'memset', [], ['epsb'], epsb[:], EPS)
        gb = _sb(es, nc, 'gb', [128, D], F32)
        em.dma('sp', 'c0', gb[:], g.rearrange("(o n) -> o n", o=1).partition_broadcast(128), [], ['gb'])
        xt = [_sb(es, nc, 'xt%d' % i, [128, D], F32) for i in range(2)]
        yt = [_sb(es, nc, 'yt%d' % i, [128, D], F32) for i in range(2)]
        ss = _sb(es, nc, 'ss', [128, 2], F32)
        rs = _sb(es, nc, 'rs', [128, 2], F32)
        tmp = _sb(es, nc, 'tmp', [128, 2], F32)
        for t in range(TL // 128):
            i = t % 2
            em.dma('sp', 'x%d' % i, xt[i][:], x[t * 128:(t + 1) * 128, :], [], ['xt%d' % i])
            em.op('act', 'activation', ['xt%d' % i], ['yt%d' % i, 'ss%d' % i], out=yt[i][:], in_=xt[i][:], func=AF.Square, accum_out=ss[:, i:i + 1])
            _rstd(em, nc, ss[:, i:i + 1], rs[:, i:i + 1], tmp[:, i:i + 1], D, 'ss%d' % i, 'rs%d' % i, 'tmp%d' % i, epsb[:, 0:1])
            em.op('dve', 'scalar_tensor_tensor', ['xt%d' % i, 'rs%d' % i, 'gb'], ['yt%d' % i], out=yt[i][:], in0=xt[i][:], scalar=rs[:, i:i + 1], in1=gb[:],
                  op0=ALU.mult, op1=ALU.mult)
            em.dma('sp', 'y%d' % i, y[t * 128:(t + 1) * 128, :], yt[i][:], ['yt%d' % i], [])
        em.finish()
    return nc


def build_moe(cfg, NIT=30, ext=True):
    S, D, KC, NT, E, FF, FC, CAP, CT = cfg.S, cfg.D, cfg.KC, cfg.NT, cfg.E, cfg.FF, cfg.FC, cfg.CAP, cfg.CT
    J = S // 128
    nc = bass.Bass("TRN2", target_bir_lowering=False)
    if ext:
        hs = nc.dram_tensor("hs", [S, D], BF16, kind="ExternalInput").ap()
        Ain = nc.dram_tensor("A", [128, 2, J], F32, kind="ExternalInput").ap()
    else:
        x = nc.dram_tensor("x", [S, D], F32, kind="ExternalInput").ap()
        gn = nc.dram_tensor("gn", [D], F32, kind="ExternalInput").ap()
        rw = nc.dram_tensor("rw", [D, E], F32, kind="ExternalInput").ap()
    wg = nc.dram_tensor("wg", [2, D, FF], F32, kind="ExternalInput").ap()
    wu = nc.dram_tensor("wu", [2, D, FF], F32, kind="ExternalInput").ap()
    wd = nc.dram_tensor("wd", [2, FF, D], F32, kind="ExternalInput").ap()
    y = nc.dram_tensor("y", [2, CAP, D], F32, kind="ExternalOutput").ap()
    idxo = nc.dram_tensor("idx", [2, 128, CT], I32, kind="ExternalOutput").ap()
    if not ext:
        hs = nc.dram_tensor("hs_scr", [S, D], BF16).ap()
        affd = nc.dram_tensor("aff_scr", [2, S], F32).ap()
    with ExitStack() as es:
        em = Em(nc, es)
        idf, idb, onesb, onesf = _consts(em, es, nc)
        epsb = _sb(es, nc, 'epsb', [128, 1], F32)
        em.op('pool', 'memset', [], ['epsb'], epsb[:], EPS)
        A = _sb(es, nc, 'A', [128, 2, J], F32)
        idxi = _sb(es, nc, 'idxi', [128, 2, CT], I32)
        gate = _sb(es, nc, 'gate', [128, 2, CT], F32)
        if ext:
            em.dma('sp', 'affi', A[:], Ain[:, :, :], [], ['A'])
        if not ext:
            with ExitStack() as pes:
                gb = _sb(pes, nc, 'gb', [128, D], F32)
                em.dma('sp', 'c0', gb[:], gn.rearrange("(o n) -> o n", o=1).partition_broadcast(128), [], ['gb'])
                rws = _sb(pes, nc, 'rws', [128, KC, E], F32)
                em.dma('sp', 'c1', rws[:], rw.rearrange("(kc p) e -> p kc e", p=128), [], ['rws'])
                xt = _sb(pes, nc, 'xt', [128, D], F32)
                h32 = _sb(pes, nc, 'h32', [128, D], F32)
                hb = _sb(pes, nc, 'hb', [128, D], BF16)
                hT32 = _sb(pes, nc, 'hT32', [128, KC, 128], F32)
                ss = _sb(pes, nc, 'ss', [128, 8], F32)
                rs = _sb(pes, nc, 'rs', [128, 8], F32)
                tmp = _sb(pes, nc, 'tmp', [128, 8], F32)
                ex = _sb(pes, nc, 'ex', [128, E], F32)
                aff2 = _sb(pes, nc, 'aff2', [128, 2], F32)
                affT = _sb(pes, nc, 'affT', [2, S], F32)
                ptf = [_ps(pes, nc, 'ptf%d' % i, [128, 4, 128], F32) for i in range(2)]
                pl = _ps(pes, nc, 'pl', [128, 512], F32)
                pa = _ps(pes, nc, 'pa', [128, 512], F32)
                for t in range(NT):
                    em.dma('sp', 'xin', xt[:], x[t * 128:(t + 1) * 128, :], [], ['xt'])
                    em.op('act', 'activation', ['xt'], ['h32', 'ss'], out=h32[:], in_=xt[:], func=AF.Square, accum_out=ss[:, 0:1])
                    _rstd(em, nc, ss[:, 0:1], rs[:, 0:1], tmp[:, 0:1], D, 'ss', 'rs', 'tmp', epsb[:, 0:1])
                    em.op('dve', 'scalar_tensor_tensor', ['xt', 'rs', 'gb'], ['h32'], out=h32[:], in0=xt[:], scalar=rs[:, 0:1], in1=gb[:], op0=ALU.mult, op1=ALU.mult)
                    em.op('act', 'copy', ['h32'], ['hb'], out=hb[:], in_=h32[:])
                    em.dma('sp', 'hout', hs[t * 128:(t + 1) * 128, :], hb[:], ['hb'], ['hs'])
                    for b in range(KC // 4):
                        pk = 'ptf%d' % (b % 2)
                        p = ptf[b % 2]
                        em.mm_multi([(p[:, j, :], h32[:, (b * 4 + j) * 128:(b * 4 + j + 1) * 128], idf[:], True) for j in range(4)], ['h32', 'idf'], [pk])
                        em.op('act', 'copy', [pk], ['hT32'], out=hT32[:, b * 4:(b + 1) * 4, :], in_=p[:, :, :])
                    em.mm(pl[:, 0:E], [(hT32[:, kc, :], rws[:, kc, :]) for kc in range(KC)], ['hT32', 'rws'], ['pl'])
                    em.op('dve', 'tensor_reduce', ['pl'], ['ss'], out=ss[:, 1:2], in_=pl[:, 0:E], axis=AX.X, op=ALU.max)
                    em.op('dve', 'tensor_scalar', ['ss'], ['ss'], out=ss[:, 2:3], in0=ss[:, 1:2], scalar1=-1.0, scalar2=None, op0=ALU.mult)
                    em.op('act', 'activation', ['pl', 'ss'], ['ex', 'ss'], out=ex[:], in_=pl[:, 0:E], func=AF.Exp, bias=ss[:, 2:3], accum_out=ss[:, 3:4])
                    em.op('dve', 'reciprocal', ['ss'], ['ss'], out=ss[:, 4:5], in_=ss[:, 3:4])
                    em.op('dve', 'tensor_scalar', ['ex', 'ss'], ['aff2'], out=aff2[:], in0=ex[:, 0:2], scalar1=ss[:, 4:5], scalar2=None, op0=ALU.mult)
                    em.mm_multi([(pa[0:2, 0:128], aff2[:, 0:2], idf[:], True)], ['aff2', 'idf'], ['pa'])
                    em.op('act', 'copy', ['pa'], ['affT'], out=affT[:, t * 128:(t + 1) * 128], in_=pa[0:2, 0:128])
                em.dma('sp', 'affo', affd[:, :], affT[:], ['affT'], ['affd'])
                em.dma('sp', 'affi', A[:], affd.rearrange("e (p j) -> p e j", j=J), ['affd'], ['A'])
        em.barrier()
        with ExitStack() as pes:
            lo = _sb(pes, nc, 'lo', [128, 2], F32)
            hi = _sb(pes, nc, 'hi', [128, 2], F32)
            mid = _sb(pes, nc, 'mid', [128, 2], F32)
            cnt = _sb(pes, nc, 'cnt', [128, 2], F32)
            gem = _sb(pes, nc, 'gem', [128, 2], U32)
            ltm = _sb(pes, nc, 'ltm', [128, 2], U32)
            cmpb = _sb(pes, nc, 'cmpb', [128, 2, J], F32)
            cs = _sb(pes, nc, 'cs', [128, 2, J], F32)
            pos = _sb(pes, nc, 'pos', [128, 2, J], F32)
            onesJ = _sb(pes, nc, 'onesJ', [128, J], F32)
            off = _sb(pes, nc, 'off', [128, 2], F32)
            totp = _sb(pes, nc, 'totp', [128, 2], F32)
            U = _sb(pes, nc, 'U', [128, 128], F32)
            iot = _sb(pes, nc, 'iot', [128, CAP], F32)
            rhsv = _sb(pes, nc, 'rhsv', [128, 2, J, 2], F32)
            OH = [_sb(pes, nc, 'OH%d' % i, [128, CAP], F32) for i in range(2)]
            res4 = _sb(pes, nc, 'res4', [128, CT, 2], F32)
            pc = _ps(pes, nc, 'pc', [128, 512], F32)
            pacc = _ps(pes, nc, 'pacc', [128, CT, 512], F32) if CT <= 7 else None
            if pacc is None:
                pacc = _ps(pes, nc, 'pacc', [128, 7, 512], F32)
            em.op('pool', 'memset', [], ['lo'], lo[:], 0.0)
            em.op('pool', 'memset', [], ['hi'], hi[:], 1.0)
            em.op('pool', 'memset', [], ['onesJ'], onesJ[:], 1.0)
            em.op('pool', 'memset', [], ['U'], U[:], 1.0)
            em.op('pool', 'affine_select', ['U'], ['U'], out=U[:], in_=U[:], pattern=[[1, 128]], compare_op=ALU.is_gt, fill=0.0, base=0, channel_multiplier=-1)
            em.op('pool', 'iota', [], ['iot'], iot[:], pattern=[[1, CAP]], base=0, channel_multiplier=0, allow_small_or_imprecise_dtypes=True)
            for e in range(2):
                em.op('pool', 'iota', [], ['rhsv'], rhsv[:, e, :, 0], pattern=[[1, J]], base=0, channel_multiplier=J, allow_small_or_imprecise_dtypes=True)
                em.op('dve', 'tensor_copy', ['A', 'rhsv'], ['rhsv'], out=rhsv[:, e, :, 1], in_=A[:, e, :])
            for it in range(NIT):
                em.op('dve', 'tensor_tensor', ['lo', 'hi'], ['mid'], out=mid[:], in0=lo[:], in1=hi[:], op=ALU.add)
                em.op('dve', 'tensor_scalar', ['mid'], ['mid'], out=mid[:], in0=mid[:], scalar1=0.5, scalar2=None, op0=ALU.mult)
                for e in range(2):
                    em.op('dve', 'tensor_scalar', ['A', 'mid'], ['cmpb', 'cnt'], out=cmpb[:, e, :], in0=A[:, e, :], scalar1=mid[:, e:e + 1], scalar2=0.0,
                          op0=ALU.is_gt, op1=ALU.add, accum_out=cnt[:, e:e + 1])
                em.mm(pc[:, 0:2], [(onesf[:], cnt[:])], ['cnt', 'onesf'], ['pc'])
                em.op('dve', 'tensor_scalar', ['pc'], ['gem'], out=gem[:], in0=pc[:, 0:2], scalar1=float(CAP), scalar2=None, op0=ALU.is_ge)
                em.op('dve', 'tensor_scalar', ['pc'], ['ltm'], out=ltm[:], in0=pc[:, 0:2], scalar1=float(CAP), scalar2=None, op0=ALU.is_lt)
                em.op('dve', 'copy_predicated', ['gem', 'mid'], ['lo'], out=lo[:], mask=gem[:], data=mid[:])
                em.op('dve', 'copy_predicated', ['ltm', 'mid'], ['hi'], out=hi[:], mask=ltm[:], data=mid[:])
            for e in range(2):
                em.op('dve', 'tensor_scalar', ['A', 'lo'], ['cmpb'], out=cmpb[:, e, :], in0=A[:, e, :], scalar1=lo[:, e:e + 1], scalar2=None, op0=ALU.is_gt)
                em.op('dve', 'tensor_tensor_scan', ['cmpb', 'onesJ'], ['cs'], out=cs[:, e, :], data0=onesJ[:], data1=cmpb[:, e, :], initial=0.0, op0=ALU.mult, op1=ALU.add)
                em.op('dve', 'tensor_copy', ['cs'], ['totp'], out=totp[:, e:e + 1], in_=cs[:, e, J - 1:J])
            em.mm(pc[:, 0:2], [(U[:], totp[:])], ['totp', 'U'], ['pc'])
            em.op('act', 'copy', ['pc'], ['off'], out=off[:], in_=pc[:, 0:2])
            for e in range(2):
                em.op('dve', 'tensor_scalar', ['cs', 'off'], ['pos'], out=pos[:, e, :], in0=cs[:, e, :], scalar1=off[:, e:e + 1], scalar2=None, op0=ALU.add)
                em.op('dve', 'tensor_tensor', ['pos', 'cmpb'], ['pos'], out=pos[:, e, :], in0=pos[:, e, :], in1=cmpb[:, e, :], op=ALU.mult)
                em.op('dve', 'tensor_scalar', ['pos'], ['pos'], out=pos[:, e, :], in0=pos[:, e, :], scalar1=-1.0, scalar2=None, op0=ALU.add)
            for e in range(2):
                for c0 in range(0, CT, 7):
                    cts = list(range(c0, min(CT, c0 + 7)))
                    for j in range(J):
                        i = j % 2
                        em.op('dve', 'tensor_scalar', ['iot', 'pos'], ['OH%d' % i], out=OH[i][:], in0=iot[:], scalar1=pos[:, e, j:j + 1], scalar2=None, op0=ALU.is_equal)
                        first, last = j == 0, j == J - 1
                        em._deps('pe', ['OH%d' % i, 'rhsv'], ['pacc'] if first else [])
                        ins = None
                        for ct in cts:
                            ins = nc.tensor.matmul(pacc[:, ct - c0, 0:2], OH[i][:, ct * 128:(ct + 1) * 128], rhsv[:, e, j, :], start=first, stop=last)
                        em.ecnt['pe'] += 1
                        ins.then_inc(em.esem['pe'], 1)
                        ev = ('s_pe', em.esem['pe'], em.ecnt['pe'], 'pe')
                        em._record(ev, ['OH%d' % i, 'rhsv'], ['pacc'] if (first or last) else [])
                    em.op('act', 'copy', ['pacc'], ['res4'], out=res4[:, c0:c0 + len(cts), :], in_=pacc[:, 0:len(cts), 0:2])
                em.op('dve', 'tensor_copy', ['res4'], ['idxi'], out=idxi[:, e, :], in_=res4[:, :, 0])
                em.op('dve', 'tensor_copy', ['res4'], ['gate'], out=gate[:, e, :], in_=res4[:, :, 1])
            em.dma('sp', 'idxo', idxo.rearrange("e p c -> p e c"), idxi[:], ['idxi'], [])
        em.barrier()
        xinT = _sb(es, nc, 'xinT', [128, KC, CAP], BF16)
        actT = _sb(es, nc, 'actT', [128, FC, CAP], BF16)
        for e in range(2):
            with ExitStack() as pes:
                xg = [_sb(pes, nc, 'xg%d' % i, [128, D], BF16) for i in range(2)]
                Wgh = _sb(pes, nc, 'Wgh', [128, KC, 512], BF16)
                Wuh = _sb(pes, nc, 'Wuh', [128, KC, 512], BF16)
                sg = _sb(pes, nc, 'sg', [128, 512], F32)
                ptr = [_ps(pes, nc, 'ptr%d' % i, [128, 8, 128], BF16) for i in range(2)]
                pg = _ps(pes, nc, 'pg', [128, 512], F32)
                pu = _ps(pes, nc, 'pu', [128, 512], F32)
                for ct in range(CT):
                    i = ct % 2
                    em.dma('pool', 'xg%d' % i, xg[i][:], hs[:, :], ['hs', 'idxi'], ['xg%d' % i],
                           indirect=dict(out_offset=None, in_offset=bass.IndirectOffsetOnAxis(ap=idxi[:, e, ct:ct + 1], axis=0)))
                    nb = max(1, KC // 8)
                    per = min(8, KC)
                    for b in range(nb):
                        pk = 'ptr%d' % (b % 2)
                        em.mm_multi([(ptr[b % 2][:, j, :], xg[i][:, (b * per + j) * 128:(b * per + j + 1) * 128], idb[:], True) for j in range(per)], ['xg%d' % i, 'idb'], [pk])
                        em.op('act', 'copy', [pk], ['xinT'], out=xinT[:, b * per:(b + 1) * per, ct * 128:(ct + 1) * 128], in_=ptr[b % 2][:, 0:per, :])
                for (f0, nfc) in [(0, 4), (4, FC - 4)]:
                    for kc in range(KC):
                        em.dma('pool', 'wgl', Wgh[:, kc, 0:nfc * 128], wg[e, kc * 128:(kc + 1) * 128, f0 * 128:(f0 + nfc) * 128], [], ['Wgh'], new_group=(kc == 0))
                        em.dma('pool', 'wul', Wuh[:, kc, 0:nfc * 128], wu[e, kc * 128:(kc + 1) * 128, f0 * 128:(f0 + nfc) * 128], [], ['Wuh'], new_group=(kc == 0))
                    for fcl in range(nfc):
                        for th in range(CAP // 512):
                            em.mm(pg[:, :], [(Wgh[:, kc, fcl * 128:(fcl + 1) * 128], xinT[:, kc, th * 512:(th + 1) * 512]) for kc in range(KC)], ['Wgh', 'xinT'], ['pg'])
                            em.mm(pu[:, :], [(Wuh[:, kc, fcl * 128:(fcl + 1) * 128], xinT[:, kc, th * 512:(th + 1) * 512]) for kc in range(KC)], ['Wuh', 'xinT'], ['pu'])
                            em.op('act', 'activation', ['pg'], ['sg'], out=sg[:], in_=pg[:, :], func=AF.Silu)
                            em.op('dve', 'tensor_tensor', ['pu', 'sg'], ['actT'], out=actT[:, f0 + fcl, th * 512:(th + 1) * 512], in0=pu[:, :], in1=sg[:], op=ALU.mult)
            em.barrier()
            with ExitStack() as pes:
                Wd = _sb(pes, nc, 'Wd', [128, FC, D], BF16)
                for fc in range(FC):
                    em.dma('pool', 'wdl', Wd[:, fc, :], wd[e, fc * 128:(fc + 1) * 128, :], [], ['Wd'], new_group=(fc == 0))
                ysb = [_sb(pes, nc, 'ysb%d' % i, [128, D], F32) for i in range(2)]
                py = [_ps(pes, nc, 'py%d' % i, [128, 512], F32) for i in range(2)]
                n = 0
                for tt in range(CT):
                    b = tt % 2
                    for cg in range(D // 512):
                        i = n % 2
                        n += 1
                        em.mm(py[i][:, :], [(actT[:, fc, tt * 128:(tt + 1) * 128], Wd[:, fc, cg * 512:(cg + 1) * 512]) for fc in range(FC)], ['actT', 'Wd'], ['py%d' % i])
                        em.op('dve', 'tensor_scalar', ['py%d' % i, 'gate'], ['ysb%d' % b], out=ysb[b][:, cg * 512:(cg + 1) * 512], in0=py[i][:, :], scalar1=gate[:, e, tt:tt + 1],
                              scalar2=None, op0=ALU.mult)
                    em.dma('sp', 'yo%d' % b, y[e, tt * 128:(tt + 1) * 128, :], ysb[b][:], ['ysb%d' % b], [])
            em.barrier()
        em.finish()
    return nc


def build_pcomb(cfg):
    S, E, CAP, CT = cfg.S, cfg.E, cfg.CAP, cfg.CT
    nc = bass.Bass("TRN2", target_bir_lowering=False)
    xm = nc.dram_tensor("xm", [S, 512], F32, kind="ExternalInput").ap()
    ys = nc.dram_tensor("ys", [E, CAP, 512], F32, kind="ExternalInput").ap()
    idx = nc.dram_tensor("idx", [E, 128, CT], I32, kind="ExternalInput").ap()
    xo = nc.dram_tensor("xo", [S, 512], F32, kind="ExternalOutput").ap()
    with ExitStack() as es:
        em = Em(nc, es)
        ii = _sb(es, nc, 'ii', [128, E, CT], I32)
        em.dma('sp', 'c0', ii[:], idx.rearrange("e p c -> p e c"), [], ['ii'])
        em.dma('sp', 'cp', xo[:, :], xm[:, :], [], ['xo'])
        yb = [_sb(es, nc, 'yb%d' % i, [128, 512], F32) for i in range(4)]
        n = 0
        for e in range(E):
            for ct in range(CT):
                i = n % 4
                n += 1
                em.dma('sp', 'yl%d' % i, yb[i][:], ys[e, ct * 128:(ct + 1) * 128, :], [], ['yb%d' % i])
                em.dma('pool', 'sc', xo[:, :], yb[i][:], ['yb%d' % i, 'ii', 'xo'], ['xo'] if ct == CT - 1 else [], new_group=(ct == 0),
                       indirect=dict(out_offset=bass.IndirectOffsetOnAxis(ap=ii[:, e, ct:ct + 1], axis=0), in_offset=None, compute_op=ALU.add))
        em.finish()
    return nc


def moe_inputs(cfg, x, gn, rw, wg, wu, wd):
    maps = []
    x = np.ascontiguousarray(x, dtype=np.float32)
    for c in range(cfg.NC):
        mine = [2 * c, 2 * c + 1]
        perm = mine + [e for e in range(cfg.E) if e not in mine]
        maps.append({"x": x, "gn": np.ascontiguousarray(gn), "rw": np.ascontiguousarray(rw[:, perm]),
                     "wg": np.ascontiguousarray(wg[mine]), "wu": np.ascontiguousarray(wu[mine]), "wd": np.ascontiguousarray(wd[mine])})
    return maps


_PROGS = {}


def _prog(name, fn):
    if name not in _PROGS:
        _PROGS[name] = fn()
    return _PROGS[name]


_PROF = []


def _run(nc, maps, n, tag=''):
    import os
    if os.environ.get('KPROF'):
        r = run_bass_kernel_spmd(nc, maps, core_ids=list(range(n)), trace=True)
        _PROF.append((tag, r.exec_time_ns))
        print('KPROF', tag, r.exec_time_ns, flush=True)
        return r.results
    return run_bass_kernel_spmd(nc, maps, core_ids=list(range(n))).results


def forward(cfg, inp):
    NC, S, D = cfg.NC, cfg.S, cfg.D
    x = np.ascontiguousarray(np.asarray(inp['x'], dtype=np.float32).reshape(S, D))
    for i in range(cfg.DEPTH):
        j = i // 2
        if i % 2 == 0:
            nc = _prog('gqa', lambda: build_att(cfg, 'gqa'))
            maps = att_inputs(cfg, 'gqa', x, inp['attn_norm'][i], np.asarray(inp['gqa_w_in'][j]),
                              dict(qg=np.asarray(inp['gqa_q_norm'][j]), kg=np.asarray(inp['gqa_k_norm'][j])))
            w_out = np.asarray(inp['gqa_w_out'][j])
        else:
            li = 0.8 - 0.6 * math.exp(-0.3 * i)
            nc = _prog('diff%d' % i, lambda: build_att(cfg, 'diff', lambda_init=li))
            maps = att_inputs(cfg, 'diff', x, inp['attn_norm'][i], np.asarray(inp['diff_w_in'][j]),
                              dict(lq1=np.asarray(inp['diff_lambda_q1'][j]), lk1=np.asarray(inp['diff_lambda_k1'][j]),
                                   lq2=np.asarray(inp['diff_lambda_q2'][j]), lk2=np.asarray(inp['diff_lambda_k2'][j]),
                                   subg=np.asarray(inp['diff_sub_norm'][j]), rel_bias=np.asarray(inp['rel_bias'])))
            w_out = np.asarray(inp['diff_w_out'][j])
        res = _run(nc, maps, NC, 'att%d' % i)
        oTf = np.ascontiguousarray(np.concatenate([np.asarray(r['oT']) for r in res], axis=0))
        TLc = S // NC
        J = S // 128
        nc = _prog('pout2', lambda: build_pout2(cfg))
        gn2 = np.ascontiguousarray(np.asarray(inp['ffn_norm'][i], dtype=np.float32))
        rwf = np.ascontiguousarray(np.asarray(inp['router_w'][i], dtype=np.float32))
        w_out = np.ascontiguousarray(w_out)
        maps = [{"oTs": np.ascontiguousarray(oTf[:, c * TLc:(c + 1) * TLc]), "wo": w_out, "xs": np.ascontiguousarray(x[c * TLc:(c + 1) * TLc]),
                 "gn": gn2, "rw": rwf} for c in range(NC)]
        res = _run(nc, maps, NC, 'pout%d' % i)
        xm = np.ascontiguousarray(np.concatenate([np.asarray(r['xm']) for r in res], axis=0))
        hbf = np.ascontiguousarray(np.concatenate([np.asarray(r['hb']) for r in res], axis=0))
        aff = np.concatenate([np.asarray(r['aff']) for r in res], axis=0)
        nc = _prog('moe', lambda: build_moe(cfg))
        wg_, wu_, wd_ = np.asarray(inp['expert_w_gate'][i]), np.asarray(inp['expert_w_up'][i]), np.asarray(inp['expert_w_down'][i])
        maps = []
        for c in range(NC):
            A_c = np.ascontiguousarray(aff[:, 2 * c:2 * c + 2].T.reshape(2, 128, J).transpose(1, 0, 2))
            maps.append({"hs": hbf, "A": A_c, "wg": np.ascontiguousarray(wg_[2 * c:2 * c + 2]), "wu": np.ascontiguousarray(wu_[2 * c:2 * c + 2]),
                         "wd": np.ascontiguousarray(wd_[2 * c:2 * c + 2])})
        res = _run(nc, maps, NC, 'moe%d' % i)
        ys = np.concatenate([np.asarray(r['y']) for r in res], axis=0)
        idx = np.ascontiguousarray(np.concatenate([np.asarray(r['idx']) for r in res], axis=0))
        nc = _prog('pcomb', lambda: build_pcomb(cfg))
        maps = [{"xm": np.ascontiguousarray(xm[:, c * 512:(c + 1) * 512]), "ys": np.ascontiguousarray(ys[:, :, c * 512:(c + 1) * 512]), "idx": idx} for c in range(NC)]
        res = _run(nc, maps, NC, 'pcomb%d' % i)
        x = np.ascontiguousarray(np.concatenate([np.asarray(r['xo']) for r in res], axis=1))
    nc = _prog('pfin', lambda: build_pfin(cfg))
    TL = S // NC
    maps = [{"x": np.ascontiguousarray(x[c * TL:(c + 1) * TL]), "g": np.ascontiguousarray(np.asarray(inp['final_norm'], dtype=np.float32))} for c in range(NC)]
    res = _run(nc, maps, NC)
    out = np.concatenate([np.asarray(r['y']) for r in res], axis=0)
    return out.reshape(1, S, D).astype(np.float32)


def kernel(**inputs):
    cfg = Cfg()
    return forward(cfg, inputs)
```
